# Optimizing a Trainium2 kernel written in Bass

```python
import jax, jax.numpy as jnp
from jax import lax
import numpy as np


D_MODEL = 1024
BATCH = 8
SEQ = 2048
DEPTH = 2

M_HEADS = 4
M_DQK = 128
M_DV = 256
CONV_K = 4
R_HEADS = 4
R_DQK = 128
R_DV = 256
CHUNK = 128
ROPE_BASE = 10000.0
N_GROUPS = 4
EXPERTS_PER_GROUP = 4
N_EXPERTS = N_GROUPS * EXPERTS_PER_GROUP
TOP_K = 2
D_FF_EXPERT = 512
EPS = 1e-6
MAX_POS_OFFSET = 1024

M_QK_W = M_HEADS * M_DQK
M_V_W = M_HEADS * M_DV
R_QK_W = R_HEADS * R_DQK
R_V_W = R_HEADS * R_DV
IN_SPLITS = (M_QK_W, M_QK_W, M_V_W, M_V_W, M_HEADS, M_HEADS, R_QK_W, R_QK_W, R_V_W, R_V_W, D_MODEL, D_MODEL)
D_IN = 2 * M_QK_W + 2 * M_V_W + 2 * M_HEADS + 2 * R_QK_W + 2 * R_V_W + 2 * D_MODEL

kernel_name = 'hybrid_mlstm_retention_hmoe_block'


def rms_norm(x, gain):
    xf = x.astype(jnp.float32)
    var = jnp.mean(xf * xf, axis=-1, keepdims=True)
    return (xf * lax.rsqrt(var + EPS)).astype(x.dtype) * gain


def split_cols(t, sizes):
    out, start = [], 0
    for s in sizes:
        out.append(t[..., start:start + s])
        start += s
    return out


def to_heads(t, n_heads):
    B, S, W = t.shape
    return jnp.swapaxes(t.astype(jnp.float32).reshape(B, S, n_heads, W // n_heads), 1, 2)


def causal_dwconv(x, w, b):
    C = x.shape[-1]
    y = lax.conv_general_dilated(x, w[:, None, :].astype(x.dtype), window_strides=(1,),
                                 padding=[(CONV_K - 1, 0)],
                                 dimension_numbers=('NWC', 'WIO', 'NWC'),
                                 feature_group_count=C)
    return y + b


def rotary(x, positions):
    d = x.shape[-1]
    inv = ROPE_BASE ** (-jnp.arange(0, d, 2, dtype=jnp.float32) / d)
    ang = positions.astype(jnp.float32)[..., None] * inv
    cos = jnp.cos(ang)[:, :, None, :]
    sin = jnp.sin(ang)[:, :, None, :]
    x1, x2 = x[..., : d // 2], x[..., d // 2:]
    return jnp.concatenate([x1 * cos - x2 * sin, x1 * sin + x2 * cos], axis=-1)


def mlstm_chunkwise(q, k, v, i_pre, f_pre):
    B, H, S, dk = q.shape
    dv = v.shape[-1]
    L = CHUNK
    NC = S // L
    q = q.reshape(B, H, NC, L, dk)
    k = k.reshape(B, H, NC, L, dk) * (dk ** -0.5)
    v = v.reshape(B, H, NC, L, dv)
    ig = i_pre.reshape(B, H, NC, L)
    lf = jax.nn.log_sigmoid(f_pre).reshape(B, H, NC, L)
    b = jnp.cumsum(lf, axis=-1)
    b_tot = b[..., -1]
    a = b_tot[..., None] - b + ig

    def step(carry, xs):
        C, n, m = carry
        bt, a_c, k_c, v_c = xs
        m_new = jnp.maximum(bt + m, jnp.max(a_c, axis=-1))
        decay = jnp.exp(bt + m - m_new)
        w = jnp.exp(a_c - m_new[..., None])
        C_new = decay[..., None, None] * C + jnp.einsum('bhl,bhld,bhle->bhde', w, k_c, v_c)
        n_new = decay[..., None] * n + jnp.einsum('bhl,bhld->bhd', w, k_c)
        return (C_new, n_new, m_new), (C, n, m)

    init = (jnp.zeros((B, H, dk, dv), jnp.float32), jnp.zeros((B, H, dk), jnp.float32),
            jnp.zeros((B, H), jnp.float32))
    xs = (jnp.moveaxis(b_tot, 2, 0), jnp.moveaxis(a, 2, 0), jnp.moveaxis(k, 2, 0), jnp.moveaxis(v, 2, 0))
    _, (C_st, n_st, m_st) = lax.scan(step, init, xs)
    C_st = jnp.moveaxis(C_st, 0, 2)
    n_st = jnp.moveaxis(n_st, 0, 2)
    m_st = jnp.moveaxis(m_st, 0, 2)

    causal = jnp.tril(jnp.ones((L, L), dtype=bool))
    log_d = jnp.where(causal, b[..., :, None] - b[..., None, :] + ig[..., None, :], -jnp.inf)
    log_inter = b + m_st[..., None]
    m_t = jnp.maximum(log_inter, jnp.max(log_d, axis=-1))
    d_mat = jnp.exp(log_d - m_t[..., None])
    s = jnp.einsum('bhcld,bhcsd->bhcls', q, k) * d_mat
    inter_w = jnp.exp(log_inter - m_t)
    num = jnp.einsum('bhcls,bhcse->bhcle', s, v) + inter_w[..., None] * jnp.einsum('bhcld,bhcde->bhcle', q, C_st)
    den = jnp.sum(s, axis=-1) + inter_w * jnp.einsum('bhcld,bhcd->bhcl', q, n_st)
    h = num / jnp.maximum(jnp.abs(den), jnp.exp(-m_t))[..., None]
    return h.reshape(B, H, S, dv)


def retention_chunkwise(q, k, v):
    B, H, S, dk = q.shape
    dv = v.shape[-1]
    L = CHUNK
    NC = S // L
    log_g = jnp.log(1.0 - 2.0 ** (-5.0 - jnp.arange(H, dtype=jnp.float32)))
    q = q.reshape(B, H, NC, L, dk)
    k = k.reshape(B, H, NC, L, dk) * (dk ** -0.5)
    v = v.reshape(B, H, NC, L, dv)
    idx = jnp.arange(L, dtype=jnp.float32)
    rel = idx[:, None] - idx[None, :]
    causal = rel >= 0
    d_mat = jnp.where(causal, jnp.exp(log_g[:, None, None] * jnp.where(causal, rel, 0.0)), 0.0)
    s = jnp.einsum('bhcld,bhcsd->bhcls', q, k) * d_mat[None, :, None]
    inner = jnp.einsum('bhcls,bhcse->bhcle', s, v)
    xi = jnp.exp(log_g[:, None] * (idx + 1.0))
    zeta = jnp.exp(log_g[:, None] * (L - 1.0 - idx))
    chunk_gamma = jnp.exp(log_g * L)
    kv = jnp.einsum('hs,bhcsd,bhcse->bhcde', zeta, k, v)

    def step(R, kv_c):
        return chunk_gamma[None, :, None, None] * R + kv_c, R

    _, R_st = lax.scan(step, jnp.zeros((B, H, dk, dv), jnp.float32), jnp.moveaxis(kv, 2, 0))
    R_st = jnp.moveaxis(R_st, 0, 2)
    cross = jnp.einsum('bhcld,bhcde->bhcle', q, R_st) * xi[None, :, None, :, None]
    return (inner + cross).reshape(B, H, S, dv)


def hybrid_mixer(h, positions, w_in, conv_w, conv_b, m_ig_b, m_fg_b, m_norm_g, r_norm_g, w_bm, w_br, w_out):
    B, S, _ = h.shape
    f32 = jnp.float32
    proj = h @ w_in
    mq, mk, mv, mo, mi, mf, rq, rk, rv, rg, ga, gb = split_cols(proj, IN_SPLITS)
    qk = jax.nn.silu(causal_dwconv(jnp.concatenate([mq, mk], axis=-1), conv_w, conv_b))
    mq, mk = qk[..., :M_QK_W], qk[..., M_QK_W:]
    hm = mlstm_chunkwise(to_heads(mq, M_HEADS), to_heads(mk, M_HEADS), to_heads(mv, M_HEADS),
                         jnp.swapaxes((mi + m_ig_b).astype(f32), 1, 2),
                         jnp.swapaxes((mf + m_fg_b).astype(f32), 1, 2))
    hm = rms_norm(jnp.swapaxes(hm, 1, 2), m_norm_g) * jax.nn.sigmoid(mo.astype(f32).reshape(B, S, M_HEADS, M_DV))
    ym = hm.reshape(B, S, M_V_W).astype(h.dtype)
    rq = rotary(rq.astype(f32).reshape(B, S, R_HEADS, R_DQK), positions)
    rk = rotary(rk.astype(f32).reshape(B, S, R_HEADS, R_DQK), positions)
    hr = retention_chunkwise(jnp.swapaxes(rq, 1, 2), jnp.swapaxes(rk, 1, 2), to_heads(rv, R_HEADS))
    hr = rms_norm(jnp.swapaxes(hr, 1, 2), r_norm_g) * jax.nn.silu(rg.astype(f32).reshape(B, S, R_HEADS, R_DV))
    yr = hr.reshape(B, S, R_V_W).astype(h.dtype)
    y = jax.nn.sigmoid(ga) * (ym @ w_bm) + jax.nn.sigmoid(gb) * (yr @ w_br)
    return y @ w_out


def hier_moe(h, w_r1, b_r1, w_r2, b_r2, w_gate, w_up, w_down):
    B, S, D = h.shape
    t = h.reshape(B * S, D)
    T = t.shape[0]
    lg1 = (t @ w_r1).astype(jnp.float32) + b_r1
    p1 = jax.nn.softmax(lg1, axis=-1)
    _, g_top = lax.top_k(lg1, 1)
    p_g = jnp.take_along_axis(p1, g_top, axis=-1)
    lg2 = ((t @ w_r2).astype(jnp.float32) + b_r2).reshape(T, N_GROUPS, EXPERTS_PER_GROUP)
    lg2_sel = jnp.take_along_axis(lg2, g_top[:, :, None], axis=1)[:, 0]
    top_v, top_i = lax.top_k(lg2_sel, TOP_K)
    w2 = jax.nn.softmax(top_v, axis=-1) * p_g
    expert_id = g_top * EXPERTS_PER_GROUP + top_i
    gates = jnp.sum(jax.nn.one_hot(expert_id, N_EXPERTS, dtype=jnp.float32) * w2[..., None], axis=1)
    out = jnp.zeros((T, D), jnp.float32)
    for e in range(N_EXPERTS):
        he = jax.nn.silu(t @ w_gate[e]) * (t @ w_up[e])
        out = out + gates[:, e:e + 1] * (he @ w_down[e]).astype(jnp.float32)
    return out.astype(h.dtype).reshape(B, S, D)


def setup_inputs(seed: int = 0) -> dict:
    key = jax.random.key(seed)
    ks = jax.random.split(key, 26)
    f32 = jnp.float32

    def nrm(k, shape, scale):
        return jax.random.normal(k, shape, f32) * scale

    x = nrm(ks[0], (BATCH, SEQ, D_MODEL), 1.0)
    c = nrm(ks[1], (BATCH, D_MODEL), 1.0)
    positions = (jax.random.randint(ks[2], (BATCH, 1), 0, MAX_POS_OFFSET, dtype=jnp.int32)
                 + jnp.arange(SEQ, dtype=jnp.int32)[None, :])
    w_ada = nrm(ks[3], (DEPTH, D_MODEL, 6 * D_MODEL), 0.5 * D_MODEL ** -0.5)
    b_ada = nrm(ks[4], (DEPTH, 6 * D_MODEL), 0.02)
    norm1_g = 1.0 + nrm(ks[5], (DEPTH, D_MODEL), 0.1)
    w_in = nrm(ks[6], (DEPTH, D_MODEL, D_IN), D_MODEL ** -0.5)
    conv_w = nrm(ks[7], (DEPTH, CONV_K, 2 * M_QK_W), CONV_K ** -0.5)
    conv_b = nrm(ks[8], (DEPTH, 2 * M_QK_W), 0.02)
    m_ig_b = nrm(ks[9], (DEPTH, M_HEADS), 0.1)
    m_fg_b = jnp.linspace(3.0, 6.0, M_HEADS, dtype=f32)[None, :] + nrm(ks[10], (DEPTH, M_HEADS), 0.1)
    m_norm_g = 1.0 + nrm(ks[11], (DEPTH, M_HEADS, M_DV), 0.1)
    r_norm_g = 1.0 + nrm(ks[12], (DEPTH, R_HEADS, R_DV), 0.1)
    w_bm = nrm(ks[13], (DEPTH, M_V_W, D_MODEL), M_V_W ** -0.5)
    w_br = nrm(ks[14], (DEPTH, R_V_W, D_MODEL), R_V_W ** -0.5)
    w_out = nrm(ks[15], (DEPTH, D_MODEL, D_MODEL), D_MODEL ** -0.5)
    norm2_g = 1.0 + nrm(ks[16], (DEPTH, D_MODEL), 0.1)
    w_r1 = nrm(ks[17], (DEPTH, D_MODEL, N_GROUPS), D_MODEL ** -0.5)
    b_r1 = nrm(ks[18], (DEPTH, N_GROUPS), 0.01)
    w_r2 = nrm(ks[19], (DEPTH, D_MODEL, N_EXPERTS), D_MODEL ** -0.5)
    b_r2 = nrm(ks[20], (DEPTH, N_EXPERTS), 0.01)
    w_gate = nrm(ks[21], (DEPTH, N_EXPERTS, D_MODEL, D_FF_EXPERT), D_MODEL ** -0.5)
    w_up = nrm(ks[22], (DEPTH, N_EXPERTS, D_MODEL, D_FF_EXPERT), D_MODEL ** -0.5)
    w_down = nrm(ks[23], (DEPTH, N_EXPERTS, D_FF_EXPERT, D_MODEL), D_FF_EXPERT ** -0.5)
    final_g = 1.0 + nrm(ks[24], (D_MODEL,), 0.1)
    return {'x': x, 'c': c, 'positions': positions, 'w_ada': w_ada, 'b_ada': b_ada, 'norm1_g': norm1_g,
            'w_in': w_in, 'conv_w': conv_w, 'conv_b': conv_b, 'm_ig_b': m_ig_b, 'm_fg_b': m_fg_b,
            'm_norm_g': m_norm_g, 'r_norm_g': r_norm_g, 'w_bm': w_bm, 'w_br': w_br, 'w_out': w_out,
            'norm2_g': norm2_g, 'w_r1': w_r1, 'b_r1': b_r1, 'w_r2': w_r2, 'b_r2': b_r2,
            'w_gate': w_gate, 'w_up': w_up, 'w_down': w_down, 'final_g': final_g}


def reference(x, c, positions, w_ada, b_ada, norm1_g, w_in, conv_w, conv_b, m_ig_b, m_fg_b, m_norm_g,
              r_norm_g, w_bm, w_br, w_out, norm2_g, w_r1, b_r1, w_r2, b_r2, w_gate, w_up, w_down, final_g):
    for l in range(DEPTH):
        mod = jax.nn.silu(c) @ w_ada[l] + b_ada[l]
        sh1, sc1, g1, sh2, sc2, g2 = jnp.split(mod[:, None, :], 6, axis=-1)
        h = rms_norm(x, norm1_g[l]) * (1.0 + sc1) + sh1
        x = x + g1 * hybrid_mixer(h, positions, w_in[l], conv_w[l], conv_b[l], m_ig_b[l], m_fg_b[l],
                                  m_norm_g[l], r_norm_g[l], w_bm[l], w_br[l], w_out[l])
        h = rms_norm(x, norm2_g[l]) * (1.0 + sc2) + sh2
        x = x + g2 * hier_moe(h, w_r1[l], b_r1[l], w_r2[l], b_r2[l], w_gate[l], w_up[l], w_down[l])
    return rms_norm(x, final_g)
```

```python
import math
import types
import numpy as np
from contextlib import ExitStack
import concourse.bass as bass
import concourse.mybir as mybir
from concourse.bass_utils import run_bass_kernel_spmd

F32 = mybir.dt.float32
BF16 = mybir.dt.bfloat16
I32 = mybir.dt.int32
AF = mybir.ActivationFunctionType
ALU = mybir.AluOpType
AX = mybir.AxisListType

S_LEN = 2048
D = 1024
NCORES = 8
EPS = 1e-6
KAPPA_M = 0.25 * 128 ** -0.5
KAPPA_R = 128 ** -0.5
OFF_MQ, OFF_MK, OFF_MV, OFF_MO, OFF_MI = 0, 512, 1024, 2048, 3072
OFF_RQ, OFF_RK, OFF_RV, OFF_RG, OFF_GA, OFF_GB = 3080, 3592, 4104, 5128, 6152, 7176


def freeze(fn):
    cells = fn.__closure__
    if not cells:
        return fn
    new = []
    for c in cells:
        try:
            new.append(types.CellType(c.cell_contents))
        except ValueError:
            new.append(c)
    return types.FunctionType(fn.__code__, fn.__globals__, fn.__name__, fn.__defaults__, tuple(new))


class Buf:
    __slots__ = ("name", "w", "r", "dsem", "dcnt")

    def __init__(self, name):
        self.name = name
        self.w = None
        self.r = []
        self.dsem = None
        self.dcnt = 0


class Sched:
    ENG = ("pe", "act", "dve", "pool", "sp")

    def __init__(self, nc, stack):
        self.nc = nc
        self.stack = stack
        self.h = {"pe": nc.tensor, "act": nc.scalar, "dve": nc.vector, "pool": nc.gpsimd, "sp": nc.sync}
        self.sem = {k: stack.enter_context(nc.semaphore("s_" + k)) for k in self.ENG}
        self.cnt = {k: 0 for k in self.ENG}
        self.seen = {k: {} for k in self.ENG}
        self.prog = {k: [] for k in self.ENG}
        self.dbufs = []

    def _deps(self, eng, reads, writes):
        need = {}

        def add(ev):
            if ev is None:
                return
            s, v = ev
            if need.get(s, 0) < v:
                need[s] = v
        for b in reads:
            add(b.w)
        for b in writes:
            add(b.w)
            for ev in b.r:
                add(ev)
        out = []
        seen = self.seen[eng]
        own = self.sem[eng]
        for s, v in need.items():
            if eng == "pe" and s is own:
                continue
            if seen.get(s, 0) < v:
                seen[s] = v
                out.append((s, v))
        return out

    def _record(self, ev, reads, writes):
        for b in reads:
            b.r.append(ev)
        for b in writes:
            b.w = ev
            b.r = []

    def op(self, eng, fn, reads=(), writes=()):
        fn = freeze(fn)
        waits = self._deps(eng, reads, writes)
        self.cnt[eng] += 1
        ev = (self.sem[eng], self.cnt[eng])
        h = self.h[eng]
        sem = self.sem[eng]

        def thunk():
            for s, v in waits:
                h.wait_ge(s, v)
            fn(h).then_inc(sem, 1)
        self.prog[eng].append(thunk)
        self._record(ev, reads, writes)
        return ev

    def dma(self, eng, fn, reads=(), writes=(), track=None):
        fn = freeze(fn)
        waits = self._deps(eng, reads, writes)
        tb = track if track is not None else writes[0]
        if tb.dsem is None:
            tb.dsem = self.stack.enter_context(self.nc.semaphore("d_%d" % len(self.dbufs)))
            self.dbufs.append(tb)
        tb.dcnt += 16
        ev = (tb.dsem, tb.dcnt)
        h = self.h[eng]
        dsem = tb.dsem

        def thunk():
            for s, v in waits:
                h.wait_ge(s, v)
            fn(h).then_inc(dsem, 16)
        self.prog[eng].append(thunk)
        self._record(ev, reads, writes)
        return ev

    def barrier(self, engs=None):
        evs = [(self.sem[k], self.cnt[k]) for k in self.ENG if self.cnt[k] > 0]
        evs += [(b.dsem, b.dcnt) for b in self.dbufs]
        for eng in (engs or self.ENG):
            seen = self.seen[eng]
            waits = []
            for s, v in evs:
                if seen.get(s, 0) < v:
                    seen[s] = v
                    waits.append((s, v))
            h = self.h[eng]

            def thunk(h=h, waits=waits):
                for s, v in waits:
                    h.wait_ge(s, v)
            self.prog[eng].append(thunk)

    def emit(self):
        nc = self.nc
        with nc.Block() as block:
            @block.tensor
            def _(e):
                for f in self.prog["pe"]:
                    f()

            @block.scalar
            def _(e):
                for f in self.prog["act"]:
                    f()

            @block.vector
            def _(e):
                for f in self.prog["dve"]:
                    f()

            @block.gpsimd
            def _(e):
                for f in self.prog["pool"]:
                    f()

            @block.sync
            def _(e):
                for f in self.prog["sp"]:
                    f()


def build(stop_after=None, dumps=()):
    nc = bass.Bass("TRN2", target_bir_lowering=False)
    dt_in = lambda name, shape, dt=F32: nc.dram_tensor(name, list(shape), dt, kind="ExternalInput").ap()
    X = dt_in("x", [S_LEN, D])
    CT = dt_in("cT", [128, 8])
    POS = dt_in("pos", [128, S_LEN], I32)
    W_ADA = dt_in("w_ada", [2, D, 6 * D])
    B_ADAT = dt_in("b_adaT", [128, 96])
    N1G = dt_in("n1gT", [128, 16])
    N2G = dt_in("n2gT", [128, 16])
    W_IN = dt_in("w_in", [2, D, 8200])
    CONVW = dt_in("convwT", [128, 64])
    CONVB = dt_in("convbT", [128, 16])
    GBIAS = dt_in("gbias", [128, 2 * 8 * 8])
    NGT = dt_in("ngT", [128, 32])
    FING = dt_in("fing", [128, D])
    W_BM = dt_in("w_bm", [2, D, D])
    W_BR = dt_in("w_br", [2, D, D])
    W_OUT = dt_in("w_out", [2, D, D])
    WR = dt_in("wr", [128, 2 * 8 * 20])
    BR = dt_in("br", [128, 2 * 4 * 20])
    W_GATE = dt_in("w_gate", [2, 16, D, 512])
    W_UP = dt_in("w_up", [2, 16, D, 512])
    W_DOWN = dt_in("w_down", [2, 16, 512, D])
    C_ID = dt_in("c_ident", [128, 128])
    C_TRI = dt_in("c_tri", [128, 128])
    C_MASKM = dt_in("c_maskm", [128, 128])
    C_RETD = dt_in("c_retd", [128, 512])
    C_XI = dt_in("c_xi", [128, 512])
    C_ZETA = dt_in("c_zeta", [128, 4])
    C_INVF = dt_in("c_invf", [128, 1])
    C_SEL = dt_in("c_sel", [16, 16 * 128])
    OUT = nc.dram_tensor("out", [S_LEN, D], F32, kind="ExternalOutput").ap()
    dump_out = {}

    with ExitStack() as st:
        S = Sched(nc, st)
        _uid = [0]

        def sbt(stack, name, shape, dt=F32):
            _uid[0] += 1
            return stack.enter_context(nc.sbuf_tensor("%s_u%d" % (name, _uid[0]), list(shape), dt))
        bank = [st.enter_context(nc.psum_tensor("bank%d" % i, [128, 512], F32)) for i in range(8)]
        pb = [Buf("pb%d" % i) for i in range(8)]
        final_evs = []

        def dump(name, ap, buf, shape, dt=F32):
            if name not in dumps:
                return
            t = nc.dram_tensor("dbg_" + name, list(shape), dt, kind="ExternalOutput").ap()
            ob = Buf("dbg_" + name)
            S.dma("sp", lambda h: h.dma_start(out=t, in_=ap), reads=[buf], writes=[ob])
            final_evs.append(ob)
            dump_out[name] = True

        xT = sbt(st, "xT", [128, 8, S_LEN]); b_xT = [Buf("xT%d" % g) for g in range(4)]
        ident = sbt(st, "ident", [128, 128]); identb = sbt(st, "identb", [128, 128], BF16)
        tri = sbt(st, "tri", [128, 128]); ones32 = sbt(st, "ones32", [128, 128])
        maskm = sbt(st, "maskm", [128, 128]); retd = sbt(st, "retd", [128, 4, 128]); xi = sbt(st, "xi", [128, 4, 128])
        zeta = sbt(st, "zeta", [128, 4]); mhalf = sbt(st, "mhalf", [128, 1])
        modT = sbt(st, "modT", [128, 2, 48]); b_adaT = sbt(st, "b_adaT_s", [128, 96])
        n1g = sbt(st, "n1g", [128, 16]); n2g = sbt(st, "n2g", [128, 16])
        scale1 = sbt(st, "scale1", [128, 2, 8]); scale2 = sbt(st, "scale2", [128, 2, 8]); g1h = sbt(st, "g1h", [128, 2, 8])
        convw = sbt(st, "convw", [128, 2, 4, 8]); convb = sbt(st, "convb", [128, 2, 8])
        gbias = sbt(st, "gbias_s", [128, 2, 8, 8])
        ngT = sbt(st, "ngT_s", [128, 2, 2, 8])
        cosT = sbt(st, "cosT", [128, S_LEN]); sinS = sbt(st, "sinS", [128, S_LEN])
        C32 = [sbt(st, "C32_%d" % i, [128, 260]) for i in range(4)]
        Cb = [sbt(st, "Cb_%d" % i, [128, 260], BF16) for i in range(4)]
        R32 = [sbt(st, "R32_%d" % i, [128, 256]) for i in range(4)]
        Rb = [sbt(st, "Rb_%d" % i, [128, 256], BF16) for i in range(4)]
        halo = sbt(st, "halo", [128, 8, 4])
        b_const = Buf("const"); b_mod = Buf("mod"); b_cs = Buf("cossin")
        b_C = [Buf("C%d" % i) for i in range(4)]; b_Cb = [Buf("Cb%d" % i) for i in range(4)]
        b_R = [Buf("R%d" % i) for i in range(4)]; b_Rb = [Buf("Rb%d" % i) for i in range(4)]
        b_halo = [Buf("halo%d" % i) for i in range(8)]

        def load_const(dst, src, buf):
            S.dma("sp", lambda h: h.dma_start(out=dst, in_=src), writes=[buf])

        cb = {}
        for nm, dst, src in [("ident", ident[:], C_ID), ("tri", tri[:], C_TRI), ("maskm", maskm[:], C_MASKM),
                             ("retd", retd[:], C_RETD.rearrange("p (h l) -> p h l", h=4)),
                             ("xi", xi[:], C_XI.rearrange("p (h l) -> p h l", h=4)), ("zeta", zeta[:], C_ZETA),
                             ("b_adaT", b_adaT[:], B_ADAT), ("n1g", n1g[:], N1G), ("n2g", n2g[:], N2G),
                             ("convw", convw[:], CONVW.rearrange("p (l j k) -> p l j k", l=2, j=4)),
                             ("convb", convb[:], CONVB.rearrange("p (l k) -> p l k", l=2)),
                             ("gbias", gbias[:], GBIAS.rearrange("p (l c g) -> p l c g", l=2, c=8)),
                             ("ngT", ngT[:], NGT.rearrange("p (b l k) -> p b l k", b=2, l=2)),
                             ]:
            cb[nm] = Buf("c_" + nm)
            load_const(dst, src, cb[nm])
        S.op("dve", lambda h: h.tensor_copy(out=identb[:], in_=ident[:]), reads=[cb["ident"]], writes=[b_const])
        S.op("dve", lambda h: h.memset(ones32[:], 1.0), writes=[b_const])
        S.op("dve", lambda h: h.memset(mhalf[:], -0.5), writes=[b_const])
        ALLC = list(cb.values()) + [b_const]

        with ExitStack() as p0:
            xs = [sbt(p0, "xs%d" % i, [128, D]) for i in range(2)]
            b_xs = [Buf("xs%d" % i) for i in range(2)]
            for t in range(16):
                S.dma("sp", lambda h, t=t: h.dma_start(out=xs[t % 2][:], in_=X[t * 128:(t + 1) * 128, :]), writes=[b_xs[t % 2]])
                for hb in range(2):
                    def tr(h, t=t, hb=hb):
                        ins = None
                        for kk in range(4):
                            k = hb * 4 + kk
                            ins = h.transpose(bank[hb][:, kk * 128:(kk + 1) * 128], xs[t % 2][:, k * 128:(k + 1) * 128], ident[:])
                        return ins
                    S.op("pe", tr, reads=[b_xs[t % 2], cb["ident"]], writes=[pb[hb]])
                    eng = "act" if hb == 0 else "dve"
                    if eng == "act":
                        S.op("act", lambda h, t=t, hb=hb: h.activation(
                            out=xT[:, hb * 4:(hb + 1) * 4, t * 128:(t + 1) * 128],
                            in_=bank[hb][:].rearrange("p (k t) -> p k t", k=4), func=AF.Copy),
                            reads=[], writes=[pb[hb], b_xT[t // 4]])
                    else:
                        S.op("dve", lambda h, t=t, hb=hb: h.tensor_copy(
                            out=xT[:, hb * 4:(hb + 1) * 4, t * 128:(t + 1) * 128],
                            in_=bank[hb][:].rearrange("p (k t) -> p k t", k=4)),
                            reads=[], writes=[pb[hb], b_xT[t // 4]])
            cT = sbt(p0, "cT_s", [128, 8]); cth = sbt(p0, "cth", [128, 8]); csil = sbt(p0, "csil", [128, 8])
            b_c = Buf("c")
            S.dma("sp", lambda h: h.dma_start(out=cT[:], in_=CT), writes=[b_c])
            S.op("act", lambda h: h.activation(out=cth[:], in_=cT[:], func=AF.Tanh, scale=0.5), reads=[b_c], writes=[b_c])
            S.op("dve", lambda h: h.scalar_tensor_tensor(out=csil[:], in0=cth[:], scalar=1.0, in1=cT[:], op0=ALU.add, op1=ALU.mult),
                 reads=[b_c], writes=[b_c])
            S.op("dve", lambda h: h.tensor_scalar(out=csil[:], in0=csil[:], scalar1=0.5, scalar2=None, op0=ALU.mult),
                 reads=[b_c], writes=[b_c])
            wa = [sbt(p0, "wa%d" % i, [128, 8, 512], BF16) for i in range(4)]
            csilb = sbt(p0, "csilb", [128, 8], BF16)
            S.op("dve", lambda h: h.tensor_copy(out=csilb[:], in_=csil[:]), reads=[b_c], writes=[b_c])
            b_wa = [Buf("wa%d" % i) for i in range(4)]
            modrow = sbt(p0, "modrow", [1, 6 * D]); b_mrow = Buf("modrow")
            for l in range(2):
                for jg in range(12):
                    i = (l * 12 + jg) % 4
                    S.dma("pool", lambda h, l=l, jg=jg, i=i: h.dma_start(
                        out=wa[i][:], in_=W_ADA[l, :, jg * 512:(jg + 1) * 512].rearrange("(k p) n -> p k n", p=128)),
                        writes=[b_wa[i]])

                    pbj = 6 + (jg % 2)

                    def mm(h, i=i, pbj=pbj):
                        ins = None
                        for k in range(8):
                            ins = h.matmul(bank[pbj][0:1, :], lhsT=csilb[:, k:k + 1], rhs=wa[i][:, k, :], start=(k == 0), stop=(k == 7))
                        return ins
                    S.op("pe", mm, reads=[b_wa[i], b_c], writes=[pb[pbj]])
                    S.op("act", lambda h, jg=jg, pbj=pbj: h.activation(out=modrow[0:1, jg * 512:(jg + 1) * 512], in_=bank[pbj][0:1, :], func=AF.Copy),
                         writes=[pb[pbj], b_mrow])

                def mtr(h):
                    ins = None
                    for j in range(48):
                        ins = h.matmul(bank[5][:, j:j + 1], lhsT=modrow[0:1, j * 128:(j + 1) * 128], rhs=ones32[0:1, 0:1], start=True, stop=True)
                    return ins
                S.op("pe", mtr, reads=[b_mrow, b_const], writes=[pb[5]])
                S.op("dve", lambda h, l=l: h.tensor_tensor(out=modT[:, l, :], in0=bank[5][:, 0:48], in1=b_adaT[:, l * 48:(l + 1) * 48],
                                                          op=ALU.add), reads=[cb["b_adaT"]], writes=[pb[5], b_mod])
                S.op("dve", lambda h, l=l: h.scalar_tensor_tensor(out=scale1[:, l, :], in0=modT[:, l, 8:16], scalar=1.0,
                                                                 in1=n1g[:, l * 8:(l + 1) * 8], op0=ALU.add, op1=ALU.mult),
                     reads=[cb["n1g"]], writes=[b_mod])
                S.op("dve", lambda h, l=l: h.tensor_scalar(out=scale1[:, l, :], in0=scale1[:, l, :], scalar1=32.0, scalar2=None, op0=ALU.mult),
                     writes=[b_mod])
                S.op("dve", lambda h, l=l: h.scalar_tensor_tensor(out=scale2[:, l, :], in0=modT[:, l, 32:40], scalar=1.0,
                                                                 in1=n2g[:, l * 8:(l + 1) * 8], op0=ALU.add, op1=ALU.mult),
                     reads=[cb["n2g"]], writes=[b_mod])
                S.op("dve", lambda h, l=l: h.tensor_scalar(out=scale2[:, l, :], in0=scale2[:, l, :], scalar1=32.0, scalar2=None, op0=ALU.mult),
                     writes=[b_mod])
                S.op("dve", lambda h, l=l: h.tensor_scalar(out=g1h[:, l, :], in0=modT[:, l, 16:24], scalar1=0.5, scalar2=None, op0=ALU.mult),
                     writes=[b_mod])
            posi = sbt(p0, "posi", [128, S_LEN], I32); ang = sbt(p0, "ang", [128, S_LEN]); rr = sbt(p0, "rr", [128, S_LEN])
            kk_i = sbt(p0, "kk_i", [128, S_LEN], I32); invf = sbt(p0, "invf", [128, 1])
            b_r = Buf("rot")
            S.dma("sp", lambda h: h.dma_start(out=posi[:], in_=POS), writes=[b_r])
            S.dma("sp", lambda h: h.dma_start(out=invf[:], in_=C_INVF), writes=[b_r], track=Buf("invf"))
            S.op("dve", lambda h: h.tensor_copy(out=ang[:], in_=posi[:]), reads=[b_r], writes=[b_r])
            S.op("dve", lambda h: h.tensor_scalar(out=ang[:], in0=ang[:], scalar1=invf[:, 0:1], scalar2=None, op0=ALU.mult), writes=[b_r])
            S.op("dve", lambda h: h.tensor_scalar(out=kk_i[:], in0=ang[:], scalar1=1.0 / (2 * math.pi), scalar2=None, op0=ALU.mult), writes=[b_r])
            S.op("dve", lambda h: h.tensor_copy(out=rr[:], in_=kk_i[:]), writes=[b_r])
            S.op("dve", lambda h: h.scalar_tensor_tensor(out=ang[:], in0=rr[:], scalar=-2 * math.pi, in1=ang[:], op0=ALU.mult, op1=ALU.add),
                 writes=[b_r])

            def wrap(src):
                S.op("dve", lambda h: h.tensor_scalar(out=rr[:], in0=src[:], scalar1=math.pi, scalar2=None, op0=ALU.is_gt), writes=[b_r])
                S.op("dve", lambda h: h.scalar_tensor_tensor(out=src[:], in0=rr[:], scalar=-2 * math.pi, in1=src[:], op0=ALU.mult, op1=ALU.add),
                     writes=[b_r])
                S.op("dve", lambda h: h.tensor_scalar(out=rr[:], in0=src[:], scalar1=-math.pi, scalar2=None, op0=ALU.is_lt), writes=[b_r])
                S.op("dve", lambda h: h.scalar_tensor_tensor(out=src[:], in0=rr[:], scalar=2 * math.pi, in1=src[:], op0=ALU.mult, op1=ALU.add),
                     writes=[b_r])
            wrap(ang)
            S.op("act", lambda h: h.activation(out=sinS[:], in_=ang[:], func=AF.Sin), reads=[b_r], writes=[b_cs])
            S.op("dve", lambda h: h.tensor_scalar(out=ang[:], in0=ang[:], scalar1=math.pi / 2, scalar2=None, op0=ALU.add), reads=[b_cs], writes=[b_r])
            wrap(ang)
            S.op("act", lambda h: h.activation(out=cosT[:], in_=ang[:], func=AF.Sin), reads=[b_r], writes=[b_cs])
            S.op("act", lambda h: h.mul(out=sinS[0:64, :], in_=sinS[0:64, :], mul=-1.0), writes=[b_cs])
            dump("modT", modT[:], b_mod, [128, 2, 48])
            dump("cosT", cosT[:], b_cs, [128, S_LEN])
            dump("sinS", sinS[:], b_cs, [128, S_LEN])
            S.barrier()
        if stop_after == "p0":
            return finish(nc, S, st, final_evs, xT, b_xT, bank, pb, ident, FING, cb, mhalf, OUT, sbt)

        for l in range(2):
            with ExitStack() as mx:
                hT = sbt(mx, "hT", [128, 8, 1024], BF16); b_hT = [Buf("hT%d" % i) for i in range(2)]
                ymT = sbt(mx, "ymT", [128, 8, 1024], BF16); b_ymT = [Buf("ymT%d" % i) for i in range(8)]
                yT = sbt(mx, "yT", [128, 8, 1024], BF16); b_yT = [Buf("yT%d" % i) for i in range(2)]
                wq = [sbt(mx, "wq%d" % i, [128, 8, 128], BF16) for i in range(2)]; b_wq = [Buf("wq%d" % i) for i in range(2)]
                wk = [sbt(mx, "wk%d" % i, [128, 8, 128], BF16) for i in range(2)]; b_wk = [Buf("wk%d" % i) for i in range(2)]
                wvo = [sbt(mx, "wvo%d" % i, [128, 8, 512], BF16) for i in range(2)]; b_wvo = [Buf("wvo%d" % i) for i in range(2)]
                wif = sbt(mx, "wif", [128, 8, 8], BF16); b_wif = Buf("wif")
                pad = [sbt(mx, "pad%d" % i, [128, 516]) for i in range(2)]; b_pad = [Buf("pad%d" % i) for i in range(2)]
                acc = sbt(mx, "acc", [128, 512]); th = sbt(mx, "th", [128, 512]); b_acc = Buf("acc"); b_th = Buf("th")
                sq = [acc, th]; b_sq = [b_acc, b_th]
                ssb = pad[0][:, 0:512]; rstd = pad[1][:, 0:512]; b_ss = b_pad[0]; b_rstd = b_pad[1]
                qTs = [sbt(mx, "qT%d" % i, [128, 1024], BF16) for i in range(2)]
                kTs = [sbt(mx, "kT%d" % i, [128, 1024], BF16) for i in range(2)]
                qxTs = [sbt(mx, "qxT%d" % i, [128, 1024], BF16) for i in range(2)]
                b_qs_ = [[Buf("q%d_%d" % (j, i)) for i in range(2)] for j in range(2)]
                b_ks_ = [[Buf("k%d_%d" % (j, i)) for i in range(2)] for j in range(2)]
                b_qxs_ = [[Buf("qx%d_%d" % (j, i)) for i in range(2)] for j in range(2)]
                vext = [sbt(mx, "vext%d" % i, [128, 260], BF16) for i in range(2)]; b_v = [Buf("v%d" % i) for i in range(2)]
                gsig = [sbt(mx, "gsig%d" % i, [128, 256], BF16) for i in range(3)]; b_gs = [Buf("gs%d" % i) for i in range(3)]
                tho = [sbt(mx, "tho%d" % i, [128, 256]) for i in range(2)]; b_tho = [Buf("tho%d" % i) for i in range(2)]
                PT = [sbt(mx, "PT%d" % i, [128, 128], BF16) for i in range(2)]; b_PT = [Buf("PT%d" % i) for i in range(2)]
                kw = [sbt(mx, "kw%d" % i, [128, 128], BF16) for i in range(2)]; b_kw = [Buf("kw%d" % i) for i in range(2)]
                ymc = [sbt(mx, "ymc%d" % i, [128, 256], BF16) for i in range(2)]; b_ymc = [Buf("ymc%d" % i) for i in range(2)]
                tinys = [sbt(mx, "tiny%d" % i, [128, 16]) for i in range(2)]; b_tinys = [Buf("tiny%d" % i) for i in range(2)]
                halo2 = sbt(mx, "halo2", [128, 2, 4]); b_halo2 = [Buf("halo2_%d" % i) for i in range(2)]
                pdb = [sbt(mx, "pdb%d" % i, [128, 516], BF16) for i in range(2)]; b_pdb = [Buf("pdb%d" % i) for i in range(2)]
                dg = sbt(mx, "dg", [128, 2, 4, 128], BF16); b_dg = [Buf("dg%d" % i) for i in range(2)]
                gpre = sbt(mx, "gpre", [128, 8, 8]); lfp = sbt(mx, "lfp", [128, 8, 4]); a_t = sbt(mx, "a_t", [128, 8, 4])
                w_t = sbt(mx, "w_t", [128, 8, 4]); el_t = sbt(mx, "el_t", [128, 8, 4]); dec_t = sbt(mx, "dec_t", [128, 8, 4])
                wdec_t = sbt(mx, "wdec_t", [128, 8, 4]); b_g = Buf("gates")
                wb = wq; b_wb = b_wq
                wg = wk; b_wg = b_wk
                for i in range(2):
                    S.op("pool", lambda h, i=i: h.memset(vext[i][:, 256:260], 1.0), writes=[b_v[i]])
                wcnt = [0]

                def load_w(dst, col0, ncols, buf, l=l):
                    S.dma("pool", lambda h: h.dma_start(out=dst, in_=W_IN[l, :, col0:col0 + ncols].rearrange("(k p) n -> p k n", p=128)),
                          writes=[buf])

                for hf in range(2):
                    T0 = hf * 1024
                    for t01 in range(2):
                        wi01 = (wcnt[0] + t01) % 2
                        load_w(wq[wi01][:], OFF_MQ + t01 * 128, 128, b_wq[wi01])
                        load_w(wk[wi01][:], OFF_MK + t01 * 128, 128, b_wk[wi01])
                    for tg in range(2):
                        g = hf * 2 + tg
                        cols = slice(g * 512, (g + 1) * 512)
                        lc = slice(tg * 512, (tg + 1) * 512)
                        for k in range(8):
                            S.op("act", lambda h, k=k, cols=cols: h.activation(out=sq[k % 2][:], in_=xT[:, k, cols], func=AF.Square),
                                 reads=[b_xT[g]], writes=[b_sq[k % 2]])
                            S.op("pe", lambda h, k=k: h.matmul(bank[0][:], lhsT=ones32[:], rhs=sq[k % 2][:], start=(k == 0), stop=(k == 7)),
                                 reads=[b_sq[k % 2], b_const], writes=[pb[0]])
                        S.op("act", lambda h: h.activation(out=ssb, in_=bank[0][:], func=AF.Ln, bias=1024.0 * EPS),
                             writes=[pb[0], b_ss])
                        S.op("act", lambda h: h.activation(out=rstd, in_=ssb, func=AF.Exp, scale=-0.5),
                             reads=[b_ss], writes=[b_rstd])
                        for k in range(8):
                            S.op("dve", lambda h, k=k, cols=cols: h.scalar_tensor_tensor(
                                out=sq[k % 2][:], in0=xT[:, k, cols], scalar=scale1[:, l, k:k + 1], in1=rstd, op0=ALU.mult, op1=ALU.mult),
                                reads=[b_xT[g], b_rstd, b_mod], writes=[b_sq[k % 2]])
                            S.op("act", lambda h, k=k, lc=lc: h.activation(out=hT[:, k, lc], in_=sq[k % 2][:], func=AF.Identity,
                                                                           bias=modT[:, l, k:k + 1]),
                                 reads=[b_sq[k % 2], b_mod], writes=[b_hT[tg]])
                    if l == 0 and hf == 0:
                        dump("hT", hT[:], b_hT[1], [128, 8, 1024], BF16)
                    load_w(wif[:], OFF_MI, 8, b_wif)
                    psG = bank[6][:, 0:64].rearrange("p (c g) -> p c g", c=8)
                    psNB = bank[6][:, 64:96].rearrange("p (c g) -> p c g", c=8)
                    psNT = bank[6][:, 96:128].rearrange("p (c g) -> p c g", c=8)

                    def gmm(h):
                        ins = None
                        for c in range(8):
                            for k in range(8):
                                ins = h.matmul(psG[:, c, :], lhsT=hT[:, k, c * 128:(c + 1) * 128], rhs=wif[:, k, :], start=(k == 0), stop=(k == 7))
                        return ins
                    S.op("pe", gmm, reads=[b_hT[0], b_hT[1], b_wif], writes=[pb[6]])
                    S.op("dve", lambda h: h.tensor_tensor(out=gpre[:], in0=psG, in1=gbias[:, l, :, :], op=ALU.add),
                         reads=[cb["gbias"]], writes=[pb[6], b_g])
                    S.op("act", lambda h: h.activation(out=lfp[:], in_=gpre[:, :, 4:8], func=AF.Exp, scale=-1.0), writes=[b_g])
                    S.op("act", lambda h: h.activation(out=lfp[:], in_=lfp[:], func=AF.Ln, bias=1.0), writes=[b_g])

                    def nbmm(h):
                        ins = None
                        for c in range(8):
                            h.matmul(psNB[:, c, :], lhsT=tri[:], rhs=lfp[:, c, :], start=True, stop=True)
                            ins = h.matmul(psNT[:, c, :], lhsT=ones32[:], rhs=lfp[:, c, :], start=True, stop=True)
                        return ins
                    S.op("pe", nbmm, reads=[b_g, cb["tri"], b_const], writes=[pb[6]])
                    S.op("dve", lambda h: h.tensor_tensor(out=a_t[:], in0=psNB, in1=gpre[:, :, 0:4], op=ALU.add), writes=[pb[6], b_g])
                    S.op("act", lambda h: h.activation(out=w_t[:], in_=a_t[:], func=AF.Exp), writes=[b_g])
                    S.op("act", lambda h: h.activation(out=el_t[:], in_=psNB, func=AF.Exp, scale=-1.0), writes=[pb[6], b_g])
                    S.op("act", lambda h: h.activation(out=dec_t[:], in_=psNT, func=AF.Exp, scale=-1.0), writes=[pb[6], b_g])
                    S.op("dve", lambda h: h.tensor_tensor(out=wdec_t[:], in0=w_t[:], in1=dec_t[:], op=ALU.mult), writes=[b_g])
                    if l == 0 and hf == 0:
                        dump("w_t", w_t[:], b_g, [128, 8, 4]); dump("el_t", el_t[:], b_g, [128, 8, 4]); dump("dec_t", dec_t[:], b_g, [128, 8, 4])

                    OFFS = {True: (OFF_MQ, OFF_MK, OFF_MV, OFF_MO), False: (OFF_RQ, OFF_RK, OFF_RV, OFF_RG)}
                    tasks = []
                    for is_m_ in (True, False):
                        for hd_ in range(4):
                            tasks.append((is_m_, hd_, wcnt[0] % 2))
                            wcnt[0] += 1

                    def loads_qk(task):
                        is_m, hd, wi = task
                        oq, ok, ov, oo = OFFS[is_m]
                        load_w(wq[wi][:], oq + hd * 128, 128, b_wq[wi])
                        load_w(wk[wi][:], ok + hd * 128, 128, b_wk[wi])

                    def loads_vo(task):
                        is_m, hd, wi = task
                        oq, ok, ov, oo = OFFS[is_m]
                        load_w(wvo[wi][:, :, 0:256], ov + hd * 256, 256, b_wvo[wi])
                        load_w(wvo[wi][:, :, 256:512], oo + hd * 256, 256, b_wvo[wi])

                    def prologue_piece(task, which, tg, part):
                        is_m, hd, wi = task
                        qT = qTs[wi]; kT = kTs[wi]; qxT = qxTs[wi]
                        b_q = b_qs_[wi]; b_k = b_ks_[wi]; b_qx = b_qxs_[wi]
                        wsel = (wq, b_wq) if which == 0 else (wk, b_wk)
                        dstT, b_dst = (qT, b_q) if which == 0 else (kT, b_k)
                        g = hf * 2 + tg
                        lc = slice(tg * 512, (tg + 1) * 512)
                        gc = slice(g * 512, (g + 1) * 512)
                        pbi = 6

                        def pmm(h):
                            ins = None
                            for k in range(8):
                                ins = h.matmul(bank[pbi][:], lhsT=wsel[0][wi][:, k, :], rhs=hT[:, k, lc], start=(k == 0), stop=(k == 7))
                            return ins
                        if part == 0:
                            S.op("pe", pmm, reads=[wsel[1][wi], b_hT[tg]], writes=[pb[pbi]])
                        if is_m and part == 0:
                            hidx = hd * 2 + which
                            pd = pdb[tg]
                            kb = which * 4 + hd
                            if g == 0:
                                S.op("dve", lambda h: h.memset(pd[:, 0:3], 0.0), writes=[b_pdb[tg]])
                            elif tg == 0:
                                S.op("dve", lambda h: h.tensor_copy(out=pd[:, 0:3], in_=halo[:, hidx, 0:3]),
                                     reads=[b_halo[hidx]], writes=[b_pdb[tg]])
                            else:
                                S.op("dve", lambda h: h.tensor_copy(out=pd[:, 0:3], in_=halo2[:, which, 0:3]),
                                     reads=[b_halo2[which]], writes=[b_pdb[tg]])
                            if tg == 0:
                                for j in range(4):
                                    S.op("dve", lambda h, j=j: h.tensor_scalar(out=dg[:, which, j, :], in0=identb[:], scalar1=convw[:, l, j, kb:kb + 1],
                                                                             scalar2=None, op0=ALU.mult),
                                         reads=[b_const, cb["convw"]], writes=[b_dg[which]])
                            S.op("act", lambda h: h.activation(out=pd[:, 3:515], in_=bank[pbi][:], func=AF.Copy),
                                 writes=[pb[pbi], b_pdb[tg]])
                            if tg == 1:
                                S.op("dve", lambda h: h.tensor_copy(out=halo[:, hidx, 0:3], in_=pd[:, 512:515]),
                                     reads=[b_pdb[tg]], writes=[b_halo[hidx]])
                            else:
                                S.op("dve", lambda h: h.tensor_copy(out=halo2[:, which, 0:3], in_=pd[:, 512:515]),
                                     reads=[b_pdb[tg]], writes=[b_halo2[which]])
                        if is_m and part == 1:
                            pd = pdb[tg]
                            kb = which * 4 + hd

                            def cmm(h):
                                ins = None
                                for j in range(4):
                                    ins = h.matmul(bank[pbi][:], lhsT=dg[:, which, j, :], rhs=pd[:, j:j + 512], start=(j == 0), stop=(j == 3))
                                return ins
                            S.op("pe", cmm, reads=[b_dg[which], b_pdb[tg]], writes=[pb[pbi]])
                            S.op("act", lambda h: h.activation(out=acc[:], in_=bank[pbi][:], func=AF.Identity, bias=convb[:, l, kb:kb + 1]),
                                 reads=[cb["convb"]], writes=[pb[pbi], b_acc])
                            S.op("act", lambda h: h.activation(out=th[:], in_=acc[:], func=AF.Tanh, scale=0.5), reads=[b_acc], writes=[b_th])
                            S.op("dve", lambda h: h.scalar_tensor_tensor(
                                out=dstT[:, lc], in0=th[:], scalar=1.0, in1=acc[:], op0=ALU.add, op1=ALU.mult),
                                reads=[b_th, b_acc], writes=[b_dst[tg]])
                        if (not is_m) and part == 0:
                            S.op("act", lambda h: h.activation(out=th[0:64, :], in_=bank[pbi][64:128, :], func=AF.Copy),
                                 writes=[pb[pbi], b_th])
                            S.op("act", lambda h: h.activation(out=th[64:128, :], in_=bank[pbi][0:64, :], func=AF.Copy),
                                 writes=[pb[pbi], b_th])
                            S.op("dve", lambda h: h.tensor_tensor(out=acc[:], in0=bank[pbi][:], in1=cosT[:, gc], op=ALU.mult),
                                 reads=[b_cs], writes=[pb[pbi], b_acc])
                        if (not is_m) and part == 1:
                            S.op("dve", lambda h: h.tensor_tensor(out=th[:], in0=th[:], in1=sinS[:, gc], op=ALU.mult),
                                 reads=[b_cs], writes=[b_th])
                            if which == 0:
                                S.op("dve", lambda h: h.tensor_tensor(out=acc[:], in0=acc[:], in1=th[:], op=ALU.add),
                                     reads=[b_th], writes=[b_acc])
                                S.op("act", lambda h: h.activation(out=qT[:, lc], in_=acc[:], func=AF.Copy),
                                     reads=[b_acc], writes=[b_q[tg]])
                                S.op("dve", lambda h: h.tensor_tensor(
                                    out=qxT[:, lc].rearrange("p (c l) -> p c l", c=4), in0=acc[:].rearrange("p (c l) -> p c l", c=4),
                                    in1=xi[:, hd:hd + 1, :].to_broadcast([128, 4, 128]), op=ALU.mult),
                                    reads=[b_acc, cb["xi"]], writes=[b_qx[tg]])
                            else:
                                S.op("dve", lambda h: h.tensor_tensor(out=kT[:, lc], in0=acc[:], in1=th[:], op=ALU.add),
                                     reads=[b_th, b_acc], writes=[b_k[tg]])

                    PIECES = [(0, 0), (1, 0), (0, 1), (1, 1)]

                    def make_stages(task):
                        is_m, hd, wi = task
                        qT = qTs[wi]; kT = kTs[wi]; qxT = qxTs[wi]
                        b_q = b_qs_[wi]; b_k = b_ks_[wi]; b_qx = b_qxs_[wi]
                        stt = (C32[hd], Cb[hd], b_C[hd], b_Cb[hd]) if is_m else (R32[hd], Rb[hd], b_R[hd], b_Rb[hd])
                        NW = 257 if is_m else 256
                        qsrc = qT if is_m else qxT
                        b_qs = b_q if is_m else b_qx
                        kap = KAPPA_M if is_m else KAPPA_R
                        VB = (7, 1); SB = (4, 4); OB = (2, 0)

                        def stage_A(c, idx):
                            tg = c // 4
                            cc = slice(c * 128, (c + 1) * 128)
                            vi = idx % 2
                            vb = VB[vi]; sbk = SB[vi]

                            def vmm(h):
                                ins = None
                                for k in range(8):
                                    ins = h.matmul(bank[vb][:], lhsT=hT[:, k, cc], rhs=wvo[wi][:, k, :], start=(k == 0), stop=(k == 7))
                                return ins
                            S.op("pe", vmm, reads=[b_hT[tg], b_wvo[wi]], writes=[pb[vb]])
                            S.op("act", lambda h: h.activation(out=vext[vi][:, 0:256], in_=bank[vb][:, 0:256], func=AF.Copy),
                                 writes=[pb[vb], b_v[vi]])
                            S.op("act", lambda h: h.activation(out=tho[vi][:], in_=bank[vb][:, 256:512], func=AF.Tanh, scale=0.5),
                                 writes=[pb[vb], b_tho[vi]])
                            if is_m:
                                S.op("dve", lambda h: h.tensor_scalar(out=gsig[idx % 3][:], in0=tho[vi][:], scalar1=1.0, scalar2=None, op0=ALU.add),
                                     reads=[b_tho[vi]], writes=[b_gs[idx % 3]])
                            else:
                                S.op("dve", lambda h: h.scalar_tensor_tensor(
                                    out=gsig[idx % 3][:], in0=tho[vi][:], scalar=1.0, in1=bank[vb][:, 256:512], op0=ALU.add, op1=ALU.mult),
                                    reads=[b_tho[vi]], writes=[pb[vb], b_gs[idx % 3]])
                            S.op("pe", lambda h: h.matmul(bank[sbk][:, 0:128], lhsT=kT[:, cc], rhs=qT[:, cc], start=True, stop=True),
                                 reads=[b_k[tg], b_q[tg]], writes=[pb[sbk]])
                            if is_m:
                                S.op("dve", lambda h: h.scalar_tensor_tensor(
                                    out=PT[vi][:], in0=bank[sbk][:, 0:128], scalar=w_t[:, c, hd:hd + 1], in1=maskm[:], op0=ALU.mult, op1=ALU.mult),
                                    reads=[b_g, cb["maskm"]], writes=[pb[sbk], b_PT[vi]])
                            else:
                                S.op("dve", lambda h: h.tensor_tensor(out=PT[vi][:], in0=bank[sbk][:, 0:128], in1=retd[:, hd, :], op=ALU.mult),
                                     reads=[cb["retd"]], writes=[pb[sbk], b_PT[vi]])
                            psT = bank[5][:, 0:64].bitcast(BF16)
                            S.op("pe", lambda h: h.transpose(psT, kT[:, cc], identb[:]), reads=[b_k[tg], b_const], writes=[pb[5]])
                            if is_m:
                                S.op("act", lambda h: h.activation(out=kw[vi][:], in_=psT, func=AF.Copy, scale=wdec_t[:, c, hd:hd + 1]),
                                     reads=[b_g], writes=[pb[5], b_kw[vi]])
                            else:
                                S.op("act", lambda h: h.activation(out=kw[vi][:], in_=psT, func=AF.Copy, scale=zeta[:, hd:hd + 1]),
                                     reads=[cb["zeta"]], writes=[pb[5], b_kw[vi]])

                        def stage_B(c, idx):
                            gci = hf * 8 + c
                            tg = c // 4
                            cc = slice(c * 128, (c + 1) * 128)
                            vi = idx % 2
                            ob_ = OB[vi]

                            def omm(h):
                                ins = h.matmul(bank[ob_][:, 0:NW], lhsT=PT[vi][:], rhs=vext[vi][:, 0:NW], start=True, stop=(gci == 0))
                                if gci > 0:
                                    ins = h.matmul(bank[ob_][:, 0:NW], lhsT=qsrc[:, cc], rhs=stt[1][:, 0:NW], start=False, stop=True)
                                return ins
                            S.op("pe", omm, reads=[b_PT[vi], b_v[vi], b_qs[tg], stt[3]], writes=[pb[ob_]])
                            S.op("pe", lambda h: h.matmul(bank[3][:, 0:NW], lhsT=kw[vi][:], rhs=vext[vi][:, 0:NW], start=True, stop=True),
                                 reads=[b_kw[vi], b_v[vi]], writes=[pb[3]])
                            if gci == 0:
                                S.op("dve", lambda h: h.tensor_copy(out=stt[0][:, 0:NW], in_=bank[3][:, 0:NW]),
                                     writes=[pb[3], stt[2]])
                            elif is_m:
                                S.op("dve", lambda h: h.scalar_tensor_tensor(
                                    out=stt[0][:, 0:NW], in0=stt[0][:, 0:NW], scalar=dec_t[:, c, hd:hd + 1], in1=bank[3][:, 0:NW],
                                    op0=ALU.mult, op1=ALU.add), reads=[b_g], writes=[pb[3], stt[2]])
                            else:
                                gam = (1.0 - 2.0 ** (-5.0 - hd)) ** 128
                                S.op("dve", lambda h: h.scalar_tensor_tensor(
                                    out=stt[0][:, 0:NW], in0=stt[0][:, 0:NW], scalar=float(gam), in1=bank[3][:, 0:NW],
                                    op0=ALU.mult, op1=ALU.add), writes=[pb[3], stt[2]])
                            S.op("act", lambda h: h.activation(out=stt[1][:, 0:NW], in_=stt[0][:, 0:NW], func=AF.Copy, scale=float(kap)),
                                 reads=[stt[2]], writes=[stt[3]])
                            tny = tinys[idx % 2]; b_tny = b_tinys[idx % 2]
                            if is_m:
                                S.op("dve", lambda h: h.tensor_scalar(out=tny[:, 0:1], in0=bank[ob_][:, 256:257], scalar1=el_t[:, c, hd:hd + 1],
                                                                      scalar2=None, op0=ALU.mult), reads=[b_g], writes=[pb[ob_], b_tny])
                                S.op("dve", lambda h: h.scalar_tensor_tensor(out=tny[:, 1:2], in0=tny[:, 0:1], scalar=-1.0, in1=tny[:, 0:1],
                                                                             op0=ALU.mult, op1=ALU.max), writes=[b_tny])
                                S.op("dve", lambda h: h.tensor_scalar(out=tny[:, 2:3], in0=tny[:, 1:2], scalar1=1.0, scalar2=None, op0=ALU.max),
                                     writes=[b_tny])
                                S.op("dve", lambda h: h.reciprocal(out=tny[:, 3:4], in_=tny[:, 2:3]), writes=[b_tny])
                                S.op("dve", lambda h: h.tensor_scalar(out=tny[:, 4:5], in0=tny[:, 3:4], scalar1=el_t[:, c, hd:hd + 1],
                                                                      scalar2=None, op0=ALU.mult), reads=[b_g], writes=[b_tny])
                                S.op("act", lambda h: h.activation(out=tho[vi][:], in_=bank[ob_][:, 0:256], func=AF.Square, scale=tny[:, 4:5],
                                                                   accum_out=tny[:, 5:6]), reads=[b_tny], writes=[pb[ob_], b_tho[vi], b_tny])
                            else:
                                S.op("act", lambda h: h.activation(out=tho[vi][:], in_=bank[ob_][:, 0:256], func=AF.Square,
                                                                   accum_out=tny[:, 5:6]), writes=[pb[ob_], b_tho[vi], b_tny])

                        def stage_C1b(c, idx):
                            vi = idx % 2
                            ob_ = OB[vi]
                            tny = tinys[idx % 2]; b_tny = b_tinys[idx % 2]
                            S.op("dve", lambda h: h.tensor_scalar(out=tny[:, 6:7], in0=tny[:, 5:6], scalar1=4.0 / 256.0, scalar2=4.0 * EPS,
                                                                  op0=ALU.mult, op1=ALU.add), writes=[b_tny])
                            S.op("pool", lambda h: h.tensor_tensor(out=tny[:, 7:8], in0=tny[:, 6:7], in1=mhalf[:, 0:1], op=ALU.pow),
                                 reads=[b_const], writes=[b_tny])
                            if is_m:
                                S.op("dve", lambda h: h.tensor_tensor(out=tny[:, 8:9], in0=tny[:, 7:8], in1=tny[:, 4:5], op=ALU.mult), writes=[b_tny])
                                sc_ap = tny[:, 8:9]
                            else:
                                sc_ap = tny[:, 7:8]
                            S.op("dve", lambda h: h.scalar_tensor_tensor(
                                out=ymc[vi][:], in0=bank[ob_][:, 0:256], scalar=sc_ap, in1=gsig[idx % 3][:], op0=ALU.mult, op1=ALU.mult),
                                reads=[b_tny, b_gs[idx % 3]], writes=[pb[ob_], b_ymc[vi]])


                        def stage_C2(c, idx):
                            cc = slice(c * 128, (c + 1) * 128)
                            vi = idx % 2
                            psY = bank[5][:, 64:192].bitcast(BF16).rearrange("p (a b) -> p a b", a=2)

                            def ytr(h):
                                h.transpose(psY[:, 0, :], ymc[vi][:, 0:128], identb[:])
                                return h.transpose(psY[:, 1, :], ymc[vi][:, 128:256], identb[:])
                            S.op("pe", ytr, reads=[b_ymc[vi], b_const], writes=[pb[5]])
                            bri = 0 if is_m else 1
                            S.op("act", lambda h: h.activation(out=ymT[:, hd * 2, cc], in_=psY[:, 0, :], func=AF.Copy, scale=ngT[:, bri, l, hd * 2:hd * 2 + 1]),
                                 reads=[cb["ngT"]], writes=[pb[5], b_ymT[c]])
                            S.op("act", lambda h: h.activation(out=ymT[:, hd * 2 + 1, cc], in_=psY[:, 1, :], func=AF.Copy, scale=ngT[:, bri, l, hd * 2 + 1:hd * 2 + 2]),
                                 reads=[cb["ngT"]], writes=[pb[5], b_ymT[c]])

                        return {"A": stage_A, "B": stage_B, "C1b": stage_C1b, "C2": stage_C2}

                    def branch_proj(is_m):
                        if l == 0 and hf == 0:
                            dump("ymT" if is_m else "yrT", ymT[:], b_ymT[7], [128, 8, 1024], BF16)
                        wsrc = W_BM if is_m else W_BR
                        og = OFF_GA if is_m else OFF_GB
                        for j in range(8):
                            ji = j % 2
                            S.dma("pool", lambda h, j=j, ji=ji, wsrc=wsrc: h.dma_start(
                                out=wb[ji][:], in_=wsrc[l, :, j * 128:(j + 1) * 128].rearrange("(k p) n -> p k n", p=128)), writes=[b_wb[ji]])
                            load_w(wg[ji][:], og + j * 128, 128, b_wg[ji])
                            for tg in range(2):
                                lc = slice(tg * 512, (tg + 1) * 512)
                                nn = (j * 2 + tg) % 2
                                bA = 0 + 2 * nn
                                bB = 1 + 2 * nn
                                tb_ = pad[nn][:, 0:512]
                                b_tb = b_pad[nn]
                                tmp_ = acc if nn == 0 else th
                                b_tmp = b_acc if nn == 0 else b_th

                                def bmm(h, ji=ji, lc=lc, bA=bA):
                                    ins = None
                                    for k in range(8):
                                        ins = h.matmul(bank[bA][:], lhsT=wb[ji][:, k, :], rhs=ymT[:, k, lc], start=(k == 0), stop=(k == 7))
                                    return ins
                                S.op("pe", bmm, reads=[b_wb[ji]] + b_ymT[tg * 4:(tg + 1) * 4], writes=[pb[bA]])

                                def gmm2(h, ji=ji, lc=lc, bB=bB):
                                    ins = None
                                    for k in range(8):
                                        ins = h.matmul(bank[bB][:], lhsT=wg[ji][:, k, :], rhs=hT[:, k, lc], start=(k == 0), stop=(k == 7))
                                    return ins
                                S.op("pe", gmm2, reads=[b_wg[ji], b_hT[tg]], writes=[pb[bB]])
                                S.op("act", lambda h: h.activation(out=tb_, in_=bank[bB][:], func=AF.Tanh, scale=0.5), writes=[pb[bB], b_tb])
                                if is_m:
                                    S.op("dve", lambda h: h.scalar_tensor_tensor(
                                        out=yT[:, j, lc], in0=tb_, scalar=1.0, in1=bank[bA][:], op0=ALU.add, op1=ALU.mult),
                                        reads=[b_tb], writes=[pb[bA], b_yT[tg]])
                                else:
                                    S.op("dve", lambda h: h.scalar_tensor_tensor(
                                        out=tmp_[:], in0=tb_, scalar=1.0, in1=bank[bA][:], op0=ALU.add, op1=ALU.mult),
                                        reads=[b_tb], writes=[pb[bA], b_tmp])
                                    S.op("dve", lambda h: h.tensor_tensor(out=yT[:, j, lc], in0=yT[:, j, lc], in1=tmp_[:], op=ALU.add),
                                         reads=[b_tmp], writes=[b_yT[tg]])
                    def run_stream(tis):
                        stg = {ti: make_stages(tasks[ti]) for ti in tis}
                        seq = [(ti, c) for ti in tis for c in range(8)]
                        n = len(seq)

                        def call(kind, i):
                            ti, c = seq[i]
                            stg[ti][kind](c, i)
                        call("A", 0)
                        for i in range(n):
                            ti, c = seq[i]
                            nxt = tasks[ti + 1] if ti + 1 < 8 else None
                            if c == 0:
                                if ti + 2 < 8 and ti + 2 != 5:
                                    loads_qk(tasks[ti + 2])
                                if ti == 4:
                                    loads_qk(tasks[5])
                                if nxt is not None:
                                    loads_vo(nxt)
                            if i + 1 < n:
                                call("A", i + 1)
                            call("B", i)
                            if i >= 1:
                                call("C1b", i - 1)
                            if i >= 2:
                                call("C2", i - 2)
                            if nxt is not None:
                                w_, t_ = PIECES[c // 2]
                                prologue_piece(nxt, w_, t_, c % 2)
                        call("C1b", n - 1)
                        call("C2", n - 2)
                        call("C2", n - 1)

                    loads_vo(tasks[0])
                    for w_, t_ in PIECES:
                        prologue_piece(tasks[0], w_, t_, 0)
                        prologue_piece(tasks[0], w_, t_, 1)
                    run_stream([0, 1, 2, 3])
                    branch_proj(True)
                    run_stream([4, 5, 6, 7])
                    branch_proj(False)
                    if l == 0 and hf == 0:
                        dump("yT", yT[:], b_yT[1], [128, 8, 1024], BF16)
                    for j in range(8):
                        ji = j % 2
                        S.dma("pool", lambda h, j=j, ji=ji: h.dma_start(
                            out=wb[ji][:], in_=W_OUT[l, :, j * 128:(j + 1) * 128].rearrange("(k p) n -> p k n", p=128)), writes=[b_wb[ji]])
                        for tg in range(2):
                            g = hf * 2 + tg
                            lc = slice(tg * 512, (tg + 1) * 512)
                            gc = slice(g * 512, (g + 1) * 512)

                            def omm2(h, ji=ji, lc=lc, tg=tg):
                                ins = None
                                for k in range(8):
                                    ins = h.matmul(bank[tg][:], lhsT=wb[ji][:, k, :], rhs=yT[:, k, lc], start=(k == 0), stop=(k == 7))
                                return ins
                            S.op("pe", omm2, reads=[b_wb[ji], b_yT[tg]], writes=[pb[tg]])
                            S.op("dve", lambda h, j=j, gc=gc, tg=tg: h.scalar_tensor_tensor(
                                out=xT[:, j, gc], in0=bank[tg][:], scalar=g1h[:, l, j:j + 1], in1=xT[:, j, gc], op0=ALU.mult, op1=ALU.add),
                                reads=[b_mod], writes=[pb[tg], b_xT[g]])
                S.barrier()
            if stop_after == "mix%d" % l:
                return finish(nc, S, st, final_evs, xT, b_xT, bank, pb, ident, FING, cb, mhalf, OUT, sbt)

            with ExitStack() as mo:
                h2T = sbt(mo, "h2T", [128, 8, S_LEN], BF16); b_h2 = [Buf("h2_%d" % i) for i in range(4)]
                gT = sbt(mo, "gT", [16, S_LEN]); b_gT = [Buf("gT%d" % i) for i in range(4)]
                mo1 = ExitStack(); mo1.__enter__()
                rts = [sbt(mo1, "rt%d" % i, [128, 4, 64]) for i in range(2)]; b_rts = [Buf("rt%d" % i) for i in range(2)]
                gfull = sbt(mo1, "gfull", [128, 4, 16]); b_gf = Buf("gfull")
                wr = sbt(mo1, "wr_s", [128, 8, 20]); brs = sbt(mo1, "br_s", [128, 4, 20])
                cb["wr"] = Buf("c_wr"); cb["br"] = Buf("c_br")
                S.dma("sp", lambda h: h.dma_start(out=wr[:], in_=WR[:, l * 160:(l + 1) * 160].rearrange("p (k n) -> p k n", k=8)), writes=[cb["wr"]])
                S.dma("sp", lambda h: h.dma_start(out=brs[:], in_=BR[:, l * 80:(l + 1) * 80].rearrange("p (t n) -> p t n", t=4)), writes=[cb["br"]])
                h32 = sbt(mo1, "h32", [128, 8, 512]); b_h32 = Buf("h32")
                sq = [sbt(mo1, "msq%d" % i, [128, 512]) for i in range(2)]; b_sq = [Buf("msq%d" % i) for i in range(2)]
                ssb = sbt(mo1, "mssb", [128, 512]); rstd = sbt(mo1, "mrstd", [128, 512]); b_ss = Buf("mss"); b_rstd = Buf("mrstd")
                psL = bank[6][:, 0:80].rearrange("p (t n) -> p t n", t=4)

                def norm_part(g):
                    cols = slice(g * 512, (g + 1) * 512)
                    for k in range(8):
                        S.op("act", lambda h, k=k: h.activation(out=sq[k % 2][:], in_=xT[:, k, cols], func=AF.Square),
                             reads=[b_xT[g]], writes=[b_sq[k % 2]])
                        S.op("pe", lambda h, k=k: h.matmul(bank[0][:], lhsT=ones32[:], rhs=sq[k % 2][:], start=(k == 0), stop=(k == 7)),
                             reads=[b_sq[k % 2], b_const], writes=[pb[0]])
                    S.op("act", lambda h: h.activation(out=ssb[:], in_=bank[0][:], func=AF.Ln, bias=1024.0 * EPS),
                         writes=[pb[0], b_ss])
                    S.op("act", lambda h: h.activation(out=rstd[:], in_=ssb[:], func=AF.Exp, scale=-0.5), reads=[b_ss], writes=[b_rstd])
                    for k in range(8):
                        S.op("dve", lambda h, k=k: h.scalar_tensor_tensor(
                            out=sq[k % 2][:], in0=xT[:, k, cols], scalar=scale2[:, l, k:k + 1], in1=rstd[:], op0=ALU.mult, op1=ALU.mult),
                            reads=[b_xT[g], b_rstd, b_mod], writes=[b_sq[k % 2]])
                        S.op("act", lambda h, k=k: h.activation(out=h32[:, k, :], in_=sq[k % 2][:], func=AF.Identity, bias=modT[:, l, 24 + k:25 + k]),
                             reads=[b_sq[k % 2], b_mod], writes=[b_h32])
                        S.op("act", lambda h, k=k: h.activation(out=h2T[:, k, cols], in_=sq[k % 2][:], func=AF.Identity, bias=modT[:, l, 24 + k:25 + k]),
                             reads=[b_sq[k % 2], b_mod], writes=[b_h2[g]])

                def router_mm(g):
                    rt = rts[g % 2]; b_rt = b_rts[g % 2]
                    def lmm(h):
                        ins = None
                        for tt in range(4):
                            for k in range(8):
                                ins = h.matmul(psL[:, tt, :], lhsT=h32[:, k, tt * 128:(tt + 1) * 128], rhs=wr[:, k, :], start=(k == 0), stop=(k == 7))
                        return ins
                    S.op("pe", lmm, reads=[b_h32, cb["wr"]], writes=[pb[6]])
                    S.op("dve", lambda h: h.tensor_tensor(out=rt[:, :, 0:20], in0=psL, in1=brs[:, :, :], op=ALU.add),
                         reads=[cb["br"]], writes=[pb[6], b_rt])

                def bc(ap):
                    return ap.to_broadcast([128, 4, 4])

                def routing(g):
                    rt = rts[g % 2]; b_rt = b_rts[g % 2]
                    cols = slice(g * 512, (g + 1) * 512)
                    D_ = lambda fn: S.op("dve", fn, writes=[b_rt])
                    D_(lambda h: h.tensor_reduce(out=rt[:, :, 20:21], in_=rt[:, :, 0:4], axis=AX.X, op=ALU.max))
                    D_(lambda h: h.tensor_tensor(out=rt[:, :, 24:28], in0=rt[:, :, 0:4], in1=bc(rt[:, :, 20:21]), op=ALU.is_ge))
                    D_(lambda h: h.tensor_tensor(out=rt[:, :, 28:32], in0=rt[:, :, 0:4], in1=bc(rt[:, :, 20:21]), op=ALU.subtract))
                    S.op("act", lambda h: h.activation(out=rt[:, :, 28:32], in_=rt[:, :, 28:32], func=AF.Exp), writes=[b_rt])
                    D_(lambda h: h.tensor_reduce(out=rt[:, :, 21:22], in_=rt[:, :, 28:32], axis=AX.X, op=ALU.add))
                    D_(lambda h: h.reciprocal(out=rt[:, :, 22:23], in_=rt[:, :, 21:22]))
                    D_(lambda h: h.tensor_tensor(out=rt[:, :, 32:36], in0=rt[:, :, 4:8], in1=bc(rt[:, :, 24:25]), op=ALU.mult))
                    for gg in range(1, 4):
                        D_(lambda h, gg=gg: h.tensor_tensor(out=rt[:, :, 56:60], in0=rt[:, :, 4 + 4 * gg:8 + 4 * gg], in1=bc(rt[:, :, 24 + gg:25 + gg]), op=ALU.mult))
                        D_(lambda h: h.tensor_tensor(out=rt[:, :, 32:36], in0=rt[:, :, 32:36], in1=rt[:, :, 56:60], op=ALU.add))
                    D_(lambda h: h.tensor_reduce(out=rt[:, :, 36:37], in_=rt[:, :, 32:36], axis=AX.X, op=ALU.max))
                    D_(lambda h: h.tensor_tensor(out=rt[:, :, 40:44], in0=rt[:, :, 32:36], in1=bc(rt[:, :, 36:37]), op=ALU.is_ge))
                    D_(lambda h: h.scalar_tensor_tensor(out=rt[:, :, 44:48], in0=rt[:, :, 40:44], scalar=-1e30, in1=rt[:, :, 32:36],
                                                        op0=ALU.mult, op1=ALU.add))
                    D_(lambda h: h.tensor_reduce(out=rt[:, :, 37:38], in_=rt[:, :, 44:48], axis=AX.X, op=ALU.max))
                    D_(lambda h: h.tensor_tensor(out=rt[:, :, 48:52], in0=rt[:, :, 44:48], in1=bc(rt[:, :, 37:38]), op=ALU.is_ge))
                    D_(lambda h: h.tensor_tensor(out=rt[:, :, 38:39], in0=rt[:, :, 37:38], in1=rt[:, :, 36:37], op=ALU.subtract))
                    S.op("act", lambda h: h.activation(out=rt[:, :, 38:39], in_=rt[:, :, 38:39], func=AF.Exp), writes=[b_rt])
                    D_(lambda h: h.tensor_scalar(out=rt[:, :, 38:39], in0=rt[:, :, 38:39], scalar1=1.0, scalar2=None, op0=ALU.add))
                    D_(lambda h: h.reciprocal(out=rt[:, :, 39:40], in_=rt[:, :, 38:39]))
                    D_(lambda h: h.tensor_tensor(out=rt[:, :, 39:40], in0=rt[:, :, 39:40], in1=rt[:, :, 22:23], op=ALU.mult))
                    D_(lambda h: h.tensor_tensor(out=rt[:, :, 23:24], in0=rt[:, :, 22:23], in1=rt[:, :, 39:40], op=ALU.subtract))
                    D_(lambda h: h.tensor_tensor(out=rt[:, :, 52:56], in0=rt[:, :, 40:44], in1=bc(rt[:, :, 39:40]), op=ALU.mult))
                    D_(lambda h: h.tensor_tensor(out=rt[:, :, 56:60], in0=rt[:, :, 48:52], in1=bc(rt[:, :, 23:24]), op=ALU.mult))
                    D_(lambda h: h.tensor_tensor(out=rt[:, :, 52:56], in0=rt[:, :, 52:56], in1=rt[:, :, 56:60], op=ALU.add))
                    for gg in range(4):
                        S.op("dve", lambda h, gg=gg: h.tensor_tensor(out=gfull[:, :, gg * 4:(gg + 1) * 4], in0=rt[:, :, 52:56],
                                                                   in1=bc(rt[:, :, 24 + gg:25 + gg]), op=ALU.mult),
                             reads=[b_rt], writes=[b_gf])

                    def gtr(h):
                        ins = None
                        for tt in range(4):
                            ins = h.transpose(bank[5][0:16, tt * 128:(tt + 1) * 128], gfull[:, tt, :], ident[:])
                        return ins
                    S.op("pe", gtr, reads=[b_gf, cb["ident"]], writes=[pb[5]])
                    S.op("act", lambda h: h.activation(out=gT[:, cols], in_=bank[5][0:16, :], func=AF.Copy), writes=[pb[5], b_gT[g]])
                    if l == 0 and g == 0:
                        dump("gfull", gfull[:], b_gf, [128, 4, 16])

                norm_part(0)
                router_mm(0)
                for g in range(1, 4):
                    norm_part(g)
                    router_mm(g)
                    routing(g - 1)
                routing(3)
                if l == 0:
                    dump("h2T", h2T[:], b_h2[3], [128, 8, S_LEN], BF16)
                S.barrier()
                mo1.__exit__(None, None, None)
                wgt = [sbt(mo, "wgt%d" % i, [128, 8, 512], BF16) for i in range(2)]; b_wgt = [Buf("wgt%d" % i) for i in range(2)]
                wup = [sbt(mo, "wup%d" % i, [128, 8, 512], BF16) for i in range(2)]; b_wup = [Buf("wup%d" % i) for i in range(2)]
                wdn = [sbt(mo, "wdn%d" % i, [128, 4, D], BF16) for i in range(2)]; b_wdn = [Buf("wdn%d" % i) for i in range(2)]
                he = [sbt(mo, "he%d" % i, [128, 4, 512], BF16) for i in range(2)]; b_he = [Buf("he%d" % i) for i in range(2)]
                Gsb = sbt(mo, "Gsb", [128, 512]); b_G = Buf("Gsb")
                Ee = [sbt(mo, "Ee%d" % i, [16, 128]) for i in range(2)]; b_Ee = [Buf("Ee%d" % i) for i in range(2)]
                tht = sbt(mo, "tht", [128, 512]); usb = sbt(mo, "usb", [128, 512])
                b_tht = Buf("tht"); b_usb = Buf("usb")
                GB = (0, 7); UB = (1, 6); DB = (2, 3, 4)

                def stage_GUDN(e, g, prev):
                    ei = e % 2
                    hi = (e * 4 + g) % 2
                    cols = slice(g * 512, (g + 1) * 512)
                    S.op("pe", lambda h: h.matmul(bank[5][:], lhsT=Ee[ei][:], rhs=gT[:, cols], start=True, stop=True),
                         reads=[b_Ee[ei], b_gT[g]], writes=[pb[5]])
                    S.op("act", lambda h: h.activation(out=Gsb[:], in_=bank[5][:], func=AF.Copy, scale=0.5), writes=[pb[5], b_G])
                    for fb in range(4):
                        gb_ = GB[fb % 2]; ub_ = UB[fb % 2]

                        def gm(h):
                            ins = None
                            for k in range(8):
                                ins = h.matmul(bank[gb_][:], lhsT=wgt[ei][:, k, fb * 128:(fb + 1) * 128], rhs=h2T[:, k, cols], start=(k == 0), stop=(k == 7))
                            return ins
                        S.op("pe", gm, reads=[b_wgt[ei], b_h2[g]], writes=[pb[gb_]])

                        def um(h):
                            ins = None
                            for k in range(8):
                                ins = h.matmul(bank[ub_][:], lhsT=wup[ei][:, k, fb * 128:(fb + 1) * 128], rhs=h2T[:, k, cols], start=(k == 0), stop=(k == 7))
                            return ins
                        S.op("pe", um, reads=[b_wup[ei], b_h2[g]], writes=[pb[ub_]])
                        S.op("act", lambda h: h.activation(out=tht[:], in_=bank[gb_][:], func=AF.Tanh, scale=0.5), writes=[pb[gb_], b_tht])
                        S.op("act", lambda h: h.activation(out=usb[:], in_=bank[ub_][:], func=AF.Copy), writes=[pb[ub_], b_usb])
                        S.op("dve", lambda h: h.tensor_tensor(out=usb[:], in0=bank[gb_][:], in1=usb[:], op=ALU.mult), writes=[pb[gb_], b_usb])
                        S.op("dve", lambda h: h.scalar_tensor_tensor(out=tht[:], in0=tht[:], scalar=1.0, in1=Gsb[:], op0=ALU.add, op1=ALU.mult),
                             reads=[b_G], writes=[b_tht])
                        S.op("pool", lambda h: h.tensor_tensor(out=he[hi][:, fb, :], in0=usb[:], in1=tht[:], op=ALU.mult),
                             reads=[b_usb, b_tht], writes=[b_he[hi]])
                        if prev is not None:
                            stage_DN(prev[0], prev[1], (2 * fb, 2 * fb + 1))

                def stage_DN(e, g, js=tuple(range(8))):
                    ei = e % 2
                    hi = (e * 4 + g) % 2
                    cols = slice(g * 512, (g + 1) * 512)
                    for j in js:
                        bi = DB[j % 3]

                        def dm(h):
                            ins = None
                            for fb in range(4):
                                ins = h.matmul(bank[bi][:], lhsT=wdn[ei][:, fb, j * 128:(j + 1) * 128], rhs=he[hi][:, fb, :], start=(fb == 0), stop=(fb == 3))
                            return ins
                        S.op("pe", dm, reads=[b_wdn[ei], b_he[hi]], writes=[pb[bi]])
                        S.op("dve", lambda h: h.scalar_tensor_tensor(
                            out=xT[:, j, cols], in0=bank[bi][:], scalar=modT[:, l, 40 + j:41 + j], in1=xT[:, j, cols], op0=ALU.mult, op1=ALU.add),
                            reads=[b_mod], writes=[pb[bi], b_xT[g]])

                def load_e(e):
                    ei = e % 2
                    S.op("pool", lambda h: h.tensor_scalar(out=Ee[ei][:], in0=ones32[0:16, :], scalar1=ident[0:16, e:e + 1], scalar2=None,
                                                           op0=ALU.mult), reads=[b_const, cb["ident"]], writes=[b_Ee[ei]])
                    S.dma("pool", lambda h: h.dma_start(out=wgt[ei][:], in_=W_GATE[l, e].rearrange("(k p) f -> p k f", p=128)), writes=[b_wgt[ei]])
                    S.dma("pool", lambda h: h.dma_start(out=wup[ei][:], in_=W_UP[l, e].rearrange("(k p) f -> p k f", p=128)), writes=[b_wup[ei]])
                    S.dma("pool", lambda h: h.dma_start(out=wdn[ei][:], in_=W_DOWN[l, e].rearrange("(k p) n -> p k n", p=128)), writes=[b_wdn[ei]])

                seq = [(e, g) for e in range(16) for g in range(4)]
                load_e(0)
                load_e(1)
                stage_GUDN(0, 0, None)
                for i in range(1, 64):
                    e, g = seq[i]
                    pe_, pg_ = seq[i - 1]
                    stage_GUDN(e, g, (pe_, pg_))
                    if pg_ == 3 and pe_ + 2 < 16:
                        load_e(pe_ + 2)
                stage_DN(15, 3)
                S.barrier()
        return finish(nc, S, st, final_evs, xT, b_xT, bank, pb, ident, FING, cb, mhalf, OUT, sbt)


def finish(nc, S, st, final_evs, xT, b_xT, bank, pb, ident, FING, cb, mhalf, OUT, sbt):
    with ExitStack() as fs:
        fing = sbt(fs, "fing_s", [128, D]); cb["fing"] = Buf("c_fing")
        S.dma("sp", lambda h: h.dma_start(out=fing[:], in_=FING), writes=[cb["fing"]])
        ob = [sbt(fs, "ob%d" % i, [128, D]) for i in range(2)]
        b_ob = [Buf("ob%d" % i) for i in range(2)]
        fj = sbt(fs, "fjunk", [128, 512]); ft = sbt(fs, "ftiny", [128, 8]); b_fj = Buf("fj"); b_ft = Buf("ft")
        b_out = Buf("outd")
        b_mh = Buf("mh2")
        fts = [ft, sbt(fs, "ftiny2", [128, 8])]; b_fts = [b_ft, Buf("ft2")]
        fjs = [fj, sbt(fs, "fjunk2", [128, 512])]; b_fjs = [b_fj, Buf("fj2")]
        for t in range(16):
            tcs = slice(t * 128, (t + 1) * 128)
            p_ = t % 2
            ft_ = fts[p_]; b_ft_ = b_fts[p_]
            for hb in range(2):
                bk = hb + 2 * p_

                def tr(h, hb=hb, tcs=tcs, bk=bk):
                    ins = None
                    for kk in range(4):
                        ins = h.transpose(bank[bk][:, kk * 128:(kk + 1) * 128], xT[:, hb * 4 + kk, tcs], ident[:])
                    return ins
                S.op("pe", tr, reads=[b_xT[t // 4], cb["ident"]], writes=[pb[bk]])
                S.op("act", lambda h, hb=hb, bk=bk: h.activation(out=fjs[hb][:], in_=bank[bk][:], func=AF.Square, accum_out=ft_[:, hb:hb + 1]),
                     writes=[pb[bk], b_fjs[hb], b_ft_])
            S.op("dve", lambda h: h.tensor_tensor(out=ft_[:, 2:3], in0=ft_[:, 0:1], in1=ft_[:, 1:2], op=ALU.add), writes=[b_ft_])
            S.op("dve", lambda h: h.tensor_scalar(out=ft_[:, 3:4], in0=ft_[:, 2:3], scalar1=1.0 / 1024.0, scalar2=EPS, op0=ALU.mult, op1=ALU.add), writes=[b_ft_])
            S.op("pool", lambda h: h.tensor_tensor(out=ft_[:, 4:5], in0=ft_[:, 3:4], in1=mhalf[:, 0:1], op=ALU.pow), writes=[b_ft_])
            for hb in range(2):
                bk = hb + 2 * p_
                S.op("dve", lambda h, hb=hb, t=t, bk=bk: h.scalar_tensor_tensor(
                    out=ob[t % 2][:, hb * 512:(hb + 1) * 512], in0=bank[bk][:], scalar=ft_[:, 4:5], in1=fing[:, hb * 512:(hb + 1) * 512],
                    op0=ALU.mult, op1=ALU.mult), reads=[b_ft_, cb["fing"]], writes=[pb[bk], b_ob[t % 2]])
            S.dma("sp", lambda h, t=t, tcs=tcs: h.dma_start(out=OUT[tcs, :], in_=ob[t % 2][:]), reads=[b_ob[t % 2]], writes=[b_out])
        S.barrier(engs=["sp"])
        S.emit()
    return nc


_CACHE = {}


def _consts():
    f = np.float32
    idx = np.arange(128)
    c = {}
    c["c_ident"] = np.eye(128, dtype=f)
    c["c_tri"] = (idx[:, None] <= idx[None, :]).astype(f)
    c["c_maskm"] = (c["c_tri"] * KAPPA_M).astype(f)
    retd = np.zeros((128, 4, 128), f)
    xi = np.zeros((128, 4, 128), f)
    zeta = np.zeros((128, 4), f)
    for h in range(4):
        lg = math.log(1.0 - 2.0 ** (-5.0 - h))
        rel = idx[None, :] - idx[:, None]
        retd[:, h, :] = np.where(rel >= 0, np.exp(lg * np.maximum(rel, 0)), 0.0) * KAPPA_R
        xi[:, h, :] = np.exp(lg * (idx + 1.0))[None, :]
        zeta[:, h] = np.exp(lg * (127.0 - idx))
    c["c_retd"] = retd.reshape(128, 512)
    c["c_xi"] = xi.reshape(128, 512)
    c["c_zeta"] = zeta
    inv = (10000.0 ** (-np.arange(0, 128, 2, dtype=np.float32) / 128.0)).astype(f)
    c["c_invf"] = np.concatenate([inv, inv]).reshape(128, 1).astype(f)
    sel = np.zeros((16, 16, 128), f)
    for e in range(16):
        sel[e, e, :] = 1.0
    c["c_sel"] = sel.reshape(16, 16 * 128)
    return c


def _prep(inp):
    f = np.float32
    A = lambda a: np.ascontiguousarray(a, dtype=f)
    sh = {}
    sh["w_ada"] = A(inp["w_ada"])
    sh["b_adaT"] = A(inp["b_ada"].reshape(2, 48, 128).transpose(2, 0, 1).reshape(128, 96))
    sh["n1gT"] = A(inp["norm1_g"].reshape(2, 8, 128).transpose(2, 0, 1).reshape(128, 16))
    sh["n2gT"] = A(inp["norm2_g"].reshape(2, 8, 128).transpose(2, 0, 1).reshape(128, 16))
    sh["w_in"] = A(inp["w_in"])
    sh["convwT"] = A(inp["conv_w"].reshape(2, 4, 8, 128).transpose(3, 0, 1, 2).reshape(128, 64))
    sh["convbT"] = A(inp["conv_b"].reshape(2, 8, 128).transpose(2, 0, 1).reshape(128, 16))
    gb = np.concatenate([inp["m_ig_b"], inp["m_fg_b"]], axis=1)
    sh["gbias"] = A(np.broadcast_to(gb[None, :, None, :], (128, 2, 8, 8)).reshape(128, 128))
    ng = np.stack([inp["m_norm_g"].reshape(2, 8, 128), inp["r_norm_g"].reshape(2, 8, 128)], axis=0)
    sh["ngT"] = A(ng.transpose(3, 0, 1, 2).reshape(128, 32))
    sh["fing"] = A(np.broadcast_to(inp["final_g"].reshape(1, 1024), (128, 1024)))
    sh["w_bm"] = A(inp["w_bm"]); sh["w_br"] = A(inp["w_br"]); sh["w_out"] = A(inp["w_out"])
    wr = np.concatenate([inp["w_r1"], inp["w_r2"]], axis=2)
    sh["wr"] = A(wr.reshape(2, 8, 128, 20).transpose(2, 0, 1, 3).reshape(128, 320))
    br = np.concatenate([inp["b_r1"], inp["b_r2"]], axis=1)
    sh["br"] = A(np.broadcast_to(br[None, :, None, :], (128, 2, 4, 20)).reshape(128, 160))
    sh["w_gate"] = A(inp["w_gate"]); sh["w_up"] = A(inp["w_up"]); sh["w_down"] = A(inp["w_down"])
    sh.update(_consts())
    maps = []
    for b in range(NCORES):
        m = dict(sh)
        m["x"] = A(inp["x"][b])
        m["cT"] = A(inp["c"][b].reshape(8, 128).T)
        m["pos"] = np.ascontiguousarray(np.broadcast_to(inp["positions"][b].astype(np.int32)[None, :], (128, S_LEN)))
        maps.append(m)
    return maps


def kernel(**inp):
    if "nc" not in _CACHE:
        _CACHE["nc"] = build()
    nc = _CACHE["nc"]
    maps = _prep(inp)
    res = run_bass_kernel_spmd(nc, maps, core_ids=list(range(NCORES)))
    return np.stack([np.asarray(r["out"], dtype=np.float32) for r in res.results], axis=0)
```

```python
import math
import types
import numpy as np
from contextlib import ExitStack
import concourse.bass as bass
import concourse.mybir as mybir
from concourse.bass_utils import run_bass_kernel_spmd

F32 = mybir.dt.float32
BF16 = mybir.dt.bfloat16
I32 = mybir.dt.int32
AF = mybir.ActivationFunctionType
ALU = mybir.AluOpType
AX = mybir.AxisListType

S_LEN = 2048
D = 1024
NCORES = 8
EPS = 1e-6
KAPPA_M = 0.25 * 128 ** -0.5
KAPPA_R = 128 ** -0.5
OFF_MQ, OFF_MK, OFF_MV, OFF_MO, OFF_MI = 0, 512, 1024, 2048, 3072
OFF_RQ, OFF_RK, OFF_RV, OFF_RG, OFF_GA, OFF_GB = 3080, 3592, 4104, 5128, 6152, 7176


def freeze(fn):
    cells = fn.__closure__
    if not cells:
        return fn
    new = []
    for c in cells:
        try:
            new.append(types.CellType(c.cell_contents))
        except ValueError:
            new.append(c)
    return types.FunctionType(fn.__code__, fn.__globals__, fn.__name__, fn.__defaults__, tuple(new))


class Buf:
    __slots__ = ("name", "w", "r", "dsem", "dcnt")

    def __init__(self, name):
        self.name = name
        self.w = None
        self.r = []
        self.dsem = None
        self.dcnt = 0


class Sched:
    ENG = ("pe", "act", "dve", "pool", "sp")

    def __init__(self, nc, stack):
        self.nc = nc
        self.stack = stack
        self.h = {"pe": nc.tensor, "act": nc.scalar, "dve": nc.vector, "pool": nc.gpsimd, "sp": nc.sync}
        self.sem = {k: stack.enter_context(nc.semaphore("s_" + k)) for k in self.ENG}
        self.cnt = {k: 0 for k in self.ENG}
        self.seen = {k: {} for k in self.ENG}
        self.prog = {k: [] for k in self.ENG}
        self.dbufs = []

    def _deps(self, eng, reads, writes):
        need = {}

        def add(ev):
            if ev is None:
                return
            s, v = ev
            if need.get(s, 0) < v:
                need[s] = v
        for b in reads:
            add(b.w)
        for b in writes:
            add(b.w)
            for ev in b.r:
                add(ev)
        out = []
        seen = self.seen[eng]
        own = self.sem[eng]
        for s, v in need.items():
            if eng == "pe" and s is own:
                continue
            if seen.get(s, 0) < v:
                seen[s] = v
                out.append((s, v))
        return out

    def _record(self, ev, reads, writes):
        for b in reads:
            b.r.append(ev)
        for b in writes:
            b.w = ev
            b.r = []

    def op(self, eng, fn, reads=(), writes=()):
        fn = freeze(fn)
        waits = self._deps(eng, reads, writes)
        self.cnt[eng] += 1
        ev = (self.sem[eng], self.cnt[eng])
        h = self.h[eng]
        sem = self.sem[eng]

        def thunk():
            for s, v in waits:
                h.wait_ge(s, v)
            fn(h).then_inc(sem, 1)
        self.prog[eng].append(thunk)
        self._record(ev, reads, writes)
        return ev

    def dma(self, eng, fn, reads=(), writes=(), track=None):
        fn = freeze(fn)
        waits = self._deps(eng, reads, writes)
        tb = track if track is not None else writes[0]
        if tb.dsem is None:
            tb.dsem = self.stack.enter_context(self.nc.semaphore("d_%d" % len(self.dbufs)))
            self.dbufs.append(tb)
        tb.dcnt += 16
        ev = (tb.dsem, tb.dcnt)
        h = self.h[eng]
        dsem = tb.dsem

        def thunk():
            for s, v in waits:
                h.wait_ge(s, v)
            fn(h).then_inc(dsem, 16)
        self.prog[eng].append(thunk)
        self._record(ev, reads, writes)
        return ev

    def barrier(self, engs=None):
        evs = [(self.sem[k], self.cnt[k]) for k in self.ENG if self.cnt[k] > 0]
        evs += [(b.dsem, b.dcnt) for b in self.dbufs]
        for eng in (engs or self.ENG):
            seen = self.seen[eng]
            waits = []
            for s, v in evs:
                if seen.get(s, 0) < v:
                    seen[s] = v
                    waits.append((s, v))
            h = self.h[eng]

            def thunk(h=h, waits=waits):
                for s, v in waits:
                    h.wait_ge(s, v)
            self.prog[eng].append(thunk)

    def emit(self):
        nc = self.nc
        with nc.Block() as block:
            @block.tensor
            def _(e):
                for f in self.prog["pe"]:
                    f()

            @block.scalar
            def _(e):
                for f in self.prog["act"]:
                    f()

            @block.vector
            def _(e):
                for f in self.prog["dve"]:
                    f()

            @block.gpsimd
            def _(e):
                for f in self.prog["pool"]:
                    f()

            @block.sync
            def _(e):
                for f in self.prog["sp"]:
                    f()


def build(stop_after=None, dumps=()):
    nc = bass.Bass("TRN2", target_bir_lowering=False)
    dt_in = lambda name, shape, dt=F32: nc.dram_tensor(name, list(shape), dt, kind="ExternalInput").ap()
    X = dt_in("x", [S_LEN, D])
    CT = dt_in("cT", [128, 8])
    POS = dt_in("pos", [128, S_LEN], I32)
    W_ADA = dt_in("w_ada", [2, D, 6 * D])
    B_ADAT = dt_in("b_adaT", [128, 96])
    N1G = dt_in("n1gT", [128, 16])
    N2G = dt_in("n2gT", [128, 16])
    W_IN = dt_in("w_in", [2, D, 8200])
    CONVW = dt_in("convwT", [128, 64])
    CONVB = dt_in("convbT", [128, 16])
    GBIAS = dt_in("gbias", [128, 2 * 8 * 8])
    NGT = dt_in("ngT", [128, 32])
    FING = dt_in("fing", [128, D])
    W_BM = dt_in("w_bm", [2, D, D])
    W_BR = dt_in("w_br", [2, D, D])
    W_OUT = dt_in("w_out", [2, D, D])
    WR = dt_in("wr", [128, 2 * 8 * 20])
    BR = dt_in("br", [128, 2 * 4 * 20])
    W_GATE = dt_in("w_gate", [2, 16, D, 512])
    W_UP = dt_in("w_up", [2, 16, D, 512])
    W_DOWN = dt_in("w_down", [2, 16, 512, D])
    C_ID = dt_in("c_ident", [128, 128])
    C_TRI = dt_in("c_tri", [128, 128])
    C_MASKM = dt_in("c_maskm", [128, 128])
    C_RETD = dt_in("c_retd", [128, 512])
    C_XI = dt_in("c_xi", [128, 512])
    C_ZETA = dt_in("c_zeta", [128, 4])
    C_INVF = dt_in("c_invf", [128, 1])
    C_SEL = dt_in("c_sel", [16, 16 * 128])
    OUT = nc.dram_tensor("out", [S_LEN, D], F32, kind="ExternalOutput").ap()
    dump_out = {}

    with ExitStack() as st:
        S = Sched(nc, st)
        _uid = [0]

        def sbt(stack, name, shape, dt=F32):
            _uid[0] += 1
            return stack.enter_context(nc.sbuf_tensor("%s_u%d" % (name, _uid[0]), list(shape), dt))
        bank = [st.enter_context(nc.psum_tensor("bank%d" % i, [128, 512], F32)) for i in range(8)]
        pb = [Buf("pb%d" % i) for i in range(8)]
        final_evs = []

        def dump(name, ap, buf, shape, dt=F32):
            if name not in dumps:
                return
            t = nc.dram_tensor("dbg_" + name, list(shape), dt, kind="ExternalOutput").ap()
            ob = Buf("dbg_" + name)
            S.dma("sp", lambda h: h.dma_start(out=t, in_=ap), reads=[buf], writes=[ob])
            final_evs.append(ob)
            dump_out[name] = True

        xT = sbt(st, "xT", [128, 8, S_LEN]); b_xT = [Buf("xT%d" % g) for g in range(4)]
        ident = sbt(st, "ident", [128, 128]); identb = sbt(st, "identb", [128, 128], BF16)
        tri = sbt(st, "tri", [128, 128]); ones32 = sbt(st, "ones32", [128, 128])
        maskm = sbt(st, "maskm", [128, 128]); retd = sbt(st, "retd", [128, 4, 128]); xi = sbt(st, "xi", [128, 4, 128])
        zeta = sbt(st, "zeta", [128, 4]); mhalf = sbt(st, "mhalf", [128, 1])
        modT = sbt(st, "modT", [128, 2, 48]); b_adaT = sbt(st, "b_adaT_s", [128, 96])
        n1g = sbt(st, "n1g", [128, 16]); n2g = sbt(st, "n2g", [128, 16])
        scale1 = sbt(st, "scale1", [128, 2, 8]); scale2 = sbt(st, "scale2", [128, 2, 8]); g1h = sbt(st, "g1h", [128, 2, 8])
        convw = sbt(st, "convw", [128, 2, 4, 8]); convb = sbt(st, "convb", [128, 2, 8])
        gbias = sbt(st, "gbias_s", [128, 2, 8, 8])
        ngT = sbt(st, "ngT_s", [128, 2, 2, 8])
        cosT = sbt(st, "cosT", [128, S_LEN]); sinS = sbt(st, "sinS", [128, S_LEN])
        C32 = [sbt(st, "C32_%d" % i, [128, 260]) for i in range(4)]
        Cb = [sbt(st, "Cb_%d" % i, [128, 260], BF16) for i in range(4)]
        R32 = [sbt(st, "R32_%d" % i, [128, 256]) for i in range(4)]
        Rb = [sbt(st, "Rb_%d" % i, [128, 256], BF16) for i in range(4)]
        halo = sbt(st, "halo", [128, 8, 4])
        b_const = Buf("const"); b_mod = Buf("mod"); b_cs = Buf("cossin")
        b_C = [Buf("C%d" % i) for i in range(4)]; b_Cb = [Buf("Cb%d" % i) for i in range(4)]
        b_R = [Buf("R%d" % i) for i in range(4)]; b_Rb = [Buf("Rb%d" % i) for i in range(4)]
        b_halo = [Buf("halo%d" % i) for i in range(8)]

        def load_const(dst, src, buf):
            S.dma("sp", lambda h: h.dma_start(out=dst, in_=src), writes=[buf])

        cb = {}
        for nm, dst, src in [("ident", ident[:], C_ID), ("tri", tri[:], C_TRI), ("maskm", maskm[:], C_MASKM),
                             ("retd", retd[:], C_RETD.rearrange("p (h l) -> p h l", h=4)),
                             ("xi", xi[:], C_XI.rearrange("p (h l) -> p h l", h=4)), ("zeta", zeta[:], C_ZETA),
                             ("b_adaT", b_adaT[:], B_ADAT), ("n1g", n1g[:], N1G), ("n2g", n2g[:], N2G),
                             ("convw", convw[:], CONVW.rearrange("p (l j k) -> p l j k", l=2, j=4)),
                             ("convb", convb[:], CONVB.rearrange("p (l k) -> p l k", l=2)),
                             ("gbias", gbias[:], GBIAS.rearrange("p (l c g) -> p l c g", l=2, c=8)),
                             ("ngT", ngT[:], NGT.rearrange("p (b l k) -> p b l k", b=2, l=2)),
                             ]:
            cb[nm] = Buf("c_" + nm)
            load_const(dst, src, cb[nm])
        S.op("dve", lambda h: h.tensor_copy(out=identb[:], in_=ident[:]), reads=[cb["ident"]], writes=[b_const])
        S.op("dve", lambda h: h.memset(ones32[:], 1.0), writes=[b_const])
        S.op("dve", lambda h: h.memset(mhalf[:], -0.5), writes=[b_const])
        ALLC = list(cb.values()) + [b_const]

        with ExitStack() as p0:
            xs = [sbt(p0, "xs%d" % i, [128, D]) for i in range(2)]
            b_xs = [Buf("xs%d" % i) for i in range(2)]
            for t in range(16):
                S.dma("sp", lambda h, t=t: h.dma_start(out=xs[t % 2][:], in_=X[t * 128:(t + 1) * 128, :]), writes=[b_xs[t % 2]])
                for hb in range(2):
                    def tr(h, t=t, hb=hb):
                        ins = None
                        for kk in range(4):
                            k = hb * 4 + kk
                            ins = h.transpose(bank[hb][:, kk * 128:(kk + 1) * 128], xs[t % 2][:, k * 128:(k + 1) * 128], ident[:])
                        return ins
                    S.op("pe", tr, reads=[b_xs[t % 2], cb["ident"]], writes=[pb[hb]])
                    eng = "act" if hb == 0 else "dve"
                    if eng == "act":
                        S.op("act", lambda h, t=t, hb=hb: h.activation(
                            out=xT[:, hb * 4:(hb + 1) * 4, t * 128:(t + 1) * 128],
                            in_=bank[hb][:].rearrange("p (k t) -> p k t", k=4), func=AF.Copy),
                            reads=[], writes=[pb[hb], b_xT[t // 4]])
                    else:
                        S.op("dve", lambda h, t=t, hb=hb: h.tensor_copy(
                            out=xT[:, hb * 4:(hb + 1) * 4, t * 128:(t + 1) * 128],
                            in_=bank[hb][:].rearrange("p (k t) -> p k t", k=4)),
                            reads=[], writes=[pb[hb], b_xT[t // 4]])
            cT = sbt(p0, "cT_s", [128, 8]); cth = sbt(p0, "cth", [128, 8]); csil = sbt(p0, "csil", [128, 8])
            b_c = Buf("c")
            S.dma("sp", lambda h: h.dma_start(out=cT[:], in_=CT), writes=[b_c])
            S.op("act", lambda h: h.activation(out=cth[:], in_=cT[:], func=AF.Tanh, scale=0.5), reads=[b_c], writes=[b_c])
            S.op("dve", lambda h: h.scalar_tensor_tensor(out=csil[:], in0=cth[:], scalar=1.0, in1=cT[:], op0=ALU.add, op1=ALU.mult),
                 reads=[b_c], writes=[b_c])
            S.op("dve", lambda h: h.tensor_scalar(out=csil[:], in0=csil[:], scalar1=0.5, scalar2=None, op0=ALU.mult),
                 reads=[b_c], writes=[b_c])
            wa = [sbt(p0, "wa%d" % i, [128, 8, 512], BF16) for i in range(4)]
            csilb = sbt(p0, "csilb", [128, 8], BF16)
            S.op("dve", lambda h: h.tensor_copy(out=csilb[:], in_=csil[:]), reads=[b_c], writes=[b_c])
            b_wa = [Buf("wa%d" % i) for i in range(4)]
            modrow = sbt(p0, "modrow", [1, 6 * D]); b_mrow = Buf("modrow")
            for l in range(2):
                for jg in range(12):
                    i = (l * 12 + jg) % 4
                    S.dma("pool", lambda h, l=l, jg=jg, i=i: h.dma_start(
                        out=wa[i][:], in_=W_ADA[l, :, jg * 512:(jg + 1) * 512].rearrange("(k p) n -> p k n", p=128)),
                        writes=[b_wa[i]])

                    pbj = 6 + (jg % 2)

                    def mm(h, i=i, pbj=pbj):
                        ins = None
                        for k in range(8):
                            ins = h.matmul(bank[pbj][0:1, :], lhsT=csilb[:, k:k + 1], rhs=wa[i][:, k, :], start=(k == 0), stop=(k == 7))
                        return ins
                    S.op("pe", mm, reads=[b_wa[i], b_c], writes=[pb[pbj]])
                    S.op("act", lambda h, jg=jg, pbj=pbj: h.activation(out=modrow[0:1, jg * 512:(jg + 1) * 512], in_=bank[pbj][0:1, :], func=AF.Copy),
                         writes=[pb[pbj], b_mrow])

                def mtr(h):
                    ins = None
                    for j in range(48):
                        ins = h.matmul(bank[5][:, j:j + 1], lhsT=modrow[0:1, j * 128:(j + 1) * 128], rhs=ones32[0:1, 0:1], start=True, stop=True)
                    return ins
                S.op("pe", mtr, reads=[b_mrow, b_const], writes=[pb[5]])
                S.op("dve", lambda h, l=l: h.tensor_tensor(out=modT[:, l, :], in0=bank[5][:, 0:48], in1=b_adaT[:, l * 48:(l + 1) * 48],
                                                          op=ALU.add), reads=[cb["b_adaT"]], writes=[pb[5], b_mod])
                S.op("dve", lambda h, l=l: h.scalar_tensor_tensor(out=scale1[:, l, :], in0=modT[:, l, 8:16], scalar=1.0,
                                                                 in1=n1g[:, l * 8:(l + 1) * 8], op0=ALU.add, op1=ALU.mult),
                     reads=[cb["n1g"]], writes=[b_mod])
                S.op("dve", lambda h, l=l: h.tensor_scalar(out=scale1[:, l, :], in0=scale1[:, l, :], scalar1=32.0, scalar2=None, op0=ALU.mult),
                     writes=[b_mod])
                S.op("dve", lambda h, l=l: h.scalar_tensor_tensor(out=scale2[:, l, :], in0=modT[:, l, 32:40], scalar=1.0,
                                                                 in1=n2g[:, l * 8:(l + 1) * 8], op0=ALU.add, op1=ALU.mult),
                     reads=[cb["n2g"]], writes=[b_mod])
                S.op("dve", lambda h, l=l: h.tensor_scalar(out=scale2[:, l, :], in0=scale2[:, l, :], scalar1=32.0, scalar2=None, op0=ALU.mult),
                     writes=[b_mod])
                S.op("dve", lambda h, l=l: h.tensor_scalar(out=g1h[:, l, :], in0=modT[:, l, 16:24], scalar1=0.5, scalar2=None, op0=ALU.mult),
                     writes=[b_mod])
            posi = sbt(p0, "posi", [128, S_LEN], I32); ang = sbt(p0, "ang", [128, S_LEN]); rr = sbt(p0, "rr", [128, S_LEN])
            kk_i = sbt(p0, "kk_i", [128, S_LEN], I32); invf = sbt(p0, "invf", [128, 1])
            b_r = Buf("rot")
            S.dma("sp", lambda h: h.dma_start(out=posi[:], in_=POS), writes=[b_r])
            S.dma("sp", lambda h: h.dma_start(out=invf[:], in_=C_INVF), writes=[b_r], track=Buf("invf"))
            S.op("dve", lambda h: h.tensor_copy(out=ang[:], in_=posi[:]), reads=[b_r], writes=[b_r])
            S.op("dve", lambda h: h.tensor_scalar(out=ang[:], in0=ang[:], scalar1=invf[:, 0:1], scalar2=None, op0=ALU.mult), writes=[b_r])
            S.op("dve", lambda h: h.tensor_scalar(out=kk_i[:], in0=ang[:], scalar1=1.0 / (2 * math.pi), scalar2=None, op0=ALU.mult), writes=[b_r])
            S.op("dve", lambda h: h.tensor_copy(out=rr[:], in_=kk_i[:]), writes=[b_r])
            S.op("dve", lambda h: h.scalar_tensor_tensor(out=ang[:], in0=rr[:], scalar=-2 * math.pi, in1=ang[:], op0=ALU.mult, op1=ALU.add),
                 writes=[b_r])

            def wrap(src):
                S.op("dve", lambda h: h.tensor_scalar(out=rr[:], in0=src[:], scalar1=math.pi, scalar2=None, op0=ALU.is_gt), writes=[b_r])
                S.op("dve", lambda h: h.scalar_tensor_tensor(out=src[:], in0=rr[:], scalar=-2 * math.pi, in1=src[:], op0=ALU.mult, op1=ALU.add),
                     writes=[b_r])
                S.op("dve", lambda h: h.tensor_scalar(out=rr[:], in0=src[:], scalar1=-math.pi, scalar2=None, op0=ALU.is_lt), writes=[b_r])
                S.op("dve", lambda h: h.scalar_tensor_tensor(out=src[:], in0=rr[:], scalar=2 * math.pi, in1=src[:], op0=ALU.mult, op1=ALU.add),
                     writes=[b_r])
            wrap(ang)
            S.op("act", lambda h: h.activation(out=sinS[:], in_=ang[:], func=AF.Sin), reads=[b_r], writes=[b_cs])
            S.op("dve", lambda h: h.tensor_scalar(out=ang[:], in0=ang[:], scalar1=math.pi / 2, scalar2=None, op0=ALU.add), reads=[b_cs], writes=[b_r])
            wrap(ang)
            S.op("act", lambda h: h.activation(out=cosT[:], in_=ang[:], func=AF.Sin), reads=[b_r], writes=[b_cs])
            S.op("act", lambda h: h.mul(out=sinS[0:64, :], in_=sinS[0:64, :], mul=-1.0), writes=[b_cs])
            dump("modT", modT[:], b_mod, [128, 2, 48])
            dump("cosT", cosT[:], b_cs, [128, S_LEN])
            dump("sinS", sinS[:], b_cs, [128, S_LEN])
            S.barrier()
        if stop_after == "p0":
            return finish(nc, S, st, final_evs, xT, b_xT, bank, pb, ident, FING, cb, mhalf, OUT, sbt)

        for l in range(2):
            with ExitStack() as mx:
                hT = sbt(mx, "hT", [128, 8, 1024], BF16); b_hT = [Buf("hT%d" % i) for i in range(2)]
                ymT = sbt(mx, "ymT", [128, 8, 1024], BF16); b_ymT = [Buf("ymT%d" % i) for i in range(8)]
                yT = sbt(mx, "yT", [128, 8, 1024], BF16); b_yT = [Buf("yT%d" % i) for i in range(2)]
                wq = [sbt(mx, "wq%d" % i, [128, 8, 128], BF16) for i in range(2)]; b_wq = [Buf("wq%d" % i) for i in range(2)]
                wk = [sbt(mx, "wk%d" % i, [128, 8, 128], BF16) for i in range(2)]; b_wk = [Buf("wk%d" % i) for i in range(2)]
                wvo = [sbt(mx, "wvo%d" % i, [128, 8, 512], BF16) for i in range(2)]; b_wvo = [Buf("wvo%d" % i) for i in range(2)]
                wif = sbt(mx, "wif", [128, 8, 8], BF16); b_wif = Buf("wif")
                pad = [sbt(mx, "pad%d" % i, [128, 516]) for i in range(2)]; b_pad = [Buf("pad%d" % i) for i in range(2)]
                acc = sbt(mx, "acc", [128, 512]); th = sbt(mx, "th", [128, 512]); b_acc = Buf("acc"); b_th = Buf("th")
                sq = [acc, th]; b_sq = [b_acc, b_th]
                ssb = pad[0][:, 0:512]; rstd = pad[1][:, 0:512]; b_ss = b_pad[0]; b_rstd = b_pad[1]
                qTs = [sbt(mx, "qT%d" % i, [128, 1024], BF16) for i in range(2)]
                kTs = [sbt(mx, "kT%d" % i, [128, 1024], BF16) for i in range(2)]
                qxTs = [sbt(mx, "qxT%d" % i, [128, 1024], BF16) for i in range(2)]
                b_qs_ = [[Buf("q%d_%d" % (j, i)) for i in range(2)] for j in range(2)]
                b_ks_ = [[Buf("k%d_%d" % (j, i)) for i in range(2)] for j in range(2)]
                b_qxs_ = [[Buf("qx%d_%d" % (j, i)) for i in range(2)] for j in range(2)]
                vext = [sbt(mx, "vext%d" % i, [128, 260], BF16) for i in range(2)]; b_v = [Buf("v%d" % i) for i in range(2)]
                gsig = [sbt(mx, "gsig%d" % i, [128, 256], BF16) for i in range(3)]; b_gs = [Buf("gs%d" % i) for i in range(3)]
                tho = [sbt(mx, "tho%d" % i, [128, 256]) for i in range(2)]; b_tho = [Buf("tho%d" % i) for i in range(2)]
                PT = [sbt(mx, "PT%d" % i, [128, 128], BF16) for i in range(2)]; b_PT = [Buf("PT%d" % i) for i in range(2)]
                kw = [sbt(mx, "kw%d" % i, [128, 128], BF16) for i in range(2)]; b_kw = [Buf("kw%d" % i) for i in range(2)]
                ymc = [sbt(mx, "ymc%d" % i, [128, 256], BF16) for i in range(2)]; b_ymc = [Buf("ymc%d" % i) for i in range(2)]
                tinys = [sbt(mx, "tiny%d" % i, [128, 16]) for i in range(2)]; b_tinys = [Buf("tiny%d" % i) for i in range(2)]
                halo2 = sbt(mx, "halo2", [128, 2, 4]); b_halo2 = [Buf("halo2_%d" % i) for i in range(2)]
                pdb = [sbt(mx, "pdb%d" % i, [128, 516], BF16) for i in range(2)]; b_pdb = [Buf("pdb%d" % i) for i in range(2)]
                dg = sbt(mx, "dg", [128, 2, 4, 128], BF16); b_dg = [Buf("dg%d" % i) for i in range(2)]
                gpre = sbt(mx, "gpre", [128, 8, 8]); lfp = sbt(mx, "lfp", [128, 8, 4]); a_t = sbt(mx, "a_t", [128, 8, 4])
                w_t = sbt(mx, "w_t", [128, 8, 4]); el_t = sbt(mx, "el_t", [128, 8, 4]); dec_t = sbt(mx, "dec_t", [128, 8, 4])
                wdec_t = sbt(mx, "wdec_t", [128, 8, 4]); b_g = Buf("gates")
                wb = wq; b_wb = b_wq
                wg = wk; b_wg = b_wk
                for i in range(2):
                    S.op("pool", lambda h, i=i: h.memset(vext[i][:, 256:260], 1.0), writes=[b_v[i]])
                wcnt = [0]

                def load_w(dst, col0, ncols, buf, l=l):
                    S.dma("pool", lambda h: h.dma_start(out=dst, in_=W_IN[l, :, col0:col0 + ncols].rearrange("(k p) n -> p k n", p=128)),
                          writes=[buf])

                for hf in range(2):
                    T0 = hf * 1024
                    for t01 in range(2):
                        wi01 = (wcnt[0] + t01) % 2
                        load_w(wq[wi01][:], OFF_MQ + t01 * 128, 128, b_wq[wi01])
                        load_w(wk[wi01][:], OFF_MK + t01 * 128, 128, b_wk[wi01])
                    for tg in range(2):
                        g = hf * 2 + tg
                        cols = slice(g * 512, (g + 1) * 512)
                        lc = slice(tg * 512, (tg + 1) * 512)
                        for k in range(8):
                            S.op("act", lambda h, k=k, cols=cols: h.activation(out=sq[k % 2][:], in_=xT[:, k, cols], func=AF.Square),
                                 reads=[b_xT[g]], writes=[b_sq[k % 2]])
                            S.op("pe", lambda h, k=k: h.matmul(bank[0][:], lhsT=ones32[:], rhs=sq[k % 2][:], start=(k == 0), stop=(k == 7)),
                                 reads=[b_sq[k % 2], b_const], writes=[pb[0]])
                        S.op("act", lambda h: h.activation(out=ssb, in_=bank[0][:], func=AF.Ln, bias=1024.0 * EPS),
                             writes=[pb[0], b_ss])
                        S.op("act", lambda h: h.activation(out=rstd, in_=ssb, func=AF.Exp, scale=-0.5),
                             reads=[b_ss], writes=[b_rstd])
                        for k in range(8):
                            S.op("dve", lambda h, k=k, cols=cols: h.scalar_tensor_tensor(
                                out=sq[k % 2][:], in0=xT[:, k, cols], scalar=scale1[:, l, k:k + 1], in1=rstd, op0=ALU.mult, op1=ALU.mult),
                                reads=[b_xT[g], b_rstd, b_mod], writes=[b_sq[k % 2]])
                            S.op("act", lambda h, k=k, lc=lc: h.activation(out=hT[:, k, lc], in_=sq[k % 2][:], func=AF.Identity,
                                                                           bias=modT[:, l, k:k + 1]),
                                 reads=[b_sq[k % 2], b_mod], writes=[b_hT[tg]])
                    if l == 0 and hf == 0:
                        dump("hT", hT[:], b_hT[1], [128, 8, 1024], BF16)
                    load_w(wif[:], OFF_MI, 8, b_wif)
                    psG = bank[6][:, 0:64].rearrange("p (c g) -> p c g", c=8)
                    psNB = bank[6][:, 64:96].rearrange("p (c g) -> p c g", c=8)
                    psNT = bank[6][:, 96:128].rearrange("p (c g) -> p c g", c=8)

                    def gmm(h):
                        ins = None
                        for c in range(8):
                            for k in range(8):
                                ins = h.matmul(psG[:, c, :], lhsT=hT[:, k, c * 128:(c + 1) * 128], rhs=wif[:, k, :], start=(k == 0), stop=(k == 7))
                        return ins
                    S.op("pe", gmm, reads=[b_hT[0], b_hT[1], b_wif], writes=[pb[6]])
                    S.op("dve", lambda h: h.tensor_tensor(out=gpre[:], in0=psG, in1=gbias[:, l, :, :], op=ALU.add),
                         reads=[cb["gbias"]], writes=[pb[6], b_g])
                    S.op("act", lambda h: h.activation(out=lfp[:], in_=gpre[:, :, 4:8], func=AF.Exp, scale=-1.0), writes=[b_g])
                    S.op("act", lambda h: h.activation(out=lfp[:], in_=lfp[:], func=AF.Ln, bias=1.0), writes=[b_g])

                    def nbmm(h):
                        ins = None
                        for c in range(8):
                            h.matmul(psNB[:, c, :], lhsT=tri[:], rhs=lfp[:, c, :], start=True, stop=True)
                            ins = h.matmul(psNT[:, c, :], lhsT=ones32[:], rhs=lfp[:, c, :], start=True, stop=True)
                        return ins
                    S.op("pe", nbmm, reads=[b_g, cb["tri"], b_const], writes=[pb[6]])
                    S.op("dve", lambda h: h.tensor_tensor(out=a_t[:], in0=psNB, in1=gpre[:, :, 0:4], op=ALU.add), writes=[pb[6], b_g])
                    S.op("act", lambda h: h.activation(out=w_t[:], in_=a_t[:], func=AF.Exp), writes=[b_g])
                    S.op("act", lambda h: h.activation(out=el_t[:], in_=psNB, func=AF.Exp, scale=-1.0), writes=[pb[6], b_g])
                    S.op("act", lambda h: h.activation(out=dec_t[:], in_=psNT, func=AF.Exp, scale=-1.0), writes=[pb[6], b_g])
                    S.op("dve", lambda h: h.tensor_tensor(out=wdec_t[:], in0=w_t[:], in1=dec_t[:], op=ALU.mult), writes=[b_g])
                    if l == 0 and hf == 0:
                        dump("w_t", w_t[:], b_g, [128, 8, 4]); dump("el_t", el_t[:], b_g, [128, 8, 4]); dump("dec_t", dec_t[:], b_g, [128, 8, 4])

                    OFFS = {True: (OFF_MQ, OFF_MK, OFF_MV, OFF_MO), False: (OFF_RQ, OFF_RK, OFF_RV, OFF_RG)}
                    tasks = []
                    for is_m_ in (True, False):
                        for hd_ in range(4):
                            tasks.append((is_m_, hd_, wcnt[0] % 2))
                            wcnt[0] += 1

                    def loads_qk(task):
                        is_m, hd, wi = task
                        oq, ok, ov, oo = OFFS[is_m]
                        load_w(wq[wi][:], oq + hd * 128, 128, b_wq[wi])
                        load_w(wk[wi][:], ok + hd * 128, 128, b_wk[wi])

                    def loads_vo(task):
                        is_m, hd, wi = task
                        oq, ok, ov, oo = OFFS[is_m]
                        load_w(wvo[wi][:, :, 0:256], ov + hd * 256, 256, b_wvo[wi])
                        load_w(wvo[wi][:, :, 256:512], oo + hd * 256, 256, b_wvo[wi])

                    def prologue_piece(task, which, tg, part):
                        is_m, hd, wi = task
                        qT = qTs[wi]; kT = kTs[wi]; qxT = qxTs[wi]
                        b_q = b_qs_[wi]; b_k = b_ks_[wi]; b_qx = b_qxs_[wi]
                        wsel = (wq, b_wq) if which == 0 else (wk, b_wk)
                        dstT, b_dst = (qT, b_q) if which == 0 else (kT, b_k)
                        g = hf * 2 + tg
                        lc = slice(tg * 512, (tg + 1) * 512)
                        gc = slice(g * 512, (g + 1) * 512)
                        pbi = 6

                        def pmm(h):
                            ins = None
                            for k in range(8):
                                ins = h.matmul(bank[pbi][:], lhsT=wsel[0][wi][:, k, :], rhs=hT[:, k, lc], start=(k == 0), stop=(k == 7))
                            return ins
                        if part == 0:
                            S.op("pe", pmm, reads=[wsel[1][wi], b_hT[tg]], writes=[pb[pbi]])
                        if is_m and part == 0:
                            hidx = hd * 2 + which
                            pd = pdb[tg]
                            kb = which * 4 + hd
                            if g == 0:
                                S.op("dve", lambda h: h.memset(pd[:, 0:3], 0.0), writes=[b_pdb[tg]])
                            elif tg == 0:
                                S.op("dve", lambda h: h.tensor_copy(out=pd[:, 0:3], in_=halo[:, hidx, 0:3]),
                                     reads=[b_halo[hidx]], writes=[b_pdb[tg]])
                            else:
                                S.op("dve", lambda h: h.tensor_copy(out=pd[:, 0:3], in_=halo2[:, which, 0:3]),
                                     reads=[b_halo2[which]], writes=[b_pdb[tg]])
                            if tg == 0:
                                for j in range(4):
                                    S.op("dve", lambda h, j=j: h.tensor_scalar(out=dg[:, which, j, :], in0=identb[:], scalar1=convw[:, l, j, kb:kb + 1],
                                                                             scalar2=None, op0=ALU.mult),
                                         reads=[b_const, cb["convw"]], writes=[b_dg[which]])
                            S.op("act", lambda h: h.activation(out=pd[:, 3:515], in_=bank[pbi][:], func=AF.Copy),
                                 writes=[pb[pbi], b_pdb[tg]])
                            if tg == 1:
                                S.op("dve", lambda h: h.tensor_copy(out=halo[:, hidx, 0:3], in_=pd[:, 512:515]),
                                     reads=[b_pdb[tg]], writes=[b_halo[hidx]])
                            else:
                                S.op("dve", lambda h: h.tensor_copy(out=halo2[:, which, 0:3], in_=pd[:, 512:515]),
                                     reads=[b_pdb[tg]], writes=[b_halo2[which]])
                        if is_m and part == 1:
                            pd = pdb[tg]
                            kb = which * 4 + hd

                            def cmm(h):
                                ins = None
                                for j in range(4):
                                    ins = h.matmul(bank[pbi][:], lhsT=dg[:, which, j, :], rhs=pd[:, j:j + 512], start=(j == 0), stop=(j == 3))
                                return ins
                            S.op("pe", cmm, reads=[b_dg[which], b_pdb[tg]], writes=[pb[pbi]])
                            S.op("act", lambda h: h.activation(out=acc[:], in_=bank[pbi][:], func=AF.Identity, bias=convb[:, l, kb:kb + 1]),
                                 reads=[cb["convb"]], writes=[pb[pbi], b_acc])
                            S.op("act", lambda h: h.activation(out=th[:], in_=acc[:], func=AF.Tanh, scale=0.5), reads=[b_acc], writes=[b_th])
                            S.op("dve", lambda h: h.scalar_tensor_tensor(
                                out=dstT[:, lc], in0=th[:], scalar=1.0, in1=acc[:], op0=ALU.add, op1=ALU.mult),
                                reads=[b_th, b_acc], writes=[b_dst[tg]])
                        if (not is_m) and part == 0:
                            S.op("act", lambda h: h.activation(out=th[0:64, :], in_=bank[pbi][64:128, :], func=AF.Copy),
                                 writes=[pb[pbi], b_th])
                            S.op("act", lambda h: h.activation(out=th[64:128, :], in_=bank[pbi][0:64, :], func=AF.Copy),
                                 writes=[pb[pbi], b_th])
                            S.op("dve", lambda h: h.tensor_tensor(out=acc[:], in0=bank[pbi][:], in1=cosT[:, gc], op=ALU.mult),
                                 reads=[b_cs], writes=[pb[pbi], b_acc])
                        if (not is_m) and part == 1:
                            S.op("dve", lambda h: h.tensor_tensor(out=th[:], in0=th[:], in1=sinS[:, gc], op=ALU.mult),
                                 reads=[b_cs], writes=[b_th])
                            if which == 0:
                                S.op("dve", lambda h: h.tensor_tensor(out=acc[:], in0=acc[:], in1=th[:], op=ALU.add),
                                     reads=[b_th], writes=[b_acc])
                                S.op("act", lambda h: h.activation(out=qT[:, lc], in_=acc[:], func=AF.Copy),
                                     reads=[b_acc], writes=[b_q[tg]])
                                S.op("dve", lambda h: h.tensor_tensor(
                                    out=qxT[:, lc].rearrange("p (c l) -> p c l", c=4), in0=acc[:].rearrange("p (c l) -> p c l", c=4),
                                    in1=xi[:, hd:hd + 1, :].to_broadcast([128, 4, 128]), op=ALU.mult),
                                    reads=[b_acc, cb["xi"]], writes=[b_qx[tg]])
                            else:
                                S.op("dve", lambda h: h.tensor_tensor(out=kT[:, lc], in0=acc[:], in1=th[:], op=ALU.add),
                                     reads=[b_th, b_acc], writes=[b_k[tg]])

                    PIECES = [(0, 0), (1, 0), (0, 1), (1, 1)]

                    def make_stages(task):
                        is_m, hd, wi = task
                        qT = qTs[wi]; kT = kTs[wi]; qxT = qxTs[wi]
                        b_q = b_qs_[wi]; b_k = b_ks_[wi]; b_qx = b_qxs_[wi]
                        stt = (C32[hd], Cb[hd], b_C[hd], b_Cb[hd]) if is_m else (R32[hd], Rb[hd], b_R[hd], b_Rb[hd])
                        NW = 257 if is_m else 256
                        qsrc = qT if is_m else qxT
                        b_qs = b_q if is_m else b_qx
                        kap = KAPPA_M if is_m else KAPPA_R
                        VB = (7, 1); SB = (4, 4); OB = (2, 0)

                        def stage_A(c, idx):
                            tg = c // 4
                            cc = slice(c * 128, (c + 1) * 128)
                            vi = idx % 2
                            vb = VB[vi]; sbk = SB[vi]

                            def vmm(h):
                                ins = None
                                for k in range(8):
                                    ins = h.matmul(bank[vb][:], lhsT=hT[:, k, cc], rhs=wvo[wi][:, k, :], start=(k == 0), stop=(k == 7))
                                return ins
                            S.op("pe", vmm, reads=[b_hT[tg], b_wvo[wi]], writes=[pb[vb]])
                            S.op("act", lambda h: h.activation(out=vext[vi][:, 0:256], in_=bank[vb][:, 0:256], func=AF.Copy),
                                 writes=[pb[vb], b_v[vi]])
                            S.op("act", lambda h: h.activation(out=tho[vi][:], in_=bank[vb][:, 256:512], func=AF.Tanh, scale=0.5),
                                 writes=[pb[vb], b_tho[vi]])
                            if is_m:
                                S.op("dve", lambda h: h.tensor_scalar(out=gsig[idx % 3][:], in0=tho[vi][:], scalar1=1.0, scalar2=None, op0=ALU.add),
                                     reads=[b_tho[vi]], writes=[b_gs[idx % 3]])
                            else:
                                S.op("dve", lambda h: h.scalar_tensor_tensor(
                                    out=gsig[idx % 3][:], in0=tho[vi][:], scalar=1.0, in1=bank[vb][:, 256:512], op0=ALU.add, op1=ALU.mult),
                                    reads=[b_tho[vi]], writes=[pb[vb], b_gs[idx % 3]])
                            S.op("pe", lambda h: h.matmul(bank[sbk][:, 0:128], lhsT=kT[:, cc], rhs=qT[:, cc], start=True, stop=True),
                                 reads=[b_k[tg], b_q[tg]], writes=[pb[sbk]])
                            if is_m:
                                S.op("dve", lambda h: h.scalar_tensor_tensor(
                                    out=PT[vi][:], in0=bank[sbk][:, 0:128], scalar=w_t[:, c, hd:hd + 1], in1=maskm[:], op0=ALU.mult, op1=ALU.mult),
                                    reads=[b_g, cb["maskm"]], writes=[pb[sbk], b_PT[vi]])
                            else:
                                S.op("dve", lambda h: h.tensor_tensor(out=PT[vi][:], in0=bank[sbk][:, 0:128], in1=retd[:, hd, :], op=ALU.mult),
                                     reads=[cb["retd"]], writes=[pb[sbk], b_PT[vi]])
                            psT = bank[5][:, 0:64].bitcast(BF16)
                            S.op("pe", lambda h: h.transpose(psT, kT[:, cc], identb[:]), reads=[b_k[tg], b_const], writes=[pb[5]])
                            if is_m:
                                S.op("act", lambda h: h.activation(out=kw[vi][:], in_=psT, func=AF.Copy, scale=wdec_t[:, c, hd:hd + 1]),
                                     reads=[b_g], writes=[pb[5], b_kw[vi]])
                            else:
                                S.op("act", lambda h: h.activation(out=kw[vi][:], in_=psT, func=AF.Copy, scale=zeta[:, hd:hd + 1]),
                                     reads=[cb["zeta"]], writes=[pb[5], b_kw[vi]])

                        def stage_B(c, idx):
                            gci = hf * 8 + c
                            tg = c // 4
                            cc = slice(c * 128, (c + 1) * 128)
                            vi = idx % 2
                            ob_ = OB[vi]

                            def omm(h):
                                ins = h.matmul(bank[ob_][:, 0:NW], lhsT=PT[vi][:], rhs=vext[vi][:, 0:NW], start=True, stop=(gci == 0))
                                if gci > 0:
                                    ins = h.matmul(bank[ob_][:, 0:NW], lhsT=qsrc[:, cc], rhs=stt[1][:, 0:NW], start=False, stop=True)
                                return ins
                            S.op("pe", omm, reads=[b_PT[vi], b_v[vi], b_qs[tg], stt[3]], writes=[pb[ob_]])
                            S.op("pe", lambda h: h.matmul(bank[3][:, 0:NW], lhsT=kw[vi][:], rhs=vext[vi][:, 0:NW], start=True, stop=True),
                                 reads=[b_kw[vi], b_v[vi]], writes=[pb[3]])
                            if gci == 0:
                                S.op("dve", lambda h: h.tensor_copy(out=stt[0][:, 0:NW], in_=bank[3][:, 0:NW]),
                                     writes=[pb[3], stt[2]])
                            elif is_m:
                                S.op("dve", lambda h: h.scalar_tensor_tensor(
                                    out=stt[0][:, 0:NW], in0=stt[0][:, 0:NW], scalar=dec_t[:, c, hd:hd + 1], in1=bank[3][:, 0:NW],
                                    op0=ALU.mult, op1=ALU.add), reads=[b_g], writes=[pb[3], stt[2]])
                            else:
                                gam = (1.0 - 2.0 ** (-5.0 - hd)) ** 128
                                S.op("dve", lambda h: h.scalar_tensor_tensor(
                                    out=stt[0][:, 0:NW], in0=stt[0][:, 0:NW], scalar=float(gam), in1=bank[3][:, 0:NW],
                                    op0=ALU.mult, op1=ALU.add), writes=[pb[3], stt[2]])
                            S.op("act", lambda h: h.activation(out=stt[1][:, 0:NW], in_=stt[0][:, 0:NW], func=AF.Copy, scale=float(kap)),
                                 reads=[stt[2]], writes=[stt[3]])
                            tny = tinys[idx % 2]; b_tny = b_tinys[idx % 2]
                            if is_m:
                                S.op("dve", lambda h: h.tensor_scalar(out=tny[:, 0:1], in0=bank[ob_][:, 256:257], scalar1=el_t[:, c, hd:hd + 1],
                                                                      scalar2=None, op0=ALU.mult), reads=[b_g], writes=[pb[ob_], b_tny])
                                S.op("dve", lambda h: h.scalar_tensor_tensor(out=tny[:, 1:2], in0=tny[:, 0:1], scalar=-1.0, in1=tny[:, 0:1],
                                                                             op0=ALU.mult, op1=ALU.max), writes=[b_tny])
                                S.op("dve", lambda h: h.tensor_scalar(out=tny[:, 2:3], in0=tny[:, 1:2], scalar1=1.0, scalar2=None, op0=ALU.max),
                                     writes=[b_tny])
                                S.op("dve", lambda h: h.reciprocal(out=tny[:, 3:4], in_=tny[:, 2:3]), writes=[b_tny])
                                S.op("dve", lambda h: h.tensor_scalar(out=tny[:, 4:5], in0=tny[:, 3:4], scalar1=el_t[:, c, hd:hd + 1],
                                                                      scalar2=None, op0=ALU.mult), reads=[b_g], writes=[b_tny])
                                S.op("act", lambda h: h.activation(out=tho[vi][:], in_=bank[ob_][:, 0:256], func=AF.Square, scale=tny[:, 4:5],
                                                                   accum_out=tny[:, 5:6]), reads=[b_tny], writes=[pb[ob_], b_tho[vi], b_tny])
                            else:
                                S.op("act", lambda h: h.activation(out=tho[vi][:], in_=bank[ob_][:, 0:256], func=AF.Square,
                                                                   accum_out=tny[:, 5:6]), writes=[pb[ob_], b_tho[vi], b_tny])

                        def stage_C1b(c, idx):
                            vi = idx % 2
                            ob_ = OB[vi]
                            tny = tinys[idx % 2]; b_tny = b_tinys[idx % 2]
                            S.op("dve", lambda h: h.tensor_scalar(out=tny[:, 6:7], in0=tny[:, 5:6], scalar1=4.0 / 256.0, scalar2=4.0 * EPS,
                                                                  op0=ALU.mult, op1=ALU.add), writes=[b_tny])
                            S.op("pool", lambda h: h.tensor_tensor(out=tny[:, 7:8], in0=tny[:, 6:7], in1=mhalf[:, 0:1], op=ALU.pow),
                                 reads=[b_const], writes=[b_tny])
                            if is_m:
                                S.op("dve", lambda h: h.tensor_tensor(out=tny[:, 8:9], in0=tny[:, 7:8], in1=tny[:, 4:5], op=ALU.mult), writes=[b_tny])
                                sc_ap = tny[:, 8:9]
                            else:
                                sc_ap = tny[:, 7:8]
                            S.op("dve", lambda h: h.scalar_tensor_tensor(
                                out=ymc[vi][:], in0=bank[ob_][:, 0:256], scalar=sc_ap, in1=gsig[idx % 3][:], op0=ALU.mult, op1=ALU.mult),
                                reads=[b_tny, b_gs[idx % 3]], writes=[pb[ob_], b_ymc[vi]])


                        def stage_C2(c, idx):
                            cc = slice(c * 128, (c + 1) * 128)
                            vi = idx % 2
                            psY = bank[5][:, 64:192].bitcast(BF16).rearrange("p (a b) -> p a b", a=2)

                            def ytr(h):
                                h.transpose(psY[:, 0, :], ymc[vi][:, 0:128], identb[:])
                                return h.transpose(psY[:, 1, :], ymc[vi][:, 128:256], identb[:])
                            S.op("pe", ytr, reads=[b_ymc[vi], b_const], writes=[pb[5]])
                            bri = 0 if is_m else 1
                            S.op("act", lambda h: h.activation(out=ymT[:, hd * 2, cc], in_=psY[:, 0, :], func=AF.Copy, scale=ngT[:, bri, l, hd * 2:hd * 2 + 1]),
                                 reads=[cb["ngT"]], writes=[pb[5], b_ymT[c]])
                            S.op("act", lambda h: h.activation(out=ymT[:, hd * 2 + 1, cc], in_=psY[:, 1, :], func=AF.Copy, scale=ngT[:, bri, l, hd * 2 + 1:hd * 2 + 2]),
                                 reads=[cb["ngT"]], writes=[pb[5], b_ymT[c]])

                        return {"A": stage_A, "B": stage_B, "C1b": stage_C1b, "C2": stage_C2}

                    def branch_proj(is_m):
                        if l == 0 and hf == 0:
                            dump("ymT" if is_m else "yrT", ymT[:], b_ymT[7], [128, 8, 1024], BF16)
                        wsrc = W_BM if is_m else W_BR
                        og = OFF_GA if is_m else OFF_GB
                        for j in range(8):
                            ji = j % 2
                            S.dma("pool", lambda h, j=j, ji=ji, wsrc=wsrc: h.dma_start(
                                out=wb[ji][:], in_=wsrc[l, :, j * 128:(j + 1) * 128].rearrange("(k p) n -> p k n", p=128)), writes=[b_wb[ji]])
                            load_w(wg[ji][:], og + j * 128, 128, b_wg[ji])
                            for tg in range(2):
                                lc = slice(tg * 512, (tg + 1) * 512)
                                nn = (j * 2 + tg) % 2
                                bA = 0 + 2 * nn
                                bB = 1 + 2 * nn
                                tb_ = pad[nn][:, 0:512]
                                b_tb = b_pad[nn]
                                tmp_ = acc if nn == 0 else th
                                b_tmp = b_acc if nn == 0 else b_th

                                def bmm(h, ji=ji, lc=lc, bA=bA):
                                    ins = None
                                    for k in range(8):
                                        ins = h.matmul(bank[bA][:], lhsT=wb[ji][:, k, :], rhs=ymT[:, k, lc], start=(k == 0), stop=(k == 7))
                                    return ins
                                S.op("pe", bmm, reads=[b_wb[ji]] + b_ymT[tg * 4:(tg + 1) * 4], writes=[pb[bA]])

                                def gmm2(h, ji=ji, lc=lc, bB=bB):
                                    ins = None
                                    for k in range(8):
                                        ins = h.matmul(bank[bB][:], lhsT=wg[ji][:, k, :], rhs=hT[:, k, lc], start=(k == 0), stop=(k == 7))
                                    return ins
                                S.op("pe", gmm2, reads=[b_wg[ji], b_hT[tg]], writes=[pb[bB]])
                                S.op("act", lambda h: h.activation(out=tb_, in_=bank[bB][:], func=AF.Tanh, scale=0.5), writes=[pb[bB], b_tb])
                                if is_m:
                                    S.op("dve", lambda h: h.scalar_tensor_tensor(
                                        out=yT[:, j, lc], in0=tb_, scalar=1.0, in1=bank[bA][:], op0=ALU.add, op1=ALU.mult),
                                        reads=[b_tb], writes=[pb[bA], b_yT[tg]])
                                else:
                                    S.op("dve", lambda h: h.scalar_tensor_tensor(
                                        out=tmp_[:], in0=tb_, scalar=1.0, in1=bank[bA][:], op0=ALU.add, op1=ALU.mult),
                                        reads=[b_tb], writes=[pb[bA], b_tmp])
                                    S.op("dve", lambda h: h.tensor_tensor(out=yT[:, j, lc], in0=yT[:, j, lc], in1=tmp_[:], op=ALU.add),
                                         reads=[b_tmp], writes=[b_yT[tg]])
                    def run_stream(tis):
                        stg = {ti: make_stages(tasks[ti]) for ti in tis}
                        seq = [(ti, c) for ti in tis for c in range(8)]
                        n = len(seq)

                        def call(kind, i):
                            ti, c = seq[i]
                            stg[ti][kind](c, i)
                        call("A", 0)
                        for i in range(n):
                            ti, c = seq[i]
                            nxt = tasks[ti + 1] if ti + 1 < 8 else None
                            if c == 0:
                                if ti + 2 < 8 and ti + 2 != 5:
                                    loads_qk(tasks[ti + 2])
                                if ti == 4:
                                    loads_qk(tasks[5])
                                if nxt is not None:
                                    loads_vo(nxt)
                            if i + 1 < n:
                                call("A", i + 1)
                            call("B", i)
                            if i >= 1:
                                call("C1b", i - 1)
                            if i >= 2:
                                call("C2", i - 2)
                            if nxt is not None:
                                w_, t_ = PIECES[c // 2]
                                prologue_piece(nxt, w_, t_, c % 2)
                        call("C1b", n - 1)
                        call("C2", n - 2)
                        call("C2", n - 1)

                    loads_vo(tasks[0])
                    for w_, t_ in PIECES:
                        prologue_piece(tasks[0], w_, t_, 0)
                        prologue_piece(tasks[0], w_, t_, 1)
                    run_stream([0, 1, 2, 3])
                    branch_proj(True)
                    run_stream([4, 5, 6, 7])
                    branch_proj(False)
                    if l == 0 and hf == 0:
                        dump("yT", yT[:], b_yT[1], [128, 8, 1024], BF16)
                    for j in range(8):
                        ji = j % 2
                        S.dma("pool", lambda h, j=j, ji=ji: h.dma_start(
                            out=wb[ji][:], in_=W_OUT[l, :, j * 128:(j + 1) * 128].rearrange("(k p) n -> p k n", p=128)), writes=[b_wb[ji]])
                        for tg in range(2):
                            g = hf * 2 + tg
                            lc = slice(tg * 512, (tg + 1) * 512)
                            gc = slice(g * 512, (g + 1) * 512)

                            def omm2(h, ji=ji, lc=lc, tg=tg):
                                ins = None
                                for k in range(8):
                                    ins = h.matmul(bank[tg][:], lhsT=wb[ji][:, k, :], rhs=yT[:, k, lc], start=(k == 0), stop=(k == 7))
                                return ins
                            S.op("pe", omm2, reads=[b_wb[ji], b_yT[tg]], writes=[pb[tg]])
                            S.op("dve", lambda h, j=j, gc=gc, tg=tg: h.scalar_tensor_tensor(
                                out=xT[:, j, gc], in0=bank[tg][:], scalar=g1h[:, l, j:j + 1], in1=xT[:, j, gc], op0=ALU.mult, op1=ALU.add),
                                reads=[b_mod], writes=[pb[tg], b_xT[g]])
                S.barrier()
            if stop_after == "mix%d" % l:
                return finish(nc, S, st, final_evs, xT, b_xT, bank, pb, ident, FING, cb, mhalf, OUT, sbt)

            with ExitStack() as mo:
                h2T = sbt(mo, "h2T", [128, 8, S_LEN], BF16); b_h2 = [Buf("h2_%d" % i) for i in range(4)]
                gT = sbt(mo, "gT", [16, S_LEN]); b_gT = [Buf("gT%d" % i) for i in range(4)]
                mo1 = ExitStack(); mo1.__enter__()
                rts = [sbt(mo1, "rt%d" % i, [128, 4, 64]) for i in range(2)]; b_rts = [Buf("rt%d" % i) for i in range(2)]
                gfull = sbt(mo1, "gfull", [128, 4, 16]); b_gf = Buf("gfull")
                wr = sbt(mo1, "wr_s", [128, 8, 20]); brs = sbt(mo1, "br_s", [128, 4, 20])
                cb["wr"] = Buf("c_wr"); cb["br"] = Buf("c_br")
                S.dma("sp", lambda h: h.dma_start(out=wr[:], in_=WR[:, l * 160:(l + 1) * 160].rearrange("p (k n) -> p k n", k=8)), writes=[cb["wr"]])
                S.dma("sp", lambda h: h.dma_start(out=brs[:], in_=BR[:, l * 80:(l + 1) * 80].rearrange("p (t n) -> p t n", t=4)), writes=[cb["br"]])
                h32k = [sbt(mo1, "h32k%d" % i, [128, 512]) for i in range(2)]; b_h32k = [Buf("h32k%d" % i) for i in range(2)]
                lgT = sbt(mo1, "lgT", [20, 512]); b_lgT = Buf("lgT")
                sq = [sbt(mo1, "msq%d" % i, [128, 512]) for i in range(2)]; b_sq = [Buf("msq%d" % i) for i in range(2)]
                ssb = sbt(mo1, "mssb", [128, 512]); rstd = sbt(mo1, "mrstd", [128, 512]); b_ss = Buf("mss"); b_rstd = Buf("mrstd")
                psL = bank[7][:, 0:80].rearrange("p (t n) -> p t n", t=4)
                psLT = bank[6][0:20, :]

                def norm_part(g):
                    cols = slice(g * 512, (g + 1) * 512)
                    for k in range(8):
                        S.op("act", lambda h, k=k: h.activation(out=sq[k % 2][:], in_=xT[:, k, cols], func=AF.Square),
                             reads=[b_xT[g]], writes=[b_sq[k % 2]])
                        S.op("pe", lambda h, k=k: h.matmul(bank[0][:], lhsT=ones32[:], rhs=sq[k % 2][:], start=(k == 0), stop=(k == 7)),
                             reads=[b_sq[k % 2], b_const], writes=[pb[0]])
                    S.op("act", lambda h: h.activation(out=ssb[:], in_=bank[0][:], func=AF.Ln, bias=1024.0 * EPS),
                         writes=[pb[0], b_ss])
                    S.op("act", lambda h: h.activation(out=rstd[:], in_=ssb[:], func=AF.Exp, scale=-0.5), reads=[b_ss], writes=[b_rstd])
                    for k in range(8):
                        S.op("dve", lambda h, k=k: h.scalar_tensor_tensor(
                            out=sq[k % 2][:], in0=xT[:, k, cols], scalar=scale2[:, l, k:k + 1], in1=rstd[:], op0=ALU.mult, op1=ALU.mult),
                            reads=[b_xT[g], b_rstd, b_mod], writes=[b_sq[k % 2]])
                        S.op("act", lambda h, k=k: h.activation(out=h32k[k % 2][:], in_=sq[k % 2][:], func=AF.Identity, bias=modT[:, l, 24 + k:25 + k]),
                             reads=[b_sq[k % 2], b_mod], writes=[b_h32k[k % 2]])
                        S.op("pe", lambda h, k=k: h.matmul(psLT, lhsT=wr[:, k, :], rhs=h32k[k % 2][:], start=(k == 0), stop=(k == 7)),
                             reads=[b_h32k[k % 2], cb["wr"]], writes=[pb[6]])
                        S.op("act", lambda h, k=k: h.activation(out=h2T[:, k, cols], in_=sq[k % 2][:], func=AF.Identity, bias=modT[:, l, 24 + k:25 + k]),
                             reads=[b_sq[k % 2], b_mod], writes=[b_h2[g]])

                def router_mm(g):
                    rt = rts[g % 2]; b_rt = b_rts[g % 2]
                    S.op("act", lambda h: h.activation(out=lgT[:], in_=psLT, func=AF.Copy), writes=[pb[6], b_lgT])

                    def ltr(h):
                        ins = None
                        for tt in range(4):
                            ins = h.transpose(psL[:, tt, :], lgT[:, tt * 128:(tt + 1) * 128], ident[0:20, 0:20])
                        return ins
                    S.op("pe", ltr, reads=[b_lgT, cb["ident"]], writes=[pb[7]])
                    S.op("dve", lambda h: h.tensor_tensor(out=rt[:, :, 0:20], in0=psL, in1=brs[:, :, :], op=ALU.add),
                         reads=[cb["br"]], writes=[pb[7], b_rt])

                def bc(ap):
                    return ap.to_broadcast([128, 4, 4])

                def routing(g):
                    rt = rts[g % 2]; b_rt = b_rts[g % 2]
                    cols = slice(g * 512, (g + 1) * 512)
                    D_ = lambda fn: S.op("dve", fn, writes=[b_rt])
                    D_(lambda h: h.tensor_reduce(out=rt[:, :, 20:21], in_=rt[:, :, 0:4], axis=AX.X, op=ALU.max))
                    D_(lambda h: h.tensor_tensor(out=rt[:, :, 24:28], in0=rt[:, :, 0:4], in1=bc(rt[:, :, 20:21]), op=ALU.is_ge))
                    D_(lambda h: h.tensor_tensor(out=rt[:, :, 28:32], in0=rt[:, :, 0:4], in1=bc(rt[:, :, 20:21]), op=ALU.subtract))
                    S.op("act", lambda h: h.activation(out=rt[:, :, 28:32], in_=rt[:, :, 28:32], func=AF.Exp), writes=[b_rt])
                    D_(lambda h: h.tensor_reduce(out=rt[:, :, 21:22], in_=rt[:, :, 28:32], axis=AX.X, op=ALU.add))
                    D_(lambda h: h.reciprocal(out=rt[:, :, 22:23], in_=rt[:, :, 21:22]))
                    D_(lambda h: h.tensor_tensor(out=rt[:, :, 32:36], in0=rt[:, :, 4:8], in1=bc(rt[:, :, 24:25]), op=ALU.mult))
                    for gg in range(1, 4):
                        D_(lambda h, gg=gg: h.tensor_tensor(out=rt[:, :, 56:60], in0=rt[:, :, 4 + 4 * gg:8 + 4 * gg], in1=bc(rt[:, :, 24 + gg:25 + gg]), op=ALU.mult))
                        D_(lambda h: h.tensor_tensor(out=rt[:, :, 32:36], in0=rt[:, :, 32:36], in1=rt[:, :, 56:60], op=ALU.add))
                    D_(lambda h: h.tensor_reduce(out=rt[:, :, 36:37], in_=rt[:, :, 32:36], axis=AX.X, op=ALU.max))
                    D_(lambda h: h.tensor_tensor(out=rt[:, :, 40:44], in0=rt[:, :, 32:36], in1=bc(rt[:, :, 36:37]), op=ALU.is_ge))
                    D_(lambda h: h.scalar_tensor_tensor(out=rt[:, :, 44:48], in0=rt[:, :, 40:44], scalar=-1e30, in1=rt[:, :, 32:36],
                                                        op0=ALU.mult, op1=ALU.add))
                    D_(lambda h: h.tensor_reduce(out=rt[:, :, 37:38], in_=rt[:, :, 44:48], axis=AX.X, op=ALU.max))
                    D_(lambda h: h.tensor_tensor(out=rt[:, :, 48:52], in0=rt[:, :, 44:48], in1=bc(rt[:, :, 37:38]), op=ALU.is_ge))
                    D_(lambda h: h.tensor_tensor(out=rt[:, :, 38:39], in0=rt[:, :, 37:38], in1=rt[:, :, 36:37], op=ALU.subtract))
                    S.op("act", lambda h: h.activation(out=rt[:, :, 38:39], in_=rt[:, :, 38:39], func=AF.Exp), writes=[b_rt])
                    D_(lambda h: h.tensor_scalar(out=rt[:, :, 38:39], in0=rt[:, :, 38:39], scalar1=1.0, scalar2=None, op0=ALU.add))
                    D_(lambda h: h.reciprocal(out=rt[:, :, 39:40], in_=rt[:, :, 38:39]))
                    D_(lambda h: h.tensor_tensor(out=rt[:, :, 39:40], in0=rt[:, :, 39:40], in1=rt[:, :, 22:23], op=ALU.mult))
                    D_(lambda h: h.tensor_tensor(out=rt[:, :, 23:24], in0=rt[:, :, 22:23], in1=rt[:, :, 39:40], op=ALU.subtract))
                    D_(lambda h: h.tensor_tensor(out=rt[:, :, 52:56], in0=rt[:, :, 40:44], in1=bc(rt[:, :, 39:40]), op=ALU.mult))
                    D_(lambda h: h.tensor_tensor(out=rt[:, :, 56:60], in0=rt[:, :, 48:52], in1=bc(rt[:, :, 23:24]), op=ALU.mult))
                    D_(lambda h: h.tensor_tensor(out=rt[:, :, 52:56], in0=rt[:, :, 52:56], in1=rt[:, :, 56:60], op=ALU.add))
                    for gg in range(4):
                        S.op("dve", lambda h, gg=gg: h.tensor_tensor(out=gfull[:, :, gg * 4:(gg + 1) * 4], in0=rt[:, :, 52:56],
                                                                   in1=bc(rt[:, :, 24 + gg:25 + gg]), op=ALU.mult),
                             reads=[b_rt], writes=[b_gf])

                    def gtr(h):
                        ins = None
                        for tt in range(4):
                            ins = h.transpose(bank[5][0:16, tt * 128:(tt + 1) * 128], gfull[:, tt, :], ident[:])
                        return ins
                    S.op("pe", gtr, reads=[b_gf, cb["ident"]], writes=[pb[5]])
                    S.op("act", lambda h: h.activation(out=gT[:, cols], in_=bank[5][0:16, :], func=AF.Copy), writes=[pb[5], b_gT[g]])
                    if l == 0 and g == 0:
                        dump("gfull", gfull[:], b_gf, [128, 4, 16])

                norm_part(0)
                router_mm(0)
                for g in range(1, 4):
                    norm_part(g)
                    router_mm(g)
                    routing(g - 1)
                routing(3)
                if l == 0:
                    dump("h2T", h2T[:], b_h2[3], [128, 8, S_LEN], BF16)
                S.barrier()
                mo1.__exit__(None, None, None)
                wgt = [sbt(mo, "wgt%d" % i, [128, 8, 512], BF16) for i in range(2)]; b_wgt = [Buf("wgt%d" % i) for i in range(2)]
                wup = [sbt(mo, "wup%d" % i, [128, 8, 512], BF16) for i in range(2)]; b_wup = [Buf("wup%d" % i) for i in range(2)]
                wdn = [sbt(mo, "wdn%d" % i, [128, 4, D], BF16) for i in range(2)]; b_wdn = [Buf("wdn%d" % i) for i in range(2)]
                he = [sbt(mo, "he%d" % i, [128, 4, 512], BF16) for i in range(2)]; b_he = [Buf("he%d" % i) for i in range(2)]
                Gsb = sbt(mo, "Gsb", [128, 512]); b_G = Buf("Gsb")
                Ee = [sbt(mo, "Ee%d" % i, [16, 128]) for i in range(2)]; b_Ee = [Buf("Ee%d" % i) for i in range(2)]
                tht = sbt(mo, "tht", [128, 512]); usb = sbt(mo, "usb", [128, 512])
                b_tht = Buf("tht"); b_usb = Buf("usb")
                GB = (0, 7); UB = (1, 6); DB = (2, 3, 4)

                def stage_GUDN(e, g, prev):
                    ei = e % 2
                    hi = (e * 4 + g) % 2
                    cols = slice(g * 512, (g + 1) * 512)
                    S.op("pe", lambda h: h.matmul(bank[5][:], lhsT=Ee[ei][:], rhs=gT[:, cols], start=True, stop=True),
                         reads=[b_Ee[ei], b_gT[g]], writes=[pb[5]])
                    S.op("act", lambda h: h.activation(out=Gsb[:], in_=bank[5][:], func=AF.Copy, scale=0.5), writes=[pb[5], b_G])
                    for fb in range(4):
                        gb_ = GB[fb % 2]; ub_ = UB[fb % 2]

                        def gm(h):
                            ins = None
                            for k in range(8):
                                ins = h.matmul(bank[gb_][:], lhsT=wgt[ei][:, k, fb * 128:(fb + 1) * 128], rhs=h2T[:, k, cols], start=(k == 0), stop=(k == 7))
                            return ins
                        S.op("pe", gm, reads=[b_wgt[ei], b_h2[g]], writes=[pb[gb_]])

                        def um(h):
                            ins = None
                            for k in range(8):
                                ins = h.matmul(bank[ub_][:], lhsT=wup[ei][:, k, fb * 128:(fb + 1) * 128], rhs=h2T[:, k, cols], start=(k == 0), stop=(k == 7))
                            return ins
                        S.op("pe", um, reads=[b_wup[ei], b_h2[g]], writes=[pb[ub_]])
                        S.op("act", lambda h: h.activation(out=tht[:], in_=bank[gb_][:], func=AF.Tanh, scale=0.5), writes=[pb[gb_], b_tht])
                        S.op("act", lambda h: h.activation(out=usb[:], in_=bank[ub_][:], func=AF.Copy), writes=[pb[ub_], b_usb])
                        S.op("dve", lambda h: h.tensor_tensor(out=usb[:], in0=bank[gb_][:], in1=usb[:], op=ALU.mult), writes=[pb[gb_], b_usb])
                        S.op("dve", lambda h: h.scalar_tensor_tensor(out=tht[:], in0=tht[:], scalar=1.0, in1=Gsb[:], op0=ALU.add, op1=ALU.mult),
                             reads=[b_G], writes=[b_tht])
                        S.op("pool", lambda h: h.tensor_tensor(out=he[hi][:, fb, :], in0=usb[:], in1=tht[:], op=ALU.mult),
                             reads=[b_usb, b_tht], writes=[b_he[hi]])
                        if prev is not None:
                            stage_DN(prev[0], prev[1], (2 * fb, 2 * fb + 1))

                def stage_DN(e, g, js=tuple(range(8))):
                    ei = e % 2
                    hi = (e * 4 + g) % 2
                    cols = slice(g * 512, (g + 1) * 512)
                    for j in js:
                        bi = DB[j % 3]

                        def dm(h):
                            ins = None
                            for fb in range(4):
                                ins = h.matmul(bank[bi][:], lhsT=wdn[ei][:, fb, j * 128:(j + 1) * 128], rhs=he[hi][:, fb, :], start=(fb == 0), stop=(fb == 3))
                            return ins
                        S.op("pe", dm, reads=[b_wdn[ei], b_he[hi]], writes=[pb[bi]])
                        S.op("dve", lambda h: h.scalar_tensor_tensor(
                            out=xT[:, j, cols], in0=bank[bi][:], scalar=modT[:, l, 40 + j:41 + j], in1=xT[:, j, cols], op0=ALU.mult, op1=ALU.add),
                            reads=[b_mod], writes=[pb[bi], b_xT[g]])

                def load_e(e):
                    ei = e % 2
                    S.op("pool", lambda h: h.tensor_scalar(out=Ee[ei][:], in0=ones32[0:16, :], scalar1=ident[0:16, e:e + 1], scalar2=None,
                                                           op0=ALU.mult), reads=[b_const, cb["ident"]], writes=[b_Ee[ei]])
                    S.dma("pool", lambda h: h.dma_start(out=wgt[ei][:], in_=W_GATE[l, e].rearrange("(k p) f -> p k f", p=128)), writes=[b_wgt[ei]])
                    S.dma("pool", lambda h: h.dma_start(out=wup[ei][:], in_=W_UP[l, e].rearrange("(k p) f -> p k f", p=128)), writes=[b_wup[ei]])
                    S.dma("pool", lambda h: h.dma_start(out=wdn[ei][:], in_=W_DOWN[l, e].rearrange("(k p) n -> p k n", p=128)), writes=[b_wdn[ei]])

                seq = [(e, g) for e in range(16) for g in range(4)]
                load_e(0)
                load_e(1)
                stage_GUDN(0, 0, None)
                for i in range(1, 64):
                    e, g = seq[i]
                    pe_, pg_ = seq[i - 1]
                    stage_GUDN(e, g, (pe_, pg_))
                    if pg_ == 3 and pe_ + 2 < 16:
                        load_e(pe_ + 2)
                stage_DN(15, 3)
                S.barrier()
        return finish(nc, S, st, final_evs, xT, b_xT, bank, pb, ident, FING, cb, mhalf, OUT, sbt)


def finish(nc, S, st, final_evs, xT, b_xT, bank, pb, ident, FING, cb, mhalf, OUT, sbt):
    with ExitStack() as fs:
        fing = sbt(fs, "fing_s", [128, D]); cb["fing"] = Buf("c_fing")
        S.dma("sp", lambda h: h.dma_start(out=fing[:], in_=FING), writes=[cb["fing"]])
        ob = [sbt(fs, "ob%d" % i, [128, D]) for i in range(2)]
        b_ob = [Buf("ob%d" % i) for i in range(2)]
        fj = sbt(fs, "fjunk", [128, 512]); ft = sbt(fs, "ftiny", [128, 8]); b_fj = Buf("fj"); b_ft = Buf("ft")
        b_out = Buf("outd")
        b_mh = Buf("mh2")
        fts = [ft, sbt(fs, "ftiny2", [128, 8])]; b_fts = [b_ft, Buf("ft2")]
        fjs = [fj, sbt(fs, "fjunk2", [128, 512])]; b_fjs = [b_fj, Buf("fj2")]
        for t in range(16):
            tcs = slice(t * 128, (t + 1) * 128)
            p_ = t % 2
            ft_ = fts[p_]; b_ft_ = b_fts[p_]
            for hb in range(2):
                bk = hb + 2 * p_

                def tr(h, hb=hb, tcs=tcs, bk=bk):
                    ins = None
                    for kk in range(4):
                        ins = h.transpose(bank[bk][:, kk * 128:(kk + 1) * 128], xT[:, hb * 4 + kk, tcs], ident[:])
                    return ins
                S.op("pe", tr, reads=[b_xT[t // 4], cb["ident"]], writes=[pb[bk]])
                S.op("act", lambda h, hb=hb, bk=bk: h.activation(out=fjs[hb][:], in_=bank[bk][:], func=AF.Square, accum_out=ft_[:, hb:hb + 1]),
                     writes=[pb[bk], b_fjs[hb], b_ft_])
            S.op("dve", lambda h: h.tensor_tensor(out=ft_[:, 2:3], in0=ft_[:, 0:1], in1=ft_[:, 1:2], op=ALU.add), writes=[b_ft_])
            S.op("dve", lambda h: h.tensor_scalar(out=ft_[:, 3:4], in0=ft_[:, 2:3], scalar1=1.0 / 1024.0, scalar2=EPS, op0=ALU.mult, op1=ALU.add), writes=[b_ft_])
            S.op("pool", lambda h: h.tensor_tensor(out=ft_[:, 4:5], in0=ft_[:, 3:4], in1=mhalf[:, 0:1], op=ALU.pow), writes=[b_ft_])
            for hb in range(2):
                bk = hb + 2 * p_
                S.op("dve", lambda h, hb=hb, t=t, bk=bk: h.scalar_tensor_tensor(
                    out=ob[t % 2][:, hb * 512:(hb + 1) * 512], in0=bank[bk][:], scalar=ft_[:, 4:5], in1=fing[:, hb * 512:(hb + 1) * 512],
                    op0=ALU.mult, op1=ALU.mult), reads=[b_ft_, cb["fing"]], writes=[pb[bk], b_ob[t % 2]])
            S.dma("sp", lambda h, t=t, tcs=tcs: h.dma_start(out=OUT[tcs, :], in_=ob[t % 2][:]), reads=[b_ob[t % 2]], writes=[b_out])
        S.barrier(engs=["sp"])
        S.emit()
    return nc


_CACHE = {}


def _consts():
    f = np.float32
    idx = np.arange(128)
    c = {}
    c["c_ident"] = np.eye(128, dtype=f)
    c["c_tri"] = (idx[:, None] <= idx[None, :]).astype(f)
    c["c_maskm"] = (c["c_tri"] * KAPPA_M).astype(f)
    retd = np.zeros((128, 4, 128), f)
    xi = np.zeros((128, 4, 128), f)
    zeta = np.zeros((128, 4), f)
    for h in range(4):
        lg = math.log(1.0 - 2.0 ** (-5.0 - h))
        rel = idx[None, :] - idx[:, None]
        retd[:, h, :] = np.where(rel >= 0, np.exp(lg * np.maximum(rel, 0)), 0.0) * KAPPA_R
        xi[:, h, :] = np.exp(lg * (idx + 1.0))[None, :]
        zeta[:, h] = np.exp(lg * (127.0 - idx))
    c["c_retd"] = retd.reshape(128, 512)
    c["c_xi"] = xi.reshape(128, 512)
    c["c_zeta"] = zeta
    inv = (10000.0 ** (-np.arange(0, 128, 2, dtype=np.float32) / 128.0)).astype(f)
    c["c_invf"] = np.concatenate([inv, inv]).reshape(128, 1).astype(f)
    sel = np.zeros((16, 16, 128), f)
    for e in range(16):
        sel[e, e, :] = 1.0
    c["c_sel"] = sel.reshape(16, 16 * 128)
    return c


def _prep(inp):
    f = np.float32
    A = lambda a: np.ascontiguousarray(a, dtype=f)
    sh = {}
    sh["w_ada"] = A(inp["w_ada"])
    sh["b_adaT"] = A(inp["b_ada"].reshape(2, 48, 128).transpose(2, 0, 1).reshape(128, 96))
    sh["n1gT"] = A(inp["norm1_g"].reshape(2, 8, 128).transpose(2, 0, 1).reshape(128, 16))
    sh["n2gT"] = A(inp["norm2_g"].reshape(2, 8, 128).transpose(2, 0, 1).reshape(128, 16))
    sh["w_in"] = A(inp["w_in"])
    sh["convwT"] = A(inp["conv_w"].reshape(2, 4, 8, 128).transpose(3, 0, 1, 2).reshape(128, 64))
    sh["convbT"] = A(inp["conv_b"].reshape(2, 8, 128).transpose(2, 0, 1).reshape(128, 16))
    gb = np.concatenate([inp["m_ig_b"], inp["m_fg_b"]], axis=1)
    sh["gbias"] = A(np.broadcast_to(gb[None, :, None, :], (128, 2, 8, 8)).reshape(128, 128))
    ng = np.stack([inp["m_norm_g"].reshape(2, 8, 128), inp["r_norm_g"].reshape(2, 8, 128)], axis=0)
    sh["ngT"] = A(ng.transpose(3, 0, 1, 2).reshape(128, 32))
    sh["fing"] = A(np.broadcast_to(inp["final_g"].reshape(1, 1024), (128, 1024)))
    sh["w_bm"] = A(inp["w_bm"]); sh["w_br"] = A(inp["w_br"]); sh["w_out"] = A(inp["w_out"])
    wr = np.concatenate([inp["w_r1"], inp["w_r2"]], axis=2)
    sh["wr"] = A(wr.reshape(2, 8, 128, 20).transpose(2, 0, 1, 3).reshape(128, 320))
    br = np.concatenate([inp["b_r1"], inp["b_r2"]], axis=1)
    sh["br"] = A(np.broadcast_to(br[None, :, None, :], (128, 2, 4, 20)).reshape(128, 160))
    sh["w_gate"] = A(inp["w_gate"]); sh["w_up"] = A(inp["w_up"]); sh["w_down"] = A(inp["w_down"])
    sh.update(_consts())
    maps = []
    for b in range(NCORES):
        m = dict(sh)
        m["x"] = A(inp["x"][b])
        m["cT"] = A(inp["c"][b].reshape(8, 128).T)
        m["pos"] = np.ascontiguousarray(np.broadcast_to(inp["positions"][b].astype(np.int32)[None, :], (128, S_LEN)))
        maps.append(m)
    return maps


def kernel(**inp):
    if "nc" not in _CACHE:
        _CACHE["nc"] = build()
    nc = _CACHE["nc"]
    maps = _prep(inp)
    res = run_bass_kernel_spmd(nc, maps, core_ids=list(range(NCORES)))
    return np.stack([np.asarray(r["out"], dtype=np.float32) for r in res.results], axis=0)
```

```python
import math
import types
import numpy as np
from contextlib import ExitStack
import concourse.bass as bass
import concourse.mybir as mybir
from concourse.bass_utils import run_bass_kernel_spmd

F32 = mybir.dt.float32
BF16 = mybir.dt.bfloat16
I32 = mybir.dt.int32
AF = mybir.ActivationFunctionType
ALU = mybir.AluOpType
AX = mybir.AxisListType

S_LEN = 2048
D = 1024
NCORES = 8
EPS = 1e-6
KAPPA_M = 0.25 * 128 ** -0.5
KAPPA_R = 128 ** -0.5
OFF_MQ, OFF_MK, OFF_MV, OFF_MO, OFF_MI = 0, 512, 1024, 2048, 3072
OFF_RQ, OFF_RK, OFF_RV, OFF_RG, OFF_GA, OFF_GB = 3080, 3592, 4104, 5128, 6152, 7176


def freeze(fn):
    cells = fn.__closure__
    if not cells:
        return fn
    new = []
    for c in cells:
        try:
            new.append(types.CellType(c.cell_contents))
        except ValueError:
            new.append(c)
    return types.FunctionType(fn.__code__, fn.__globals__, fn.__name__, fn.__defaults__, tuple(new))


class Buf:
    __slots__ = ("name", "w", "r", "dsem", "dcnt")

    def __init__(self, name):
        self.name = name
        self.w = None
        self.r = []
        self.dsem = None
        self.dcnt = 0


class Sched:
    ENG = ("pe", "act", "dve", "pool", "sp")

    def __init__(self, nc, stack):
        self.nc = nc
        self.stack = stack
        self.h = {"pe": nc.tensor, "act": nc.scalar, "dve": nc.vector, "pool": nc.gpsimd, "sp": nc.sync}
        self.sem = {k: stack.enter_context(nc.semaphore("s_" + k)) for k in self.ENG}
        self.cnt = {k: 0 for k in self.ENG}
        self.seen = {k: {} for k in self.ENG}
        self.prog = {k: [] for k in self.ENG}
        self.dbufs = []

    def _deps(self, eng, reads, writes):
        need = {}

        def add(ev):
            if ev is None:
                return
            s, v = ev
            if need.get(s, 0) < v:
                need[s] = v
        for b in reads:
            add(b.w)
        for b in writes:
            add(b.w)
            for ev in b.r:
                add(ev)
        out = []
        seen = self.seen[eng]
        own = self.sem[eng]
        for s, v in need.items():
            if eng == "pe" and s is own:
                continue
            if seen.get(s, 0) < v:
                seen[s] = v
                out.append((s, v))
        return out

    def _record(self, ev, reads, writes):
        for b in reads:
            b.r.append(ev)
        for b in writes:
            b.w = ev
            b.r = []

    def op(self, eng, fn, reads=(), writes=()):
        fn = freeze(fn)
        waits = self._deps(eng, reads, writes)
        self.cnt[eng] += 1
        ev = (self.sem[eng], self.cnt[eng])
        h = self.h[eng]
        sem = self.sem[eng]

        def thunk():
            for s, v in waits:
                h.wait_ge(s, v)
            fn(h).then_inc(sem, 1)
        self.prog[eng].append(thunk)
        self._record(ev, reads, writes)
        return ev

    def dma(self, eng, fn, reads=(), writes=(), track=None):
        fn = freeze(fn)
        waits = self._deps(eng, reads, writes)
        tb = track if track is not None else writes[0]
        if tb.dsem is None:
            tb.dsem = self.stack.enter_context(self.nc.semaphore("d_%d" % len(self.dbufs)))
            self.dbufs.append(tb)
        tb.dcnt += 16
        ev = (tb.dsem, tb.dcnt)
        h = self.h[eng]
        dsem = tb.dsem

        def thunk():
            for s, v in waits:
                h.wait_ge(s, v)
            fn(h).then_inc(dsem, 16)
        self.prog[eng].append(thunk)
        self._record(ev, reads, writes)
        return ev

    def barrier(self, engs=None):
        evs = [(self.sem[k], self.cnt[k]) for k in self.ENG if self.cnt[k] > 0]
        evs += [(b.dsem, b.dcnt) for b in self.dbufs]
        for eng in (engs or self.ENG):
            seen = self.seen[eng]
            waits = []
            for s, v in evs:
                if seen.get(s, 0) < v:
                    seen[s] = v
                    waits.append((s, v))
            h = self.h[eng]

            def thunk(h=h, waits=waits):
                for s, v in waits:
                    h.wait_ge(s, v)
            self.prog[eng].append(thunk)

    def emit(self):
        nc = self.nc
        with nc.Block() as block:
            @block.tensor
            def _(e):
                for f in self.prog["pe"]:
                    f()

            @block.scalar
            def _(e):
                for f in self.prog["act"]:
                    f()

            @block.vector
            def _(e):
                for f in self.prog["dve"]:
                    f()

            @block.gpsimd
            def _(e):
                for f in self.prog["pool"]:
                    f()

            @block.sync
            def _(e):
                for f in self.prog["sp"]:
                    f()


def build(stop_after=None, dumps=()):
    nc = bass.Bass("TRN2", target_bir_lowering=False)
    dt_in = lambda name, shape, dt=F32: nc.dram_tensor(name, list(shape), dt, kind="ExternalInput").ap()
    X = dt_in("x", [S_LEN, D])
    CT = dt_in("cT", [128, 8])
    POS = dt_in("pos", [128, S_LEN], I32)
    W_ADA = dt_in("w_ada", [2, D, 6 * D])
    B_ADAT = dt_in("b_adaT", [128, 96])
    N1G = dt_in("n1gT", [128, 16])
    N2G = dt_in("n2gT", [128, 16])
    W_IN = dt_in("w_in", [2, D, 8200])
    CONVW = dt_in("convwT", [128, 64])
    CONVB = dt_in("convbT", [128, 16])
    GBIAS = dt_in("gbias", [128, 2 * 8 * 8])
    NGT = dt_in("ngT", [128, 32])
    FING = dt_in("fing", [128, D])
    W_BM = dt_in("w_bm", [2, D, D])
    W_BR = dt_in("w_br", [2, D, D])
    W_OUT = dt_in("w_out", [2, D, D])
    WR = dt_in("wr", [128, 2 * 8 * 20])
    BR = dt_in("br", [128, 2 * 4 * 20])
    W_GATE = dt_in("w_gate", [2, 16, D, 512])
    W_UP = dt_in("w_up", [2, 16, D, 512])
    W_DOWN = dt_in("w_down", [2, 16, 512, D])
    C_ID = dt_in("c_ident", [128, 128])
    C_TRI = dt_in("c_tri", [128, 128])
    C_MASKM = dt_in("c_maskm", [128, 128])
    C_RETD = dt_in("c_retd", [128, 512])
    C_XI = dt_in("c_xi", [128, 512])
    C_ZETA = dt_in("c_zeta", [128, 4])
    C_INVF = dt_in("c_invf", [128, 1])
    C_SEL = dt_in("c_sel", [16, 16 * 128])
    OUT = nc.dram_tensor("out", [S_LEN, D], F32, kind="ExternalOutput").ap()
    dump_out = {}

    with ExitStack() as st:
        S = Sched(nc, st)
        _uid = [0]

        def sbt(stack, name, shape, dt=F32):
            _uid[0] += 1
            return stack.enter_context(nc.sbuf_tensor("%s_u%d" % (name, _uid[0]), list(shape), dt))
        bank = [st.enter_context(nc.psum_tensor("bank%d" % i, [128, 512], F32)) for i in range(8)]
        pb = [Buf("pb%d" % i) for i in range(8)]
        final_evs = []

        def dump(name, ap, buf, shape, dt=F32):
            if name not in dumps:
                return
            t = nc.dram_tensor("dbg_" + name, list(shape), dt, kind="ExternalOutput").ap()
            ob = Buf("dbg_" + name)
            S.dma("sp", lambda h: h.dma_start(out=t, in_=ap), reads=[buf], writes=[ob])
            final_evs.append(ob)
            dump_out[name] = True

        xT = sbt(st, "xT", [128, 8, S_LEN]); b_xT = [Buf("xT%d" % g) for g in range(4)]
        ident = sbt(st, "ident", [128, 128]); identb = sbt(st, "identb", [128, 128], BF16)
        tri = sbt(st, "tri", [128, 128]); ones32 = sbt(st, "ones32", [128, 128])
        maskm = sbt(st, "maskm", [128, 128]); retd = sbt(st, "retd", [128, 4, 128]); xi = sbt(st, "xi", [128, 4, 128])
        zeta = sbt(st, "zeta", [128, 4]); mhalf = sbt(st, "mhalf", [128, 1])
        modT = sbt(st, "modT", [128, 2, 48]); b_adaT = sbt(st, "b_adaT_s", [128, 96])
        n1g = sbt(st, "n1g", [128, 16]); n2g = sbt(st, "n2g", [128, 16])
        scale1 = sbt(st, "scale1", [128, 2, 8]); scale2 = sbt(st, "scale2", [128, 2, 8]); g1h = sbt(st, "g1h", [128, 2, 8])
        convw = sbt(st, "convw", [128, 2, 4, 8]); convb = sbt(st, "convb", [128, 2, 8])
        gbias = sbt(st, "gbias_s", [128, 2, 8, 8])
        ngT = sbt(st, "ngT_s", [128, 2, 2, 8])
        cosT = sbt(st, "cosT", [128, S_LEN]); sinS = sbt(st, "sinS", [128, S_LEN])
        C32 = [sbt(st, "C32_%d" % i, [128, 260]) for i in range(4)]
        Cb = [sbt(st, "Cb_%d" % i, [128, 260], BF16) for i in range(4)]
        R32 = [sbt(st, "R32_%d" % i, [128, 256]) for i in range(4)]
        Rb = [sbt(st, "Rb_%d" % i, [128, 256], BF16) for i in range(4)]
        halo = sbt(st, "halo", [128, 8, 4])
        b_const = Buf("const"); b_mod = Buf("mod"); b_cs = Buf("cossin")
        b_C = [Buf("C%d" % i) for i in range(4)]; b_Cb = [Buf("Cb%d" % i) for i in range(4)]
        b_R = [Buf("R%d" % i) for i in range(4)]; b_Rb = [Buf("Rb%d" % i) for i in range(4)]
        b_halo = [Buf("halo%d" % i) for i in range(8)]

        def load_const(dst, src, buf):
            S.dma("sp", lambda h: h.dma_start(out=dst, in_=src), writes=[buf])

        cb = {}
        for nm, dst, src in [("ident", ident[:], C_ID), ("tri", tri[:], C_TRI), ("maskm", maskm[:], C_MASKM),
                             ("retd", retd[:], C_RETD.rearrange("p (h l) -> p h l", h=4)),
                             ("xi", xi[:], C_XI.rearrange("p (h l) -> p h l", h=4)), ("zeta", zeta[:], C_ZETA),
                             ("b_adaT", b_adaT[:], B_ADAT), ("n1g", n1g[:], N1G), ("n2g", n2g[:], N2G),
                             ("convw", convw[:], CONVW.rearrange("p (l j k) -> p l j k", l=2, j=4)),
                             ("convb", convb[:], CONVB.rearrange("p (l k) -> p l k", l=2)),
                             ("gbias", gbias[:], GBIAS.rearrange("p (l c g) -> p l c g", l=2, c=8)),
                             ("ngT", ngT[:], NGT.rearrange("p (b l k) -> p b l k", b=2, l=2)),
                             ]:
            cb[nm] = Buf("c_" + nm)
            load_const(dst, src, cb[nm])
        S.op("dve", lambda h: h.tensor_copy(out=identb[:], in_=ident[:]), reads=[cb["ident"]], writes=[b_const])
        S.op("dve", lambda h: h.memset(ones32[:], 1.0), writes=[b_const])
        S.op("dve", lambda h: h.memset(mhalf[:], -0.5), writes=[b_const])
        ALLC = list(cb.values()) + [b_const]

        with ExitStack() as p0:
            xs = [sbt(p0, "xs%d" % i, [128, D]) for i in range(2)]
            b_xs = [Buf("xs%d" % i) for i in range(2)]
            for t in range(16):
                S.dma("sp", lambda h, t=t: h.dma_start(out=xs[t % 2][:], in_=X[t * 128:(t + 1) * 128, :]), writes=[b_xs[t % 2]])
                for hb in range(2):
                    def tr(h, t=t, hb=hb):
                        ins = None
                        for kk in range(4):
                            k = hb * 4 + kk
                            ins = h.transpose(bank[hb][:, kk * 128:(kk + 1) * 128], xs[t % 2][:, k * 128:(k + 1) * 128], ident[:])
                        return ins
                    S.op("pe", tr, reads=[b_xs[t % 2], cb["ident"]], writes=[pb[hb]])
                    eng = "act" if hb == 0 else "dve"
                    if eng == "act":
                        S.op("act", lambda h, t=t, hb=hb: h.activation(
                            out=xT[:, hb * 4:(hb + 1) * 4, t * 128:(t + 1) * 128],
                            in_=bank[hb][:].rearrange("p (k t) -> p k t", k=4), func=AF.Copy),
                            reads=[], writes=[pb[hb], b_xT[t // 4]])
                    else:
                        S.op("dve", lambda h, t=t, hb=hb: h.tensor_copy(
                            out=xT[:, hb * 4:(hb + 1) * 4, t * 128:(t + 1) * 128],
                            in_=bank[hb][:].rearrange("p (k t) -> p k t", k=4)),
                            reads=[], writes=[pb[hb], b_xT[t // 4]])
            cT = sbt(p0, "cT_s", [128, 8]); cth = sbt(p0, "cth", [128, 8]); csil = sbt(p0, "csil", [128, 8])
            b_c = Buf("c")
            S.dma("sp", lambda h: h.dma_start(out=cT[:], in_=CT), writes=[b_c])
            S.op("act", lambda h: h.activation(out=cth[:], in_=cT[:], func=AF.Tanh, scale=0.5), reads=[b_c], writes=[b_c])
            S.op("dve", lambda h: h.scalar_tensor_tensor(out=csil[:], in0=cth[:], scalar=1.0, in1=cT[:], op0=ALU.add, op1=ALU.mult),
                 reads=[b_c], writes=[b_c])
            S.op("dve", lambda h: h.tensor_scalar(out=csil[:], in0=csil[:], scalar1=0.5, scalar2=None, op0=ALU.mult),
                 reads=[b_c], writes=[b_c])
            wa = [sbt(p0, "wa%d" % i, [128, 8, 512], BF16) for i in range(4)]
            csilb = sbt(p0, "csilb", [128, 8], BF16)
            S.op("dve", lambda h: h.tensor_copy(out=csilb[:], in_=csil[:]), reads=[b_c], writes=[b_c])
            b_wa = [Buf("wa%d" % i) for i in range(4)]
            modrow = sbt(p0, "modrow", [1, 6 * D]); b_mrow = Buf("modrow")
            for l in range(2):
                for jg in range(12):
                    i = (l * 12 + jg) % 4
                    S.dma("pool", lambda h, l=l, jg=jg, i=i: h.dma_start(
                        out=wa[i][:], in_=W_ADA[l, :, jg * 512:(jg + 1) * 512].rearrange("(k p) n -> p k n", p=128)),
                        writes=[b_wa[i]])

                    pbj = 6 + (jg % 2)

                    def mm(h, i=i, pbj=pbj):
                        ins = None
                        for k in range(8):
                            ins = h.matmul(bank[pbj][0:1, :], lhsT=csilb[:, k:k + 1], rhs=wa[i][:, k, :], start=(k == 0), stop=(k == 7))
                        return ins
                    S.op("pe", mm, reads=[b_wa[i], b_c], writes=[pb[pbj]])
                    S.op("act", lambda h, jg=jg, pbj=pbj: h.activation(out=modrow[0:1, jg * 512:(jg + 1) * 512], in_=bank[pbj][0:1, :], func=AF.Copy),
                         writes=[pb[pbj], b_mrow])

                def mtr(h):
                    ins = None
                    for j in range(48):
                        ins = h.matmul(bank[5][:, j:j + 1], lhsT=modrow[0:1, j * 128:(j + 1) * 128], rhs=ones32[0:1, 0:1], start=True, stop=True)
                    return ins
                S.op("pe", mtr, reads=[b_mrow, b_const], writes=[pb[5]])
                S.op("dve", lambda h, l=l: h.tensor_tensor(out=modT[:, l, :], in0=bank[5][:, 0:48], in1=b_adaT[:, l * 48:(l + 1) * 48],
                                                          op=ALU.add), reads=[cb["b_adaT"]], writes=[pb[5], b_mod])
                S.op("dve", lambda h, l=l: h.scalar_tensor_tensor(out=scale1[:, l, :], in0=modT[:, l, 8:16], scalar=1.0,
                                                                 in1=n1g[:, l * 8:(l + 1) * 8], op0=ALU.add, op1=ALU.mult),
                     reads=[cb["n1g"]], writes=[b_mod])
                S.op("dve", lambda h, l=l: h.tensor_scalar(out=scale1[:, l, :], in0=scale1[:, l, :], scalar1=32.0, scalar2=None, op0=ALU.mult),
                     writes=[b_mod])
                S.op("dve", lambda h, l=l: h.scalar_tensor_tensor(out=scale2[:, l, :], in0=modT[:, l, 32:40], scalar=1.0,
                                                                 in1=n2g[:, l * 8:(l + 1) * 8], op0=ALU.add, op1=ALU.mult),
                     reads=[cb["n2g"]], writes=[b_mod])
                S.op("dve", lambda h, l=l: h.tensor_scalar(out=scale2[:, l, :], in0=scale2[:, l, :], scalar1=32.0, scalar2=None, op0=ALU.mult),
                     writes=[b_mod])
                S.op("dve", lambda h, l=l: h.tensor_scalar(out=g1h[:, l, :], in0=modT[:, l, 16:24], scalar1=0.5, scalar2=None, op0=ALU.mult),
                     writes=[b_mod])
            posi = sbt(p0, "posi", [128, S_LEN], I32); ang = sbt(p0, "ang", [128, S_LEN]); rr = sbt(p0, "rr", [128, S_LEN])
            kk_i = sbt(p0, "kk_i", [128, S_LEN], I32); invf = sbt(p0, "invf", [128, 1])
            b_r = Buf("rot")
            S.dma("sp", lambda h: h.dma_start(out=posi[:], in_=POS), writes=[b_r])
            S.dma("sp", lambda h: h.dma_start(out=invf[:], in_=C_INVF), writes=[b_r], track=Buf("invf"))
            S.op("dve", lambda h: h.tensor_copy(out=ang[:], in_=posi[:]), reads=[b_r], writes=[b_r])
            S.op("dve", lambda h: h.tensor_scalar(out=ang[:], in0=ang[:], scalar1=invf[:, 0:1], scalar2=None, op0=ALU.mult), writes=[b_r])
            S.op("dve", lambda h: h.tensor_scalar(out=kk_i[:], in0=ang[:], scalar1=1.0 / (2 * math.pi), scalar2=None, op0=ALU.mult), writes=[b_r])
            S.op("dve", lambda h: h.tensor_copy(out=rr[:], in_=kk_i[:]), writes=[b_r])
            S.op("dve", lambda h: h.scalar_tensor_tensor(out=ang[:], in0=rr[:], scalar=-2 * math.pi, in1=ang[:], op0=ALU.mult, op1=ALU.add),
                 writes=[b_r])

            def wrap(src):
                S.op("dve", lambda h: h.tensor_scalar(out=rr[:], in0=src[:], scalar1=math.pi, scalar2=None, op0=ALU.is_gt), writes=[b_r])
                S.op("dve", lambda h: h.scalar_tensor_tensor(out=src[:], in0=rr[:], scalar=-2 * math.pi, in1=src[:], op0=ALU.mult, op1=ALU.add),
                     writes=[b_r])
                S.op("dve", lambda h: h.tensor_scalar(out=rr[:], in0=src[:], scalar1=-math.pi, scalar2=None, op0=ALU.is_lt), writes=[b_r])
                S.op("dve", lambda h: h.scalar_tensor_tensor(out=src[:], in0=rr[:], scalar=2 * math.pi, in1=src[:], op0=ALU.mult, op1=ALU.add),
                     writes=[b_r])
            wrap(ang)
            S.op("act", lambda h: h.activation(out=sinS[:], in_=ang[:], func=AF.Sin), reads=[b_r], writes=[b_cs])
            S.op("dve", lambda h: h.tensor_scalar(out=ang[:], in0=ang[:], scalar1=math.pi / 2, scalar2=None, op0=ALU.add), reads=[b_cs], writes=[b_r])
            wrap(ang)
            S.op("act", lambda h: h.activation(out=cosT[:], in_=ang[:], func=AF.Sin), reads=[b_r], writes=[b_cs])
            S.op("act", lambda h: h.mul(out=sinS[0:64, :], in_=sinS[0:64, :], mul=-1.0), writes=[b_cs])
            dump("modT", modT[:], b_mod, [128, 2, 48])
            dump("cosT", cosT[:], b_cs, [128, S_LEN])
            dump("sinS", sinS[:], b_cs, [128, S_LEN])
            S.barrier()
        if stop_after == "p0":
            return finish(nc, S, st, final_evs, xT, b_xT, bank, pb, ident, FING, cb, mhalf, OUT, sbt)

        for l in range(2):
            with ExitStack() as mx:
                hT = sbt(mx, "hT", [128, 8, 1024], BF16); b_hT = [Buf("hT%d" % i) for i in range(2)]
                ymT = sbt(mx, "ymT", [128, 8, 1024], BF16); b_ymT = [Buf("ymT%d" % i) for i in range(8)]
                yT = sbt(mx, "yT", [128, 8, 1024], BF16); b_yT = [Buf("yT%d" % i) for i in range(2)]
                wq = [sbt(mx, "wq%d" % i, [128, 8, 128], BF16) for i in range(2)]; b_wq = [Buf("wq%d" % i) for i in range(2)]
                wk = [sbt(mx, "wk%d" % i, [128, 8, 128], BF16) for i in range(2)]; b_wk = [Buf("wk%d" % i) for i in range(2)]
                wvo = [sbt(mx, "wvo%d" % i, [128, 8, 512], BF16) for i in range(2)]; b_wvo = [Buf("wvo%d" % i) for i in range(2)]
                wif = sbt(mx, "wif", [128, 8, 8], BF16); b_wif = Buf("wif")
                pad = [sbt(mx, "pad%d" % i, [128, 516]) for i in range(2)]; b_pad = [Buf("pad%d" % i) for i in range(2)]
                acc = sbt(mx, "acc", [128, 512]); th = sbt(mx, "th", [128, 512]); b_acc = Buf("acc"); b_th = Buf("th")
                sq = [acc, th]; b_sq = [b_acc, b_th]
                ssb = pad[0][:, 0:512]; rstd = pad[1][:, 0:512]; b_ss = b_pad[0]; b_rstd = b_pad[1]
                qTs = [sbt(mx, "qT%d" % i, [128, 1024], BF16) for i in range(2)]
                kTs = [sbt(mx, "kT%d" % i, [128, 1024], BF16) for i in range(2)]
                qxTs = [sbt(mx, "qxT%d" % i, [128, 1024], BF16) for i in range(2)]
                b_qs_ = [[Buf("q%d_%d" % (j, i)) for i in range(2)] for j in range(2)]
                b_ks_ = [[Buf("k%d_%d" % (j, i)) for i in range(2)] for j in range(2)]
                b_qxs_ = [[Buf("qx%d_%d" % (j, i)) for i in range(2)] for j in range(2)]
                vext = [sbt(mx, "vext%d" % i, [128, 260], BF16) for i in range(2)]; b_v = [Buf("v%d" % i) for i in range(2)]
                gsig = [sbt(mx, "gsig%d" % i, [128, 256], BF16) for i in range(3)]; b_gs = [Buf("gs%d" % i) for i in range(3)]
                tho = [sbt(mx, "tho%d" % i, [128, 256]) for i in range(2)]; b_tho = [Buf("tho%d" % i) for i in range(2)]
                PT = [sbt(mx, "PT%d" % i, [128, 128], BF16) for i in range(2)]; b_PT = [Buf("PT%d" % i) for i in range(2)]
                kw = [sbt(mx, "kw%d" % i, [128, 128], BF16) for i in range(2)]; b_kw = [Buf("kw%d" % i) for i in range(2)]
                ymc = [sbt(mx, "ymc%d" % i, [128, 256], BF16) for i in range(2)]; b_ymc = [Buf("ymc%d" % i) for i in range(2)]
                tinys = [sbt(mx, "tiny%d" % i, [128, 16]) for i in range(2)]; b_tinys = [Buf("tiny%d" % i) for i in range(2)]
                halo2 = sbt(mx, "halo2", [128, 2, 4]); b_halo2 = [Buf("halo2_%d" % i) for i in range(2)]
                pdb = [sbt(mx, "pdb%d" % i, [128, 516], BF16) for i in range(2)]; b_pdb = [Buf("pdb%d" % i) for i in range(2)]
                dg = sbt(mx, "dg", [128, 2, 4, 128], BF16); b_dg = [Buf("dg%d" % i) for i in range(2)]
                gpre = sbt(mx, "gpre", [128, 8, 8]); lfp = sbt(mx, "lfp", [128, 8, 4]); a_t = sbt(mx, "a_t", [128, 8, 4])
                w_t = sbt(mx, "w_t", [128, 8, 4]); el_t = sbt(mx, "el_t", [128, 8, 4]); dec_t = sbt(mx, "dec_t", [128, 8, 4])
                wdec_t = sbt(mx, "wdec_t", [128, 8, 4]); b_g = Buf("gates")
                wb = wq; b_wb = b_wq
                wg = wk; b_wg = b_wk
                for i in range(2):
                    S.op("pool", lambda h, i=i: h.memset(vext[i][:, 256:260], 1.0), writes=[b_v[i]])
                wcnt = [0]

                def load_w(dst, col0, ncols, buf, l=l):
                    S.dma("pool", lambda h: h.dma_start(out=dst, in_=W_IN[l, :, col0:col0 + ncols].rearrange("(k p) n -> p k n", p=128)),
                          writes=[buf])

                for hf in range(2):
                    T0 = hf * 1024
                    for t01 in range(2):
                        wi01 = (wcnt[0] + t01) % 2
                        load_w(wq[wi01][:], OFF_MQ + t01 * 128, 128, b_wq[wi01])
                        load_w(wk[wi01][:], OFF_MK + t01 * 128, 128, b_wk[wi01])
                    for tg in range(2):
                        g = hf * 2 + tg
                        cols = slice(g * 512, (g + 1) * 512)
                        lc = slice(tg * 512, (tg + 1) * 512)
                        for k in range(8):
                            S.op("act", lambda h, k=k, cols=cols: h.activation(out=sq[k % 2][:], in_=xT[:, k, cols], func=AF.Square),
                                 reads=[b_xT[g]], writes=[b_sq[k % 2]])
                            S.op("pe", lambda h, k=k: h.matmul(bank[0][:], lhsT=ones32[:], rhs=sq[k % 2][:], start=(k == 0), stop=(k == 7)),
                                 reads=[b_sq[k % 2], b_const], writes=[pb[0]])
                        S.op("act", lambda h: h.activation(out=ssb, in_=bank[0][:], func=AF.Ln, bias=1024.0 * EPS),
                             writes=[pb[0], b_ss])
                        S.op("act", lambda h: h.activation(out=rstd, in_=ssb, func=AF.Exp, scale=-0.5),
                             reads=[b_ss], writes=[b_rstd])
                        for k in range(8):
                            S.op("dve", lambda h, k=k, cols=cols: h.scalar_tensor_tensor(
                                out=sq[k % 2][:], in0=xT[:, k, cols], scalar=scale1[:, l, k:k + 1], in1=rstd, op0=ALU.mult, op1=ALU.mult),
                                reads=[b_xT[g], b_rstd, b_mod], writes=[b_sq[k % 2]])
                            S.op("act", lambda h, k=k, lc=lc: h.activation(out=hT[:, k, lc], in_=sq[k % 2][:], func=AF.Identity,
                                                                           bias=modT[:, l, k:k + 1]),
                                 reads=[b_sq[k % 2], b_mod], writes=[b_hT[tg]])
                    if l == 0 and hf == 0:
                        dump("hT", hT[:], b_hT[1], [128, 8, 1024], BF16)
                    load_w(wif[:], OFF_MI, 8, b_wif)
                    psG = bank[6][:, 0:64].rearrange("p (c g) -> p c g", c=8)
                    psNB = bank[6][:, 64:96].rearrange("p (c g) -> p c g", c=8)
                    psNT = bank[6][:, 96:128].rearrange("p (c g) -> p c g", c=8)

                    def gmm(h):
                        ins = None
                        for c in range(8):
                            for k in range(8):
                                ins = h.matmul(psG[:, c, :], lhsT=hT[:, k, c * 128:(c + 1) * 128], rhs=wif[:, k, :], start=(k == 0), stop=(k == 7))
                        return ins
                    S.op("pe", gmm, reads=[b_hT[0], b_hT[1], b_wif], writes=[pb[6]])
                    S.op("dve", lambda h: h.tensor_tensor(out=gpre[:], in0=psG, in1=gbias[:, l, :, :], op=ALU.add),
                         reads=[cb["gbias"]], writes=[pb[6], b_g])
                    S.op("act", lambda h: h.activation(out=lfp[:], in_=gpre[:, :, 4:8], func=AF.Exp, scale=-1.0), writes=[b_g])
                    S.op("act", lambda h: h.activation(out=lfp[:], in_=lfp[:], func=AF.Ln, bias=1.0), writes=[b_g])

                    def nbmm(h):
                        ins = None
                        for c in range(8):
                            h.matmul(psNB[:, c, :], lhsT=tri[:], rhs=lfp[:, c, :], start=True, stop=True)
                            ins = h.matmul(psNT[:, c, :], lhsT=ones32[:], rhs=lfp[:, c, :], start=True, stop=True)
                        return ins
                    S.op("pe", nbmm, reads=[b_g, cb["tri"], b_const], writes=[pb[6]])
                    S.op("dve", lambda h: h.tensor_tensor(out=a_t[:], in0=psNB, in1=gpre[:, :, 0:4], op=ALU.add), writes=[pb[6], b_g])
                    S.op("act", lambda h: h.activation(out=w_t[:], in_=a_t[:], func=AF.Exp), writes=[b_g])
                    S.op("act", lambda h: h.activation(out=el_t[:], in_=psNB, func=AF.Exp, scale=-1.0), writes=[pb[6], b_g])
                    S.op("act", lambda h: h.activation(out=dec_t[:], in_=psNT, func=AF.Exp, scale=-1.0), writes=[pb[6], b_g])
                    S.op("dve", lambda h: h.tensor_tensor(out=wdec_t[:], in0=w_t[:], in1=dec_t[:], op=ALU.mult), writes=[b_g])
                    if l == 0 and hf == 0:
                        dump("w_t", w_t[:], b_g, [128, 8, 4]); dump("el_t", el_t[:], b_g, [128, 8, 4]); dump("dec_t", dec_t[:], b_g, [128, 8, 4])

                    OFFS = {True: (OFF_MQ, OFF_MK, OFF_MV, OFF_MO), False: (OFF_RQ, OFF_RK, OFF_RV, OFF_RG)}
                    tasks = []
                    for is_m_ in (True, False):
                        for hd_ in range(4):
                            tasks.append((is_m_, hd_, wcnt[0] % 2))
                            wcnt[0] += 1

                    def loads_qk(task):
                        is_m, hd, wi = task
                        oq, ok, ov, oo = OFFS[is_m]
                        load_w(wq[wi][:], oq + hd * 128, 128, b_wq[wi])
                        load_w(wk[wi][:], ok + hd * 128, 128, b_wk[wi])

                    def loads_vo(task):
                        is_m, hd, wi = task
                        oq, ok, ov, oo = OFFS[is_m]
                        load_w(wvo[wi][:, :, 0:256], ov + hd * 256, 256, b_wvo[wi])
                        load_w(wvo[wi][:, :, 256:512], oo + hd * 256, 256, b_wvo[wi])

                    def prologue_piece(task, which, tg, part):
                        is_m, hd, wi = task
                        qT = qTs[wi]; kT = kTs[wi]; qxT = qxTs[wi]
                        b_q = b_qs_[wi]; b_k = b_ks_[wi]; b_qx = b_qxs_[wi]
                        wsel = (wq, b_wq) if which == 0 else (wk, b_wk)
                        dstT, b_dst = (qT, b_q) if which == 0 else (kT, b_k)
                        g = hf * 2 + tg
                        lc = slice(tg * 512, (tg + 1) * 512)
                        gc = slice(g * 512, (g + 1) * 512)
                        pbi = 6

                        def pmm(h):
                            ins = None
                            for k in range(8):
                                ins = h.matmul(bank[pbi][:], lhsT=wsel[0][wi][:, k, :], rhs=hT[:, k, lc], start=(k == 0), stop=(k == 7))
                            return ins
                        if part == 0:
                            S.op("pe", pmm, reads=[wsel[1][wi], b_hT[tg]], writes=[pb[pbi]])
                        if is_m and part == 0:
                            hidx = hd * 2 + which
                            pd = pdb[tg]
                            kb = which * 4 + hd
                            if g == 0:
                                S.op("dve", lambda h: h.memset(pd[:, 0:3], 0.0), writes=[b_pdb[tg]])
                            elif tg == 0:
                                S.op("dve", lambda h: h.tensor_copy(out=pd[:, 0:3], in_=halo[:, hidx, 0:3]),
                                     reads=[b_halo[hidx]], writes=[b_pdb[tg]])
                            else:
                                S.op("dve", lambda h: h.tensor_copy(out=pd[:, 0:3], in_=halo2[:, which, 0:3]),
                                     reads=[b_halo2[which]], writes=[b_pdb[tg]])
                            if tg == 0:
                                for j in range(4):
                                    S.op("dve", lambda h, j=j: h.tensor_scalar(out=dg[:, which, j, :], in0=identb[:], scalar1=convw[:, l, j, kb:kb + 1],
                                                                             scalar2=None, op0=ALU.mult),
                                         reads=[b_const, cb["convw"]], writes=[b_dg[which]])
                            S.op("act", lambda h: h.activation(out=pd[:, 3:515], in_=bank[pbi][:], func=AF.Copy),
                                 writes=[pb[pbi], b_pdb[tg]])
                            if tg == 1:
                                S.op("dve", lambda h: h.tensor_copy(out=halo[:, hidx, 0:3], in_=pd[:, 512:515]),
                                     reads=[b_pdb[tg]], writes=[b_halo[hidx]])
                            else:
                                S.op("dve", lambda h: h.tensor_copy(out=halo2[:, which, 0:3], in_=pd[:, 512:515]),
                                     reads=[b_pdb[tg]], writes=[b_halo2[which]])
                        if is_m and part == 1:
                            pd = pdb[tg]
                            kb = which * 4 + hd

                            def cmm(h):
                                ins = None
                                for j in range(4):
                                    ins = h.matmul(bank[pbi][:], lhsT=dg[:, which, j, :], rhs=pd[:, j:j + 512], start=(j == 0), stop=(j == 3))
                                return ins
                            S.op("pe", cmm, reads=[b_dg[which], b_pdb[tg]], writes=[pb[pbi]])
                            S.op("act", lambda h: h.activation(out=acc[:], in_=bank[pbi][:], func=AF.Identity, bias=convb[:, l, kb:kb + 1]),
                                 reads=[cb["convb"]], writes=[pb[pbi], b_acc])
                            S.op("act", lambda h: h.activation(out=th[:], in_=acc[:], func=AF.Tanh, scale=0.5), reads=[b_acc], writes=[b_th])
                            S.op("dve", lambda h: h.scalar_tensor_tensor(
                                out=dstT[:, lc], in0=th[:], scalar=1.0, in1=acc[:], op0=ALU.add, op1=ALU.mult),
                                reads=[b_th, b_acc], writes=[b_dst[tg]])
                        if (not is_m) and part == 0:
                            S.op("act", lambda h: h.activation(out=th[0:64, :], in_=bank[pbi][64:128, :], func=AF.Copy),
                                 writes=[pb[pbi], b_th])
                            S.op("act", lambda h: h.activation(out=th[64:128, :], in_=bank[pbi][0:64, :], func=AF.Copy),
                                 writes=[pb[pbi], b_th])
                            S.op("dve", lambda h: h.tensor_tensor(out=acc[:], in0=bank[pbi][:], in1=cosT[:, gc], op=ALU.mult),
                                 reads=[b_cs], writes=[pb[pbi], b_acc])
                        if (not is_m) and part == 1:
                            S.op("dve", lambda h: h.tensor_tensor(out=th[:], in0=th[:], in1=sinS[:, gc], op=ALU.mult),
                                 reads=[b_cs], writes=[b_th])
                            if which == 0:
                                S.op("dve", lambda h: h.tensor_tensor(out=acc[:], in0=acc[:], in1=th[:], op=ALU.add),
                                     reads=[b_th], writes=[b_acc])
                                S.op("act", lambda h: h.activation(out=qT[:, lc], in_=acc[:], func=AF.Copy),
                                     reads=[b_acc], writes=[b_q[tg]])
                                S.op("dve", lambda h: h.tensor_tensor(
                                    out=qxT[:, lc].rearrange("p (c l) -> p c l", c=4), in0=acc[:].rearrange("p (c l) -> p c l", c=4),
                                    in1=xi[:, hd:hd + 1, :].to_broadcast([128, 4, 128]), op=ALU.mult),
                                    reads=[b_acc, cb["xi"]], writes=[b_qx[tg]])
                            else:
                                S.op("dve", lambda h: h.tensor_tensor(out=kT[:, lc], in0=acc[:], in1=th[:], op=ALU.add),
                                     reads=[b_th, b_acc], writes=[b_k[tg]])

                    PIECES = [(0, 0), (1, 0), (0, 1), (1, 1)]

                    def make_stages(task):
                        is_m, hd, wi = task
                        qT = qTs[wi]; kT = kTs[wi]; qxT = qxTs[wi]
                        b_q = b_qs_[wi]; b_k = b_ks_[wi]; b_qx = b_qxs_[wi]
                        stt = (C32[hd], Cb[hd], b_C[hd], b_Cb[hd]) if is_m else (R32[hd], Rb[hd], b_R[hd], b_Rb[hd])
                        NW = 257 if is_m else 256
                        qsrc = qT if is_m else qxT
                        b_qs = b_q if is_m else b_qx
                        kap = KAPPA_M if is_m else KAPPA_R
                        VB = (7, 1); SB = (4, 4); OB = (2, 0)

                        def stage_A(c, idx):
                            tg = c // 4
                            cc = slice(c * 128, (c + 1) * 128)
                            vi = idx % 2
                            vb = VB[vi]; sbk = SB[vi]

                            def vmm(h):
                                ins = None
                                for k in range(8):
                                    ins = h.matmul(bank[vb][:], lhsT=hT[:, k, cc], rhs=wvo[wi][:, k, :], start=(k == 0), stop=(k == 7))
                                return ins
                            S.op("pe", vmm, reads=[b_hT[tg], b_wvo[wi]], writes=[pb[vb]])
                            S.op("act", lambda h: h.activation(out=vext[vi][:, 0:256], in_=bank[vb][:, 0:256], func=AF.Copy),
                                 writes=[pb[vb], b_v[vi]])
                            S.op("act", lambda h: h.activation(out=tho[vi][:], in_=bank[vb][:, 256:512], func=AF.Tanh, scale=0.5),
                                 writes=[pb[vb], b_tho[vi]])
                            if is_m:
                                S.op("dve", lambda h: h.tensor_scalar(out=gsig[idx % 3][:], in0=tho[vi][:], scalar1=1.0, scalar2=None, op0=ALU.add),
                                     reads=[b_tho[vi]], writes=[b_gs[idx % 3]])
                            else:
                                S.op("dve", lambda h: h.scalar_tensor_tensor(
                                    out=gsig[idx % 3][:], in0=tho[vi][:], scalar=1.0, in1=bank[vb][:, 256:512], op0=ALU.add, op1=ALU.mult),
                                    reads=[b_tho[vi]], writes=[pb[vb], b_gs[idx % 3]])
                            S.op("pe", lambda h: h.matmul(bank[sbk][:, 0:128], lhsT=kT[:, cc], rhs=qT[:, cc], start=True, stop=True),
                                 reads=[b_k[tg], b_q[tg]], writes=[pb[sbk]])
                            if is_m:
                                S.op("dve", lambda h: h.scalar_tensor_tensor(
                                    out=PT[vi][:], in0=bank[sbk][:, 0:128], scalar=w_t[:, c, hd:hd + 1], in1=maskm[:], op0=ALU.mult, op1=ALU.mult),
                                    reads=[b_g, cb["maskm"]], writes=[pb[sbk], b_PT[vi]])
                            else:
                                S.op("dve", lambda h: h.tensor_tensor(out=PT[vi][:], in0=bank[sbk][:, 0:128], in1=retd[:, hd, :], op=ALU.mult),
                                     reads=[cb["retd"]], writes=[pb[sbk], b_PT[vi]])
                            psT = bank[5][:, 0:64].bitcast(BF16)
                            S.op("pe", lambda h: h.transpose(psT, kT[:, cc], identb[:]), reads=[b_k[tg], b_const], writes=[pb[5]])
                            if is_m:
                                S.op("act", lambda h: h.activation(out=kw[vi][:], in_=psT, func=AF.Copy, scale=wdec_t[:, c, hd:hd + 1]),
                                     reads=[b_g], writes=[pb[5], b_kw[vi]])
                            else:
                                S.op("act", lambda h: h.activation(out=kw[vi][:], in_=psT, func=AF.Copy, scale=zeta[:, hd:hd + 1]),
                                     reads=[cb["zeta"]], writes=[pb[5], b_kw[vi]])

                        def stage_B(c, idx):
                            gci = hf * 8 + c
                            tg = c // 4
                            cc = slice(c * 128, (c + 1) * 128)
                            vi = idx % 2
                            ob_ = OB[vi]

                            def omm(h):
                                ins = h.matmul(bank[ob_][:, 0:NW], lhsT=PT[vi][:], rhs=vext[vi][:, 0:NW], start=True, stop=(gci == 0))
                                if gci > 0:
                                    ins = h.matmul(bank[ob_][:, 0:NW], lhsT=qsrc[:, cc], rhs=stt[1][:, 0:NW], start=False, stop=True)
                                return ins
                            S.op("pe", omm, reads=[b_PT[vi], b_v[vi], b_qs[tg], stt[3]], writes=[pb[ob_]])
                            S.op("pe", lambda h: h.matmul(bank[3][:, 0:NW], lhsT=kw[vi][:], rhs=vext[vi][:, 0:NW], start=True, stop=True),
                                 reads=[b_kw[vi], b_v[vi]], writes=[pb[3]])
                            if gci == 0:
                                S.op("dve", lambda h: h.tensor_copy(out=stt[0][:, 0:NW], in_=bank[3][:, 0:NW]),
                                     writes=[pb[3], stt[2]])
                            elif is_m:
                                S.op("dve", lambda h: h.scalar_tensor_tensor(
                                    out=stt[0][:, 0:NW], in0=stt[0][:, 0:NW], scalar=dec_t[:, c, hd:hd + 1], in1=bank[3][:, 0:NW],
                                    op0=ALU.mult, op1=ALU.add), reads=[b_g], writes=[pb[3], stt[2]])
                            else:
                                gam = (1.0 - 2.0 ** (-5.0 - hd)) ** 128
                                S.op("dve", lambda h: h.scalar_tensor_tensor(
                                    out=stt[0][:, 0:NW], in0=stt[0][:, 0:NW], scalar=float(gam), in1=bank[3][:, 0:NW],
                                    op0=ALU.mult, op1=ALU.add), writes=[pb[3], stt[2]])
                            S.op("act", lambda h: h.activation(out=stt[1][:, 0:NW], in_=stt[0][:, 0:NW], func=AF.Copy, scale=float(kap)),
                                 reads=[stt[2]], writes=[stt[3]])
                            tny = tinys[idx % 2]; b_tny = b_tinys[idx % 2]
                            if is_m:
                                S.op("dve", lambda h: h.tensor_scalar(out=tny[:, 0:1], in0=bank[ob_][:, 256:257], scalar1=el_t[:, c, hd:hd + 1],
                                                                      scalar2=None, op0=ALU.mult), reads=[b_g], writes=[pb[ob_], b_tny])
                                S.op("dve", lambda h: h.scalar_tensor_tensor(out=tny[:, 1:2], in0=tny[:, 0:1], scalar=-1.0, in1=tny[:, 0:1],
                                                                             op0=ALU.mult, op1=ALU.max), writes=[b_tny])
                                S.op("dve", lambda h: h.tensor_scalar(out=tny[:, 2:3], in0=tny[:, 1:2], scalar1=1.0, scalar2=None, op0=ALU.max),
                                     writes=[b_tny])
                                S.op("dve", lambda h: h.reciprocal(out=tny[:, 3:4], in_=tny[:, 2:3]), writes=[b_tny])
                                S.op("dve", lambda h: h.tensor_scalar(out=tny[:, 4:5], in0=tny[:, 3:4], scalar1=el_t[:, c, hd:hd + 1],
                                                                      scalar2=None, op0=ALU.mult), reads=[b_g], writes=[b_tny])
                                S.op("act", lambda h: h.activation(out=tho[vi][:], in_=bank[ob_][:, 0:256], func=AF.Square, scale=tny[:, 4:5],
                                                                   accum_out=tny[:, 5:6]), reads=[b_tny], writes=[pb[ob_], b_tho[vi], b_tny])
                            else:
                                S.op("act", lambda h: h.activation(out=tho[vi][:], in_=bank[ob_][:, 0:256], func=AF.Square,
                                                                   accum_out=tny[:, 5:6]), writes=[pb[ob_], b_tho[vi], b_tny])

                        def stage_C1b(c, idx):
                            vi = idx % 2
                            ob_ = OB[vi]
                            tny = tinys[idx % 2]; b_tny = b_tinys[idx % 2]
                            S.op("dve", lambda h: h.tensor_scalar(out=tny[:, 6:7], in0=tny[:, 5:6], scalar1=4.0 / 256.0, scalar2=4.0 * EPS,
                                                                  op0=ALU.mult, op1=ALU.add), writes=[b_tny])
                            S.op("pool", lambda h: h.tensor_tensor(out=tny[:, 7:8], in0=tny[:, 6:7], in1=mhalf[:, 0:1], op=ALU.pow),
                                 reads=[b_const], writes=[b_tny])
                            if is_m:
                                S.op("dve", lambda h: h.tensor_tensor(out=tny[:, 8:9], in0=tny[:, 7:8], in1=tny[:, 4:5], op=ALU.mult), writes=[b_tny])
                                sc_ap = tny[:, 8:9]
                            else:
                                sc_ap = tny[:, 7:8]
                            S.op("dve", lambda h: h.scalar_tensor_tensor(
                                out=ymc[vi][:], in0=bank[ob_][:, 0:256], scalar=sc_ap, in1=gsig[idx % 3][:], op0=ALU.mult, op1=ALU.mult),
                                reads=[b_tny, b_gs[idx % 3]], writes=[pb[ob_], b_ymc[vi]])


                        def stage_C2(c, idx):
                            cc = slice(c * 128, (c + 1) * 128)
                            vi = idx % 2
                            psY = bank[5][:, 64:192].bitcast(BF16).rearrange("p (a b) -> p a b", a=2)

                            def ytr(h):
                                h.transpose(psY[:, 0, :], ymc[vi][:, 0:128], identb[:])
                                return h.transpose(psY[:, 1, :], ymc[vi][:, 128:256], identb[:])
                            S.op("pe", ytr, reads=[b_ymc[vi], b_const], writes=[pb[5]])
                            bri = 0 if is_m else 1
                            S.op("act", lambda h: h.activation(out=ymT[:, hd * 2, cc], in_=psY[:, 0, :], func=AF.Copy, scale=ngT[:, bri, l, hd * 2:hd * 2 + 1]),
                                 reads=[cb["ngT"]], writes=[pb[5], b_ymT[c]])
                            S.op("act", lambda h: h.activation(out=ymT[:, hd * 2 + 1, cc], in_=psY[:, 1, :], func=AF.Copy, scale=ngT[:, bri, l, hd * 2 + 1:hd * 2 + 2]),
                                 reads=[cb["ngT"]], writes=[pb[5], b_ymT[c]])

                        return {"A": stage_A, "B": stage_B, "C1b": stage_C1b, "C2": stage_C2}

                    def branch_proj(is_m):
                        if l == 0 and hf == 0:
                            dump("ymT" if is_m else "yrT", ymT[:], b_ymT[7], [128, 8, 1024], BF16)
                        wsrc = W_BM if is_m else W_BR
                        og = OFF_GA if is_m else OFF_GB
                        for j in range(8):
                            ji = j % 2
                            S.dma("pool", lambda h, j=j, ji=ji, wsrc=wsrc: h.dma_start(
                                out=wb[ji][:], in_=wsrc[l, :, j * 128:(j + 1) * 128].rearrange("(k p) n -> p k n", p=128)), writes=[b_wb[ji]])
                            load_w(wg[ji][:], og + j * 128, 128, b_wg[ji])
                            for tg in range(2):
                                lc = slice(tg * 512, (tg + 1) * 512)
                                nn = (j * 2 + tg) % 2
                                bA = 0 + 2 * nn
                                bB = 1 + 2 * nn
                                tb_ = pad[nn][:, 0:512]
                                b_tb = b_pad[nn]
                                tmp_ = acc if nn == 0 else th
                                b_tmp = b_acc if nn == 0 else b_th

                                def bmm(h, ji=ji, lc=lc, bA=bA):
                                    ins = None
                                    for k in range(8):
                                        ins = h.matmul(bank[bA][:], lhsT=wb[ji][:, k, :], rhs=ymT[:, k, lc], start=(k == 0), stop=(k == 7))
                                    return ins
                                S.op("pe", bmm, reads=[b_wb[ji]] + b_ymT[tg * 4:(tg + 1) * 4], writes=[pb[bA]])

                                def gmm2(h, ji=ji, lc=lc, bB=bB):
                                    ins = None
                                    for k in range(8):
                                        ins = h.matmul(bank[bB][:], lhsT=wg[ji][:, k, :], rhs=hT[:, k, lc], start=(k == 0), stop=(k == 7))
                                    return ins
                                S.op("pe", gmm2, reads=[b_wg[ji], b_hT[tg]], writes=[pb[bB]])
                                S.op("act", lambda h: h.activation(out=tb_, in_=bank[bB][:], func=AF.Tanh, scale=0.5), writes=[pb[bB], b_tb])
                                if is_m:
                                    S.op("dve", lambda h: h.scalar_tensor_tensor(
                                        out=yT[:, j, lc], in0=tb_, scalar=1.0, in1=bank[bA][:], op0=ALU.add, op1=ALU.mult),
                                        reads=[b_tb], writes=[pb[bA], b_yT[tg]])
                                else:
                                    S.op("dve", lambda h: h.scalar_tensor_tensor(
                                        out=tmp_[:], in0=tb_, scalar=1.0, in1=bank[bA][:], op0=ALU.add, op1=ALU.mult),
                                        reads=[b_tb], writes=[pb[bA], b_tmp])
                                    S.op("dve", lambda h: h.tensor_tensor(out=yT[:, j, lc], in0=yT[:, j, lc], in1=tmp_[:], op=ALU.add),
                                         reads=[b_tmp], writes=[b_yT[tg]])
                    def run_stream(tis):
                        stg = {ti: make_stages(tasks[ti]) for ti in tis}
                        seq = [(ti, c) for ti in tis for c in range(8)]
                        n = len(seq)

                        def call(kind, i):
                            ti, c = seq[i]
                            stg[ti][kind](c, i)
                        call("A", 0)
                        for i in range(n):
                            ti, c = seq[i]
                            nxt = tasks[ti + 1] if ti + 1 < 8 else None
                            if c == 0:
                                if ti + 2 < 8 and ti + 2 != 5:
                                    loads_qk(tasks[ti + 2])
                                if ti == 4:
                                    loads_qk(tasks[5])
                                if nxt is not None:
                                    loads_vo(nxt)
                            if i + 1 < n:
                                call("A", i + 1)
                            call("B", i)
                            if i >= 1:
                                call("C1b", i - 1)
                            if i >= 2:
                                call("C2", i - 2)
                            if nxt is not None:
                                w_, t_ = PIECES[c // 2]
                                prologue_piece(nxt, w_, t_, c % 2)
                        call("C1b", n - 1)
                        call("C2", n - 2)
                        call("C2", n - 1)

                    loads_vo(tasks[0])
                    for w_, t_ in PIECES:
                        prologue_piece(tasks[0], w_, t_, 0)
                        prologue_piece(tasks[0], w_, t_, 1)
                    run_stream([0, 1, 2, 3])
                    branch_proj(True)
                    run_stream([4, 5, 6, 7])
                    branch_proj(False)
                    if l == 0 and hf == 0:
                        dump("yT", yT[:], b_yT[1], [128, 8, 1024], BF16)
                    for j in range(8):
                        ji = j % 2
                        S.dma("pool", lambda h, j=j, ji=ji: h.dma_start(
                            out=wb[ji][:], in_=W_OUT[l, :, j * 128:(j + 1) * 128].rearrange("(k p) n -> p k n", p=128)), writes=[b_wb[ji]])
                        for tg in range(2):
                            g = hf * 2 + tg
                            lc = slice(tg * 512, (tg + 1) * 512)
                            gc = slice(g * 512, (g + 1) * 512)

                            def omm2(h, ji=ji, lc=lc, tg=tg):
                                ins = None
                                for k in range(8):
                                    ins = h.matmul(bank[tg][:], lhsT=wb[ji][:, k, :], rhs=yT[:, k, lc], start=(k == 0), stop=(k == 7))
                                return ins
                            S.op("pe", omm2, reads=[b_wb[ji], b_yT[tg]], writes=[pb[tg]])
                            S.op("dve", lambda h, j=j, gc=gc, tg=tg: h.scalar_tensor_tensor(
                                out=xT[:, j, gc], in0=bank[tg][:], scalar=g1h[:, l, j:j + 1], in1=xT[:, j, gc], op0=ALU.mult, op1=ALU.add),
                                reads=[b_mod], writes=[pb[tg], b_xT[g]])
                S.barrier()
            if stop_after == "mix%d" % l:
                return finish(nc, S, st, final_evs, xT, b_xT, bank, pb, ident, FING, cb, mhalf, OUT, sbt)

            with ExitStack() as mo:
                h2T = sbt(mo, "h2T", [128, 8, S_LEN], BF16); b_h2 = [Buf("h2_%d" % i) for i in range(4)]
                gT = sbt(mo, "gT", [16, S_LEN]); b_gT = [Buf("gT%d" % i) for i in range(4)]
                wgt = [sbt(mo, "wgt%d" % i, [128, 8, 512], BF16) for i in range(2)]; b_wgt = [Buf("wgt%d" % i) for i in range(2)]
                wup = [sbt(mo, "wup%d" % i, [128, 8, 512], BF16) for i in range(2)]; b_wup = [Buf("wup%d" % i) for i in range(2)]
                wdn = [sbt(mo, "wdn%d" % i, [128, 4, D], BF16) for i in range(2)]; b_wdn = [Buf("wdn%d" % i) for i in range(2)]
                Ee = [sbt(mo, "Ee%d" % i, [16, 128]) for i in range(2)]; b_Ee = [Buf("Ee%d" % i) for i in range(2)]
                def load_e(e):
                    ei = e % 2
                    S.op("pool", lambda h: h.tensor_scalar(out=Ee[ei][:], in0=ones32[0:16, :], scalar1=ident[0:16, e:e + 1], scalar2=None,
                                                           op0=ALU.mult), reads=[b_const, cb["ident"]], writes=[b_Ee[ei]])
                    S.dma("pool", lambda h: h.dma_start(out=wgt[ei][:], in_=W_GATE[l, e].rearrange("(k p) f -> p k f", p=128)), writes=[b_wgt[ei]])
                    S.dma("pool", lambda h: h.dma_start(out=wup[ei][:], in_=W_UP[l, e].rearrange("(k p) f -> p k f", p=128)), writes=[b_wup[ei]])
                    S.dma("pool", lambda h: h.dma_start(out=wdn[ei][:], in_=W_DOWN[l, e].rearrange("(k p) n -> p k n", p=128)), writes=[b_wdn[ei]])

                load_e(0)
                load_e(1)
                mo1 = ExitStack(); mo1.__enter__()
                rts = [sbt(mo1, "rt%d" % i, [128, 4, 64]) for i in range(2)]; b_rts = [Buf("rt%d" % i) for i in range(2)]
                gfull = sbt(mo1, "gfull", [128, 4, 16]); b_gf = Buf("gfull")
                wr = sbt(mo1, "wr_s", [128, 8, 20]); brs = sbt(mo1, "br_s", [128, 4, 20])
                cb["wr"] = Buf("c_wr"); cb["br"] = Buf("c_br")
                S.dma("sp", lambda h: h.dma_start(out=wr[:], in_=WR[:, l * 160:(l + 1) * 160].rearrange("p (k n) -> p k n", k=8)), writes=[cb["wr"]])
                S.dma("sp", lambda h: h.dma_start(out=brs[:], in_=BR[:, l * 80:(l + 1) * 80].rearrange("p (t n) -> p t n", t=4)), writes=[cb["br"]])
                h32k = [sbt(mo1, "h32k%d" % i, [128, 512]) for i in range(2)]; b_h32k = [Buf("h32k%d" % i) for i in range(2)]
                lgT = sbt(mo1, "lgT", [20, 512]); b_lgT = Buf("lgT")
                sq = [sbt(mo1, "msq%d" % i, [128, 512]) for i in range(2)]; b_sq = [Buf("msq%d" % i) for i in range(2)]
                ssb = sbt(mo1, "mssb", [128, 512]); rstd = sbt(mo1, "mrstd", [128, 512]); b_ss = Buf("mss"); b_rstd = Buf("mrstd")
                psL = bank[7][:, 0:80].rearrange("p (t n) -> p t n", t=4)
                psLT = bank[6][0:20, :]

                def norm_part(g):
                    cols = slice(g * 512, (g + 1) * 512)
                    for k in range(8):
                        S.op("act", lambda h, k=k: h.activation(out=sq[k % 2][:], in_=xT[:, k, cols], func=AF.Square),
                             reads=[b_xT[g]], writes=[b_sq[k % 2]])
                        S.op("pe", lambda h, k=k: h.matmul(bank[0][:], lhsT=ones32[:], rhs=sq[k % 2][:], start=(k == 0), stop=(k == 7)),
                             reads=[b_sq[k % 2], b_const], writes=[pb[0]])
                    S.op("act", lambda h: h.activation(out=ssb[:], in_=bank[0][:], func=AF.Ln, bias=1024.0 * EPS),
                         writes=[pb[0], b_ss])
                    S.op("act", lambda h: h.activation(out=rstd[:], in_=ssb[:], func=AF.Exp, scale=-0.5), reads=[b_ss], writes=[b_rstd])
                    for k in range(8):
                        S.op("dve", lambda h, k=k: h.scalar_tensor_tensor(
                            out=sq[k % 2][:], in0=xT[:, k, cols], scalar=scale2[:, l, k:k + 1], in1=rstd[:], op0=ALU.mult, op1=ALU.mult),
                            reads=[b_xT[g], b_rstd, b_mod], writes=[b_sq[k % 2]])
                        S.op("act", lambda h, k=k: h.activation(out=h32k[k % 2][:], in_=sq[k % 2][:], func=AF.Identity, bias=modT[:, l, 24 + k:25 + k]),
                             reads=[b_sq[k % 2], b_mod], writes=[b_h32k[k % 2]])
                        S.op("pe", lambda h, k=k: h.matmul(psLT, lhsT=wr[:, k, :], rhs=h32k[k % 2][:], start=(k == 0), stop=(k == 7)),
                             reads=[b_h32k[k % 2], cb["wr"]], writes=[pb[6]])
                        S.op("act", lambda h, k=k: h.activation(out=h2T[:, k, cols], in_=sq[k % 2][:], func=AF.Identity, bias=modT[:, l, 24 + k:25 + k]),
                             reads=[b_sq[k % 2], b_mod], writes=[b_h2[g]])

                def router_mm(g):
                    rt = rts[g % 2]; b_rt = b_rts[g % 2]
                    S.op("act", lambda h: h.activation(out=lgT[:], in_=psLT, func=AF.Copy), writes=[pb[6], b_lgT])

                    def ltr(h):
                        ins = None
                        for tt in range(4):
                            ins = h.transpose(psL[:, tt, :], lgT[:, tt * 128:(tt + 1) * 128], ident[0:20, 0:20])
                        return ins
                    S.op("pe", ltr, reads=[b_lgT, cb["ident"]], writes=[pb[7]])
                    S.op("dve", lambda h: h.tensor_tensor(out=rt[:, :, 0:20], in0=psL, in1=brs[:, :, :], op=ALU.add),
                         reads=[cb["br"]], writes=[pb[7], b_rt])

                def bc(ap):
                    return ap.to_broadcast([128, 4, 4])

                def routing(g):
                    rt = rts[g % 2]; b_rt = b_rts[g % 2]
                    cols = slice(g * 512, (g + 1) * 512)
                    D_ = lambda fn: S.op("dve", fn, writes=[b_rt])
                    D_(lambda h: h.tensor_reduce(out=rt[:, :, 20:21], in_=rt[:, :, 0:4], axis=AX.X, op=ALU.max))
                    D_(lambda h: h.tensor_tensor(out=rt[:, :, 24:28], in0=rt[:, :, 0:4], in1=bc(rt[:, :, 20:21]), op=ALU.is_ge))
                    D_(lambda h: h.tensor_tensor(out=rt[:, :, 28:32], in0=rt[:, :, 0:4], in1=bc(rt[:, :, 20:21]), op=ALU.subtract))
                    S.op("act", lambda h: h.activation(out=rt[:, :, 28:32], in_=rt[:, :, 28:32], func=AF.Exp), writes=[b_rt])
                    D_(lambda h: h.tensor_reduce(out=rt[:, :, 21:22], in_=rt[:, :, 28:32], axis=AX.X, op=ALU.add))
                    D_(lambda h: h.reciprocal(out=rt[:, :, 22:23], in_=rt[:, :, 21:22]))
                    D_(lambda h: h.tensor_tensor(out=rt[:, :, 32:36], in0=rt[:, :, 4:8], in1=bc(rt[:, :, 24:25]), op=ALU.mult))
                    for gg in range(1, 4):
                        D_(lambda h, gg=gg: h.tensor_tensor(out=rt[:, :, 56:60], in0=rt[:, :, 4 + 4 * gg:8 + 4 * gg], in1=bc(rt[:, :, 24 + gg:25 + gg]), op=ALU.mult))
                        D_(lambda h: h.tensor_tensor(out=rt[:, :, 32:36], in0=rt[:, :, 32:36], in1=rt[:, :, 56:60], op=ALU.add))
                    D_(lambda h: h.tensor_reduce(out=rt[:, :, 36:37], in_=rt[:, :, 32:36], axis=AX.X, op=ALU.max))
                    D_(lambda h: h.tensor_tensor(out=rt[:, :, 40:44], in0=rt[:, :, 32:36], in1=bc(rt[:, :, 36:37]), op=ALU.is_ge))
                    D_(lambda h: h.scalar_tensor_tensor(out=rt[:, :, 44:48], in0=rt[:, :, 40:44], scalar=-1e30, in1=rt[:, :, 32:36],
                                                        op0=ALU.mult, op1=ALU.add))
                    D_(lambda h: h.tensor_reduce(out=rt[:, :, 37:38], in_=rt[:, :, 44:48], axis=AX.X, op=ALU.max))
                    D_(lambda h: h.tensor_tensor(out=rt[:, :, 48:52], in0=rt[:, :, 44:48], in1=bc(rt[:, :, 37:38]), op=ALU.is_ge))
                    D_(lambda h: h.tensor_tensor(out=rt[:, :, 38:39], in0=rt[:, :, 37:38], in1=rt[:, :, 36:37], op=ALU.subtract))
                    S.op("act", lambda h: h.activation(out=rt[:, :, 38:39], in_=rt[:, :, 38:39], func=AF.Exp), writes=[b_rt])
                    D_(lambda h: h.tensor_scalar(out=rt[:, :, 38:39], in0=rt[:, :, 38:39], scalar1=1.0, scalar2=None, op0=ALU.add))
                    D_(lambda h: h.reciprocal(out=rt[:, :, 39:40], in_=rt[:, :, 38:39]))
                    D_(lambda h: h.tensor_tensor(out=rt[:, :, 39:40], in0=rt[:, :, 39:40], in1=rt[:, :, 22:23], op=ALU.mult))
                    D_(lambda h: h.tensor_tensor(out=rt[:, :, 23:24], in0=rt[:, :, 22:23], in1=rt[:, :, 39:40], op=ALU.subtract))
                    D_(lambda h: h.tensor_tensor(out=rt[:, :, 52:56], in0=rt[:, :, 40:44], in1=bc(rt[:, :, 39:40]), op=ALU.mult))
                    D_(lambda h: h.tensor_tensor(out=rt[:, :, 56:60], in0=rt[:, :, 48:52], in1=bc(rt[:, :, 23:24]), op=ALU.mult))
                    D_(lambda h: h.tensor_tensor(out=rt[:, :, 52:56], in0=rt[:, :, 52:56], in1=rt[:, :, 56:60], op=ALU.add))
                    for gg in range(4):
                        S.op("dve", lambda h, gg=gg: h.tensor_tensor(out=gfull[:, :, gg * 4:(gg + 1) * 4], in0=rt[:, :, 52:56],
                                                                   in1=bc(rt[:, :, 24 + gg:25 + gg]), op=ALU.mult),
                             reads=[b_rt], writes=[b_gf])

                    def gtr(h):
                        ins = None
                        for tt in range(4):
                            ins = h.transpose(bank[5][0:16, tt * 128:(tt + 1) * 128], gfull[:, tt, :], ident[:])
                        return ins
                    S.op("pe", gtr, reads=[b_gf, cb["ident"]], writes=[pb[5]])
                    S.op("act", lambda h: h.activation(out=gT[:, cols], in_=bank[5][0:16, :], func=AF.Copy), writes=[pb[5], b_gT[g]])
                    if l == 0 and g == 0:
                        dump("gfull", gfull[:], b_gf, [128, 4, 16])

                norm_part(0)
                router_mm(0)
                for g in range(1, 4):
                    norm_part(g)
                    router_mm(g)
                    routing(g - 1)
                routing(3)
                if l == 0:
                    dump("h2T", h2T[:], b_h2[3], [128, 8, S_LEN], BF16)
                S.barrier()
                mo1.__exit__(None, None, None)
                he = [sbt(mo, "he%d" % i, [128, 4, 512], BF16) for i in range(2)]; b_he = [Buf("he%d" % i) for i in range(2)]
                Gsb = sbt(mo, "Gsb", [128, 512]); b_G = Buf("Gsb")
                tht = sbt(mo, "tht", [128, 512]); usb = sbt(mo, "usb", [128, 512])
                b_tht = Buf("tht"); b_usb = Buf("usb")
                GB = (0, 7); UB = (1, 6); DB = (2, 3, 4)

                def stage_GUDN(e, g, prev):
                    ei = e % 2
                    hi = (e * 4 + g) % 2
                    cols = slice(g * 512, (g + 1) * 512)
                    S.op("pe", lambda h: h.matmul(bank[5][:], lhsT=Ee[ei][:], rhs=gT[:, cols], start=True, stop=True),
                         reads=[b_Ee[ei], b_gT[g]], writes=[pb[5]])
                    S.op("act", lambda h: h.activation(out=Gsb[:], in_=bank[5][:], func=AF.Copy, scale=0.5), writes=[pb[5], b_G])
                    for fb in range(4):
                        gb_ = GB[fb % 2]; ub_ = UB[fb % 2]

                        def gm(h):
                            ins = None
                            for k in range(8):
                                ins = h.matmul(bank[gb_][:], lhsT=wgt[ei][:, k, fb * 128:(fb + 1) * 128], rhs=h2T[:, k, cols], start=(k == 0), stop=(k == 7))
                            return ins
                        S.op("pe", gm, reads=[b_wgt[ei], b_h2[g]], writes=[pb[gb_]])

                        def um(h):
                            ins = None
                            for k in range(8):
                                ins = h.matmul(bank[ub_][:], lhsT=wup[ei][:, k, fb * 128:(fb + 1) * 128], rhs=h2T[:, k, cols], start=(k == 0), stop=(k == 7))
                            return ins
                        S.op("pe", um, reads=[b_wup[ei], b_h2[g]], writes=[pb[ub_]])
                        S.op("act", lambda h: h.activation(out=tht[:], in_=bank[gb_][:], func=AF.Tanh, scale=0.5), writes=[pb[gb_], b_tht])
                        S.op("act", lambda h: h.activation(out=usb[:], in_=bank[ub_][:], func=AF.Copy), writes=[pb[ub_], b_usb])
                        S.op("dve", lambda h: h.tensor_tensor(out=usb[:], in0=bank[gb_][:], in1=usb[:], op=ALU.mult), writes=[pb[gb_], b_usb])
                        S.op("dve", lambda h: h.scalar_tensor_tensor(out=tht[:], in0=tht[:], scalar=1.0, in1=Gsb[:], op0=ALU.add, op1=ALU.mult),
                             reads=[b_G], writes=[b_tht])
                        S.op("pool", lambda h: h.tensor_tensor(out=he[hi][:, fb, :], in0=usb[:], in1=tht[:], op=ALU.mult),
                             reads=[b_usb, b_tht], writes=[b_he[hi]])
                        if prev is not None:
                            stage_DN(prev[0], prev[1], (2 * fb, 2 * fb + 1))

                def stage_DN(e, g, js=tuple(range(8))):
                    ei = e % 2
                    hi = (e * 4 + g) % 2
                    cols = slice(g * 512, (g + 1) * 512)
                    for j in js:
                        bi = DB[j % 3]

                        def dm(h):
                            ins = None
                            for fb in range(4):
                                ins = h.matmul(bank[bi][:], lhsT=wdn[ei][:, fb, j * 128:(j + 1) * 128], rhs=he[hi][:, fb, :], start=(fb == 0), stop=(fb == 3))
                            return ins
                        S.op("pe", dm, reads=[b_wdn[ei], b_he[hi]], writes=[pb[bi]])
                        S.op("dve", lambda h: h.scalar_tensor_tensor(
                            out=xT[:, j, cols], in0=bank[bi][:], scalar=modT[:, l, 40 + j:41 + j], in1=xT[:, j, cols], op0=ALU.mult, op1=ALU.add),
                            reads=[b_mod], writes=[pb[bi], b_xT[g]])

                seq = [(e, g) for e in range(16) for g in range(4)]
                stage_GUDN(0, 0, None)
                for i in range(1, 64):
                    e, g = seq[i]
                    pe_, pg_ = seq[i - 1]
                    stage_GUDN(e, g, (pe_, pg_))
                    if pg_ == 3 and pe_ + 2 < 16:
                        load_e(pe_ + 2)
                stage_DN(15, 3)
                S.barrier()
        return finish(nc, S, st, final_evs, xT, b_xT, bank, pb, ident, FING, cb, mhalf, OUT, sbt)


def finish(nc, S, st, final_evs, xT, b_xT, bank, pb, ident, FING, cb, mhalf, OUT, sbt):
    with ExitStack() as fs:
        fing = sbt(fs, "fing_s", [128, D]); cb["fing"] = Buf("c_fing")
        S.dma("sp", lambda h: h.dma_start(out=fing[:], in_=FING), writes=[cb["fing"]])
        ob = [sbt(fs, "ob%d" % i, [128, D]) for i in range(2)]
        b_ob = [Buf("ob%d" % i) for i in range(2)]
        fj = sbt(fs, "fjunk", [128, 512]); ft = sbt(fs, "ftiny", [128, 8]); b_fj = Buf("fj"); b_ft = Buf("ft")
        b_out = Buf("outd")
        b_mh = Buf("mh2")
        fts = [ft, sbt(fs, "ftiny2", [128, 8])]; b_fts = [b_ft, Buf("ft2")]
        fjs = [fj, sbt(fs, "fjunk2", [128, 512])]; b_fjs = [b_fj, Buf("fj2")]
        for t in range(16):
            tcs = slice(t * 128, (t + 1) * 128)
            p_ = t % 2
            ft_ = fts[p_]; b_ft_ = b_fts[p_]
            for hb in range(2):
                bk = hb + 2 * p_

                def tr(h, hb=hb, tcs=tcs, bk=bk):
                    ins = None
                    for kk in range(4):
                        ins = h.transpose(bank[bk][:, kk * 128:(kk + 1) * 128], xT[:, hb * 4 + kk, tcs], ident[:])
                    return ins
                S.op("pe", tr, reads=[b_xT[t // 4], cb["ident"]], writes=[pb[bk]])
                S.op("act", lambda h, hb=hb, bk=bk: h.activation(out=fjs[hb][:], in_=bank[bk][:], func=AF.Square, accum_out=ft_[:, hb:hb + 1]),
                     writes=[pb[bk], b_fjs[hb], b_ft_])
            S.op("dve", lambda h: h.tensor_tensor(out=ft_[:, 2:3], in0=ft_[:, 0:1], in1=ft_[:, 1:2], op=ALU.add), writes=[b_ft_])
            S.op("dve", lambda h: h.tensor_scalar(out=ft_[:, 3:4], in0=ft_[:, 2:3], scalar1=1.0 / 1024.0, scalar2=EPS, op0=ALU.mult, op1=ALU.add), writes=[b_ft_])
            S.op("pool", lambda h: h.tensor_tensor(out=ft_[:, 4:5], in0=ft_[:, 3:4], in1=mhalf[:, 0:1], op=ALU.pow), writes=[b_ft_])
            for hb in range(2):
                bk = hb + 2 * p_
                S.op("dve", lambda h, hb=hb, t=t, bk=bk: h.scalar_tensor_tensor(
                    out=ob[t % 2][:, hb * 512:(hb + 1) * 512], in0=bank[bk][:], scalar=ft_[:, 4:5], in1=fing[:, hb * 512:(hb + 1) * 512],
                    op0=ALU.mult, op1=ALU.mult), reads=[b_ft_, cb["fing"]], writes=[pb[bk], b_ob[t % 2]])
            S.dma("sp", lambda h, t=t, tcs=tcs: h.dma_start(out=OUT[tcs, :], in_=ob[t % 2][:]), reads=[b_ob[t % 2]], writes=[b_out])
        S.barrier(engs=["sp"])
        S.emit()
    return nc


_CACHE = {}


def _consts():
    f = np.float32
    idx = np.arange(128)
    c = {}
    c["c_ident"] = np.eye(128, dtype=f)
    c["c_tri"] = (idx[:, None] <= idx[None, :]).astype(f)
    c["c_maskm"] = (c["c_tri"] * KAPPA_M).astype(f)
    retd = np.zeros((128, 4, 128), f)
    xi = np.zeros((128, 4, 128), f)
    zeta = np.zeros((128, 4), f)
    for h in range(4):
        lg = math.log(1.0 - 2.0 ** (-5.0 - h))
        rel = idx[None, :] - idx[:, None]
        retd[:, h, :] = np.where(rel >= 0, np.exp(lg * np.maximum(rel, 0)), 0.0) * KAPPA_R
        xi[:, h, :] = np.exp(lg * (idx + 1.0))[None, :]
        zeta[:, h] = np.exp(lg * (127.0 - idx))
    c["c_retd"] = retd.reshape(128, 512)
    c["c_xi"] = xi.reshape(128, 512)
    c["c_zeta"] = zeta
    inv = (10000.0 ** (-np.arange(0, 128, 2, dtype=np.float32) / 128.0)).astype(f)
    c["c_invf"] = np.concatenate([inv, inv]).reshape(128, 1).astype(f)
    sel = np.zeros((16, 16, 128), f)
    for e in range(16):
        sel[e, e, :] = 1.0
    c["c_sel"] = sel.reshape(16, 16 * 128)
    return c


def _prep(inp):
    f = np.float32
    A = lambda a: np.ascontiguousarray(a, dtype=f)
    sh = {}
    sh["w_ada"] = A(inp["w_ada"])
    sh["b_adaT"] = A(inp["b_ada"].reshape(2, 48, 128).transpose(2, 0, 1).reshape(128, 96))
    sh["n1gT"] = A(inp["norm1_g"].reshape(2, 8, 128).transpose(2, 0, 1).reshape(128, 16))
    sh["n2gT"] = A(inp["norm2_g"].reshape(2, 8, 128).transpose(2, 0, 1).reshape(128, 16))
    sh["w_in"] = A(inp["w_in"])
    sh["convwT"] = A(inp["conv_w"].reshape(2, 4, 8, 128).transpose(3, 0, 1, 2).reshape(128, 64))
    sh["convbT"] = A(inp["conv_b"].reshape(2, 8, 128).transpose(2, 0, 1).reshape(128, 16))
    gb = np.concatenate([inp["m_ig_b"], inp["m_fg_b"]], axis=1)
    sh["gbias"] = A(np.broadcast_to(gb[None, :, None, :], (128, 2, 8, 8)).reshape(128, 128))
    ng = np.stack([inp["m_norm_g"].reshape(2, 8, 128), inp["r_norm_g"].reshape(2, 8, 128)], axis=0)
    sh["ngT"] = A(ng.transpose(3, 0, 1, 2).reshape(128, 32))
    sh["fing"] = A(np.broadcast_to(inp["final_g"].reshape(1, 1024), (128, 1024)))
    sh["w_bm"] = A(inp["w_bm"]); sh["w_br"] = A(inp["w_br"]); sh["w_out"] = A(inp["w_out"])
    wr = np.concatenate([inp["w_r1"], inp["w_r2"]], axis=2)
    sh["wr"] = A(wr.reshape(2, 8, 128, 20).transpose(2, 0, 1, 3).reshape(128, 320))
    br = np.concatenate([inp["b_r1"], inp["b_r2"]], axis=1)
    sh["br"] = A(np.broadcast_to(br[None, :, None, :], (128, 2, 4, 20)).reshape(128, 160))
    sh["w_gate"] = A(inp["w_gate"]); sh["w_up"] = A(inp["w_up"]); sh["w_down"] = A(inp["w_down"])
    sh.update(_consts())
    maps = []
    for b in range(NCORES):
        m = dict(sh)
        m["x"] = A(inp["x"][b])
        m["cT"] = A(inp["c"][b].reshape(8, 128).T)
        m["pos"] = np.ascontiguousarray(np.broadcast_to(inp["positions"][b].astype(np.int32)[None, :], (128, S_LEN)))
        maps.append(m)
    return maps


def kernel(**inp):
    if "nc" not in _CACHE:
        _CACHE["nc"] = build()
    nc = _CACHE["nc"]
    maps = _prep(inp)
    res = run_bass_kernel_spmd(nc, maps, core_ids=list(range(NCORES)))
    return np.stack([np.asarray(r["out"], dtype=np.float32) for r in res.results], axis=0)
```
